# Optimizing a Trainium2 kernel written in Bass

```python
import math
import jax
import jax.numpy as jnp
from jax import lax
import numpy as np

D_MODEL = 1024
BATCH = 8
SEQ = 4096
DEPTH = 4

CHUNK = 64
HEAD_DIM = 64
EPS = 1e-6

ATTN_HEADS = 8
ATTN_WIDTH = ATTN_HEADS * HEAD_DIM
LEFT_CHUNKS = 8
BAND_CHUNKS = LEFT_CHUNKS + 1
BAND_LEN = BAND_CHUNKS * CHUNK
MAX_REL = 128
REL_TABLE = MAX_REL + CHUNK

SSM_WIDTH = D_MODEL // 4
SSM_GROUP = 16
SSM_GROUPS = SSM_WIDTH // SSM_GROUP
SSM_STATE = 64
DT_MIN = 1e-3
DT_MAX = 1e-1

RET_HEADS = 4
RET_WIDTH = RET_HEADS * HEAD_DIM
ROPE_BASE = 10000.0

MIX_WIDTH = ATTN_WIDTH + SSM_WIDTH + RET_WIDTH
IN_WIDTH = 3 * ATTN_WIDTH + SSM_WIDTH + 4 * RET_WIDTH

N_GROUPS = 4
EXPERTS_PER_GROUP = 8
N_EXPERTS = N_GROUPS * EXPERTS_PER_GROUP
TOP_K = 2
EXPERT_FF = 256

kernel_name = "hybrid_streaming_encoder_hmoe"


def _rmsnorm(x, g):
    xf = x.astype(jnp.float32)
    y = xf * lax.rsqrt(jnp.mean(xf * xf, axis=-1, keepdims=True) + EPS)
    return (y * g.astype(jnp.float32)).astype(x.dtype)


def _rotary(x, pos):
    half = x.shape[-1] // 2
    inv_freq = ROPE_BASE ** (-jnp.arange(half, dtype=jnp.float32) / half)
    ang = pos.astype(jnp.float32)[:, None] * inv_freq[None, :]
    cos = jnp.cos(ang)[None, :, None, :]
    sin = jnp.sin(ang)[None, :, None, :]
    x1, x2 = x[..., :half], x[..., half:]
    return jnp.concatenate([x1 * cos - x2 * sin, x1 * sin + x2 * cos], axis=-1)


def _chunk_band_attention(q, k, v, rel_bias):
    b, s, h, dh = q.shape
    nc = s // CHUNK
    f32 = jnp.float32
    qc = q.reshape(b, nc, CHUNK, h, dh)
    pad = ((0, 0), (LEFT_CHUNKS, 0), (0, 0), (0, 0), (0, 0))
    kp = jnp.pad(k.reshape(b, nc, CHUNK, h, dh), pad)
    vp = jnp.pad(v.reshape(b, nc, CHUNK, h, dh), pad)
    band = jnp.arange(nc)[:, None] + jnp.arange(BAND_CHUNKS)[None, :]
    kb = kp[:, band].reshape(b, nc, BAND_LEN, h, dh)
    vb = vp[:, band].reshape(b, nc, BAND_LEN, h, dh)
    valid = jnp.repeat(band >= LEFT_CHUNKS, CHUNK, axis=1)
    key_off = jnp.arange(BAND_LEN) - LEFT_CHUNKS * CHUNK
    rel = key_off[None, :] - jnp.arange(CHUNK)[:, None]
    rel_idx = jnp.clip(rel, -MAX_REL, CHUNK - 1) + MAX_REL
    bias = rel_bias.astype(f32)[:, rel_idx]
    scores = jnp.einsum('bnqhd,bnkhd->bnhqk', qc, kb,
                        preferred_element_type=f32) * (dh ** -0.5) + bias
    scores = jnp.where(valid[None, :, None, None, :], scores, -jnp.inf)
    probs = jax.nn.softmax(scores, axis=-1).astype(v.dtype)
    out = jnp.einsum('bnhqk,bnkhd->bnqhd', probs, vb)
    return out.reshape(b, s, h * dh)


def _linear_recurrence_combine(left, right):
    a_l, b_l = left
    a_r, b_r = right
    return a_r * a_l, a_r * b_l + b_r


def _s5(u, a_re, a_im, log_dt, b_re, b_im, c_re, c_im, d_skip, w_glu, b_glu):
    bsz, s, _ = u.shape
    f32 = jnp.float32
    lam = lax.complex(a_re.astype(f32), a_im.astype(f32))
    dt = jnp.exp(log_dt.astype(f32))[:, None]
    a_bar = jnp.exp(lam * dt)
    b_mat = lax.complex(b_re.astype(f32), b_im.astype(f32))
    b_bar = ((a_bar - 1.0) / lam)[:, :, None] * b_mat
    ug = u.astype(f32).reshape(bsz, s, SSM_GROUPS, SSM_GROUP)
    bu = jnp.einsum('bsgc,gpc->bsgp', ug.astype(jnp.complex64), b_bar)
    a_seq = jnp.broadcast_to(a_bar, bu.shape)
    _, states = lax.associative_scan(_linear_recurrence_combine, (a_seq, bu), axis=1)
    c_mat = lax.complex(c_re.astype(f32), c_im.astype(f32))
    y = jnp.real(jnp.einsum('bsgp,gcp->bsgc', states, c_mat))
    y = y + d_skip.astype(f32).reshape(SSM_GROUPS, SSM_GROUP) * ug
    y = jax.nn.gelu(y.reshape(bsz, s, SSM_WIDTH))
    y = y * jax.nn.sigmoid(y @ w_glu.astype(f32) + b_glu.astype(f32))
    return y.astype(u.dtype)


def _retention(q, k, v, gate, gn_g, pos):
    b, s, h, dh = q.shape
    nc = s // CHUNK
    f32 = jnp.float32
    q = _rotary(q.astype(f32), pos)
    k = _rotary(k.astype(f32), pos) * (dh ** -0.5)
    v = v.astype(f32)
    log_gamma = jnp.log1p(-jnp.exp2(-5.0 - jnp.arange(h, dtype=f32)))
    t = jnp.arange(CHUNK, dtype=f32)
    diff = t[:, None] - t[None, :]
    decay = jnp.where(diff >= 0,
                      jnp.exp(log_gamma[:, None, None] * jnp.maximum(diff, 0.0)), 0.0)
    qc = q.reshape(b, nc, CHUNK, h, dh)
    kc = k.reshape(b, nc, CHUNK, h, dh)
    vc = v.reshape(b, nc, CHUNK, h, dh)
    inner = jnp.einsum('bnihd,bnjhd->bnhij', qc, kc) * decay
    inner = jnp.einsum('bnhij,bnjhe->bnihe', inner, vc)
    zeta = jnp.exp(log_gamma[:, None] * (CHUNK - 1 - t))
    kv = jnp.einsum('bnjhd,hj,bnjhe->nbhde', kc, zeta, vc)
    chunk_decay = jnp.exp(log_gamma * CHUNK)[None, :, None, None]

    def step(state, kv_n):
        return chunk_decay * state + kv_n, state

    _, prev = lax.scan(step, jnp.zeros((b, h, dh, dh), f32), kv)
    xi = jnp.exp(log_gamma[:, None] * (t + 1.0))
    cross = jnp.einsum('bnihd,nbhde,hi->bnihe', qc, prev, xi)
    y = inner + cross
    mu = jnp.mean(y, axis=-1, keepdims=True)
    var = jnp.mean(jnp.square(y - mu), axis=-1, keepdims=True)
    y = ((y - mu) * lax.rsqrt(var + EPS)).reshape(b, s, h * dh) * gn_g.astype(f32)
    return (jax.nn.silu(gate.astype(f32)) * y).astype(gate.dtype)


def _hier_moe(h, w_group, b_group, w_expert, b_expert, w_gate, w_up, w_down):
    bsz, s, d = h.shape
    f32 = jnp.float32
    t = h.reshape(bsz * s, d)
    group_logits = (t @ w_group + b_group).astype(f32)
    group_idx = jnp.argmax(group_logits, axis=-1)
    group_w = jnp.max(jax.nn.softmax(group_logits, axis=-1), axis=-1, keepdims=True)
    expert_logits = (t @ w_expert + b_expert).astype(f32)
    expert_logits = expert_logits.reshape(-1, N_GROUPS, EXPERTS_PER_GROUP)
    in_group = jnp.einsum('ng,nge->ne', jax.nn.one_hot(group_idx, N_GROUPS, dtype=f32), expert_logits)
    top_val, top_idx = lax.top_k(in_group, TOP_K)
    top_w = jax.nn.softmax(top_val, axis=-1) * group_w
    expert_id = group_idx[:, None] * EXPERTS_PER_GROUP + top_idx
    combine = jnp.sum(jax.nn.one_hot(expert_id, N_EXPERTS, dtype=f32) * top_w[..., None], axis=1)
    out = jnp.zeros((t.shape[0], d), f32)
    for gi in range(N_GROUPS):
        e = slice(gi * EXPERTS_PER_GROUP, (gi + 1) * EXPERTS_PER_GROUP)
        hg = jnp.einsum('nd,edf->nef', t, w_gate[e])
        hu = jnp.einsum('nd,edf->nef', t, w_up[e])
        act = jax.nn.silu(hg) * hu * combine[:, e, None].astype(t.dtype)
        out = out + jnp.einsum('nef,efd->nd', act, w_down[e]).astype(f32)
    return out.astype(h.dtype).reshape(bsz, s, d)


def setup_inputs(seed: int = 0) -> dict:
    key = jax.random.key(seed)
    ks = iter(jax.random.split(key, 32))
    f32 = jnp.float32

    def nrm(shape, scale):
        return jax.random.normal(next(ks), shape, f32) * scale

    L, D = DEPTH, D_MODEL
    G, P, Cg = SSM_GROUPS, SSM_STATE, SSM_GROUP
    x = nrm((BATCH, SEQ, D), 1.0)
    c = nrm((BATCH, D), 1.0)
    norm1_g = 1.0 + nrm((L, D), 0.05)
    norm2_g = 1.0 + nrm((L, D), 0.05)
    w_ada = nrm((L, D, 6 * D), 0.5 * D ** -0.5)
    b_ada = nrm((L, 6 * D), 0.01)
    w_in = nrm((L, D, IN_WIDTH), D ** -0.5)
    attn_rel_bias = nrm((L, ATTN_HEADS, REL_TABLE), 0.2)
    ssm_a_re = -0.5 + nrm((L, G, P), 0.01)
    ssm_a_im = jnp.tile(math.pi * jnp.arange(P, dtype=f32), (L, G, 1))
    ssm_log_dt = jax.random.uniform(next(ks), (L, G), f32, math.log(DT_MIN), math.log(DT_MAX))
    ssm_b_re = nrm((L, G, P, Cg), (2 * Cg) ** -0.5)
    ssm_b_im = nrm((L, G, P, Cg), (2 * Cg) ** -0.5)
    ssm_c_re = nrm((L, G, Cg, P), (2 * P) ** -0.5)
    ssm_c_im = nrm((L, G, Cg, P), (2 * P) ** -0.5)
    ssm_d = nrm((L, SSM_WIDTH), 1.0)
    ssm_w_glu = nrm((L, SSM_WIDTH, SSM_WIDTH), SSM_WIDTH ** -0.5)
    ssm_b_glu = nrm((L, SSM_WIDTH), 0.01)
    ret_gn_g = 1.0 + nrm((L, RET_WIDTH), 0.05)
    w_out = nrm((L, MIX_WIDTH, D), MIX_WIDTH ** -0.5)
    moe_w_group = nrm((L, D, N_GROUPS), D ** -0.5)
    moe_b_group = nrm((L, N_GROUPS), 0.01)
    moe_w_expert = nrm((L, D, N_EXPERTS), D ** -0.5)
    moe_b_expert = nrm((L, N_EXPERTS), 0.01)
    moe_w_gate = nrm((L, N_EXPERTS, D, EXPERT_FF), D ** -0.5)
    moe_w_up = nrm((L, N_EXPERTS, D, EXPERT_FF), D ** -0.5)
    moe_w_down = nrm((L, N_EXPERTS, EXPERT_FF, D), EXPERT_FF ** -0.5)
    final_g = 1.0 + nrm((D,), 0.05)
    return {"x": x, "c": c, "norm1_g": norm1_g, "norm2_g": norm2_g,
            "w_ada": w_ada, "b_ada": b_ada, "w_in": w_in, "attn_rel_bias": attn_rel_bias,
            "ssm_a_re": ssm_a_re, "ssm_a_im": ssm_a_im, "ssm_log_dt": ssm_log_dt,
            "ssm_b_re": ssm_b_re, "ssm_b_im": ssm_b_im, "ssm_c_re": ssm_c_re, "ssm_c_im": ssm_c_im,
            "ssm_d": ssm_d, "ssm_w_glu": ssm_w_glu, "ssm_b_glu": ssm_b_glu,
            "ret_gn_g": ret_gn_g, "w_out": w_out,
            "moe_w_group": moe_w_group, "moe_b_group": moe_b_group,
            "moe_w_expert": moe_w_expert, "moe_b_expert": moe_b_expert,
            "moe_w_gate": moe_w_gate, "moe_w_up": moe_w_up, "moe_w_down": moe_w_down,
            "final_g": final_g}


def reference(x, c, norm1_g, norm2_g, w_ada, b_ada, w_in, attn_rel_bias,
              ssm_a_re, ssm_a_im, ssm_log_dt, ssm_b_re, ssm_b_im, ssm_c_re, ssm_c_im,
              ssm_d, ssm_w_glu, ssm_b_glu, ret_gn_g, w_out,
              moe_w_group, moe_b_group, moe_w_expert, moe_b_expert,
              moe_w_gate, moe_w_up, moe_w_down, final_g):
    b, s, _ = x.shape
    pos = jnp.arange(s)
    cond = jax.nn.silu(c)
    split_at = np.cumsum([ATTN_WIDTH] * 3 + [SSM_WIDTH] + [RET_WIDTH] * 3).tolist()
    for i in range(DEPTH):
        mod = (cond @ w_ada[i] + b_ada[i])[:, None, :]
        sh1, sc1, g1, sh2, sc2, g2 = jnp.split(mod, 6, axis=-1)
        h = _rmsnorm(x, norm1_g[i]) * (1.0 + sc1) + sh1
        q_a, k_a, v_a, u_s, q_r, k_r, v_r, g_r = jnp.split(h @ w_in[i], split_at, axis=-1)
        y_a = _chunk_band_attention(q_a.reshape(b, s, ATTN_HEADS, HEAD_DIM),
                                    k_a.reshape(b, s, ATTN_HEADS, HEAD_DIM),
                                    v_a.reshape(b, s, ATTN_HEADS, HEAD_DIM),
                                    attn_rel_bias[i])
        y_s = _s5(u_s, ssm_a_re[i], ssm_a_im[i], ssm_log_dt[i], ssm_b_re[i], ssm_b_im[i],
                  ssm_c_re[i], ssm_c_im[i], ssm_d[i], ssm_w_glu[i], ssm_b_glu[i])
        y_r = _retention(q_r.reshape(b, s, RET_HEADS, HEAD_DIM),
                         k_r.reshape(b, s, RET_HEADS, HEAD_DIM),
                         v_r.reshape(b, s, RET_HEADS, HEAD_DIM),
                         g_r, ret_gn_g[i], pos)
        mixed = jnp.concatenate([y_a.astype(x.dtype), y_s, y_r], axis=-1) @ w_out[i]
        x = x + g1 * mixed
        h = _rmsnorm(x, norm2_g[i]) * (1.0 + sc2) + sh2
        x = x + g2 * _hier_moe(h, moe_w_group[i], moe_b_group[i], moe_w_expert[i], moe_b_expert[i],
                               moe_w_gate[i], moe_w_up[i], moe_w_down[i])
    return _rmsnorm(x, final_g)
```

```python
import math
import numpy as np
import concourse.bass as bass
import concourse.mybir as mybir
from concourse.bass_utils import run_bass_kernel_spmd

F32 = mybir.dt.float32
BF16 = mybir.dt.bfloat16
I32 = mybir.dt.int32
AF = mybir.ActivationFunctionType
ALU = mybir.AluOpType
AX = mybir.AxisListType

D = 1024
NCH = 8
IN_W = 2816
EPS = 1e-6
N_EXP = 32
FF = 256


class Buf:
    __slots__ = ("w", "rs", "name", "excl")

    def __init__(self, name="", excl=False):
        self.w = None
        self.rs = {}
        self.name = name
        self.excl = excl


class Prog:
    NRING = 8

    def __init__(self, nc):
        self.nc = nc
        self.E = {"pe": nc.tensor, "act": nc.scalar, "dve": nc.vector, "pool": nc.gpsimd, "sp": nc.sync}
        self.sem = {}
        self.cnt = {}
        for e in ("pe", "act", "dve", "pool"):
            self.sem[e] = nc.alloc_semaphore("s_" + e)
            self.cnt[e] = 0
        for q in ("sp", "act", "pool"):
            for i in range(self.NRING):
                k = "d_%s%d" % (q, i)
                self.sem[k] = nc.alloc_semaphore(k)
                self.cnt[k] = 0
        self.dma_i = {"sp": 0, "act": 0, "pool": 0}
        self.seen = {e: {} for e in ("pe", "act", "dve", "pool", "sp")}
        self.n_ins = 0

    def _deps(self, r, w, eng=None):
        deps = {}
        for b in r:
            if b.w is not None and deps.get(b.w[0], 0) < b.w[1]:
                deps[b.w[0]] = b.w[1]
            if b.excl:
                for e, c in b.rs.items():
                    if e != eng and deps.get(e, 0) < c:
                        deps[e] = c
        for b in w:
            if b.w is not None and deps.get(b.w[0], 0) < b.w[1]:
                deps[b.w[0]] = b.w[1]
            for e, c in b.rs.items():
                if deps.get(e, 0) < c:
                    deps[e] = c
        return deps

    def _wait(self, eng, deps):
        for e2, c in deps.items():
            if c <= 0:
                continue
            if e2 == eng and eng == "pe":
                continue
            if self.seen[eng].get(e2, 0) >= c:
                continue
            self.E[eng].wait_ge(self.sem[e2], c)
            self.seen[eng][e2] = c

    @staticmethod
    def _flat(bs):
        out = []
        for b in bs:
            if isinstance(b, (list, tuple)):
                out.extend(Prog._flat(b))
            else:
                out.append(b)
        return out

    def op(self, eng, fn, r=(), w=(), inc=True):
        r = self._flat(r)
        w = self._flat(w)
        self._wait(eng, self._deps(r, w, eng))
        ins = fn(self.E[eng])
        self.n_ins += 1
        if inc:
            ins.then_inc(self.sem[eng], 1)
            self.cnt[eng] += 1
            c = self.cnt[eng]
        else:
            c = self.cnt[eng] + 1
        for b in w:
            b.w = (eng, c)
            b.rs = {}
        for b in r:
            if b.rs.get(eng, 0) < c:
                b.rs[eng] = c
        return ins

    def dma(self, q, out, in_, r=(), w=(), **kw):
        i = self.dma_i[q]
        self.dma_i[q] += 1
        k = "d_%s%d" % (q, i % self.NRING)
        r = self._flat(r)
        w = self._flat(w)
        deps = self._deps(r, w)
        if self.cnt[k] > 0 and deps.get(k, 0) < self.cnt[k]:
            deps[k] = self.cnt[k]
        self._wait(q, deps)
        ins = self.E[q].dma_start(out=out, in_=in_, **kw)
        self.n_ins += 1
        ins.then_inc(self.sem[k], 16)
        self.cnt[k] += 16
        c = self.cnt[k]
        for b in w:
            b.w = (k, c)
            b.rs = {}
        for b in r:
            if b.rs.get(k, 0) < c:
                b.rs[k] = c
        return ins

    def finish(self, bufs):
        deps = {}
        for b in bufs:
            if b.w is not None and deps.get(b.w[0], 0) < b.w[1]:
                deps[b.w[0]] = b.w[1]
        for k, c in self.cnt.items():
            if c > 0 and deps.get(k, 0) < c:
                deps[k] = c
        self._wait("sp", deps)


class _EngProxy:
    def __init__(self):
        self.call = None

    def __getattr__(self, name):
        def f(*a, **k):
            self.call = (name, a, k)
            return self
        return f


class Deferred:
    def __init__(self):
        self.items = []
        self.grp = None

    def _add(self, it):
        (self.grp if self.grp is not None else self.items).append(it)

    def op(self, eng, fn, r=(), w=(), inc=True):
        px = _EngProxy()
        fn(px)
        name, a, k = px.call
        self._add(("op", (eng, (lambda e, name=name, a=a, k=k: getattr(e, name)(*a, **k))),
                   dict(r=list(r), w=list(w), inc=inc)))

    def dma(self, *a, **k):
        self._add(("dma", a, k))

    def begin(self):
        self.grp = []

    def end(self):
        g, self.grp = self.grp, None
        self.items.append(("group", g, None))


def _emit_item(P, it):
    if it[0] == "group":
        for sub in it[1]:
            _emit_item(P, sub)
    else:
        getattr(P, it[0])(*it[1], **it[2])


def run_interleaved(P, ds):
    ds = [d for d in ds if d.items]
    pos = [0] * len(ds)
    while True:
        best, bf = None, None
        for i, d in enumerate(ds):
            if pos[i] < len(d.items):
                f = pos[i] / float(len(d.items))
                if bf is None or f < bf:
                    best, bf = i, f
        if best is None:
            break
        _emit_item(P, ds[best].items[pos[best]])
        pos[best] += 1


def _rev_last(ap, n):
    pat = [list(p) for p in ap.ap]
    st = pat[-1][0]
    pat[-1] = [-st, n]
    return bass.AP(ap.tensor, ap.offset + st * (n - 1), pat)


def host_constants(S):
    cst = {}
    cst["ident"] = np.eye(128, dtype=np.float32)
    j = np.arange(128)[:, None]
    i = np.arange(128)[None, :]
    cst["tri"] = (j <= i).astype(np.float32)
    h = np.arange(4, dtype=np.float64)
    gam = 1.0 - np.exp2(-5.0 - h)
    G = np.zeros((2, 64, 2, 64))
    for c_ in range(2):
        for hh_ in range(2):
            G[hh_, :, c_, :] = gam[2 * c_ + hh_] ** 128
    cst["retG"] = G.reshape(128, 128).astype(np.float32)
    pos = np.arange(S, dtype=np.float64)
    half = 32
    inv_freq = (10000.0 ** (-np.arange(half, dtype=np.float32) / half)).astype(np.float32)
    ang = (pos.astype(np.float32)[:, None] * inv_freq[None, :]).astype(np.float32).astype(np.float64)
    cosv = np.cos(ang)
    sinv = np.sin(ang)
    il = (np.arange(S) % 128).astype(np.float64)
    facq = gam[None, :] ** (il[:, None] + 1.0)
    fack = gam[None, :] ** (-(il[:, None] + 1.0)) / 8.0
    fac = np.concatenate([facq, fack], axis=1)
    tab = np.zeros((S, 2, 8, 32), dtype=np.float64)
    tab[:, 0] = cosv[:, None, :] * fac[:, :, None]
    tab[:, 1] = sinv[:, None, :] * fac[:, :, None]
    cst["retcs"] = tab.reshape(S, 512).astype(np.float32)
    am = np.ones((128, 5, 128), dtype=np.float32)
    kk = np.arange(128)[:, None]
    qi = np.arange(128)[None, :]
    am[:, 4, :] = 1.0 - ((kk >= 64) & (qi < 64)).astype(np.float32)
    am[:, 0, :] = 1.0 - ((kk < 64) & (qi >= 64)).astype(np.float32)
    cst["amask"] = am.reshape(128, 640)
    return cst


CONST_SHAPES = lambda S: {"ident": [128, 128], "tri": [128, 128], "retG": [128, 128],
                          "retcs": [S, 512], "amask": [128, 640]}

WEIGHT_SHAPES = lambda L: {
    "norm1_g": [L, D], "norm2_g": [L, D], "w_ada": [L, D, 6 * D], "b_ada": [L, 6 * D],
    "w_in": [L, D, IN_W], "attn_rel_bias": [L, 8, 192],
    "ssm_a_re": [L, 16, 64], "ssm_a_im": [L, 16, 64], "ssm_log_dt": [L, 16],
    "ssm_b_re": [L, 16, 64, 16], "ssm_b_im": [L, 16, 64, 16],
    "ssm_c_re": [L, 16, 16, 64], "ssm_c_im": [L, 16, 16, 64],
    "ssm_d": [L, 256], "ssm_w_glu": [L, 256, 256], "ssm_b_glu": [L, 256],
    "ret_gn_g": [L, 256], "w_out": [L, D, D],
    "moe_w_group": [L, D, 4], "moe_b_group": [L, 4], "moe_w_expert": [L, D, 32], "moe_b_expert": [L, 32],
    "moe_w_gate": [L, 32, D, FF], "moe_w_up": [L, 32, D, FF], "moe_w_down": [L, 32, FF, D],
    "final_g": [D],
}


def build_program(nc, S, L, taps=(), do_mixer=True, do_moe=True, run_layers=True, **extra):
    P = Prog(nc)
    NT = S // 512
    NBLK = S // 128
    tapset = set(taps)

    def din(name, shape, dt=F32):
        return nc.dram_tensor(name, list(shape), dt, kind="ExternalInput")

    x_d = din("x", [S, D])
    c_d = din("c", [8, 128])
    Wd = {k: din(k, shp) for k, shp in WEIGHT_SHAPES(L).items()}
    Cd = {k: din(k, shp) for k, shp in CONST_SHAPES(S).items()}
    y_d = nc.dram_tensor("y", [S, D], F32, kind="ExternalOutput")
    tap_d = {}

    def tap_out(name, shape):
        tap_d[name] = nc.dram_tensor("tap_" + name, list(shape), F32, kind="ExternalOutput")
        return tap_d[name]

    xT_d = nc.dram_tensor("xT_scr", [D, S], F32, kind="Internal")
    x1T_d = nc.dram_tensor("x1T_scr", [D, S], F32, kind="Internal")
    xT_b = [Buf("xT%d" % i) for i in range(NT)]
    x1T_b = [Buf("x1T%d" % i) for i in range(NT)]
    xT_v = xT_d.ap().rearrange("(c p) t -> p c t", p=128)
    x1T_v = x1T_d.ap().rearrange("(c p) t -> p c t", p=128)
    y_b = Buf("y")

    def sb(name, shape, dt):
        return nc.alloc_sbuf_tensor("sb_" + name, list(shape), dt)

    pb = [nc.alloc_psum_tensor("pb%d" % i, [128, 512], F32) for i in range(8)]
    pbB = [Buf("pb%d" % i, excl=True) for i in range(8)]

    ident = sb("ident", [128, 128], F32)
    identb = sb("identb", [128, 128], BF16)
    ones_bf = sb("ones_bf", [128, 128], BF16)
    B_const = Buf("const")
    P.dma("sp", ident[:], Cd["ident"].ap(), w=[B_const])
    P.op("dve", lambda e: e.tensor_copy(identb[:], ident[:]), r=[B_const], w=[B_const])
    P.op("dve", lambda e: e.memset(ones_bf[:], 1.0), w=[B_const])

    def load_cols(name, rows_ap, R, tag):
        st = sb("st_" + tag, [R, 128], F32)
        stB = Buf("st_" + tag)
        P.dma("sp", st[:], rows_ap, w=[stB])
        return st, stB

    def transpose_to(dst_ap, dstB, src_ap, srcB, R, bank=7):
        P.op("pe", lambda e: e.transpose(pb[bank][:, 0:R], src_ap, ident[0:R, 0:R]),
             r=[srcB, B_const], w=[pbB[bank]])
        P.op("dve", lambda e: e.tensor_copy(dst_ap, pb[bank][:, 0:R]), r=[pbB[bank]], w=[dstB])

    condT = sb("condT", [128, 8], F32)
    condB = Buf("condT")
    st_c, st_cB = load_cols("c", c_d.ap(), 8, "c")
    transpose_to(condT[:], condB, st_c[:], st_cB, 8)
    P.op("act", lambda e: e.activation(out=condT[:], in_=condT[:], func=AF.Silu), r=[condB], w=[condB])
    condTb = sb("condTb", [128, 8], BF16)
    P.op("dve", lambda e: e.tensor_copy(condTb[:], condT[:]), r=[condB], w=[condB])
    fgT = sb("fgT", [128, 8], F32)
    fgB = Buf("fgT")
    st_f, st_fB = load_cols("fg", Wd["final_g"].ap().rearrange("(r p) -> r p", p=128), 8, "fg")
    transpose_to(fgT[:], fgB, st_f[:], st_fB, 8)

    from contextlib import ExitStack
    es0 = ExitStack()
    xin = [es0.enter_context(nc.sbuf_tensor("T0_xin%d" % i, [128, D], F32)) for i in range(2)]
    xinB = [Buf("xin%d" % i) for i in range(2)]
    xtr = [es0.enter_context(nc.sbuf_tensor("T0_xtr%d" % i, [128, NCH, 128], F32)) for i in range(2)]
    xtrB = [Buf("xtr%d" % i) for i in range(2)]
    for blk in range(NBLK):
        i2 = blk % 2
        P.dma("sp", xin[i2][:], x_d.ap()[blk * 128:(blk + 1) * 128, :], w=[xinB[i2]])
        for half in range(2):
            bank = (blk * 2 + half) % 2
            for k4 in range(4):
                k = half * 4 + k4
                P.op("pe", lambda e, k=k, k4=k4, bank=bank: e.transpose(
                    pb[bank][:, k4 * 128:(k4 + 1) * 128], xin[i2][:, k * 128:(k + 1) * 128], ident[:]),
                    r=[xinB[i2], B_const], w=[pbB[bank]], inc=(k4 == 3))
            P.op("act" if half == 0 else "dve",
                 (lambda e, half=half, bank=bank: e.copy(
                     out=xtr[i2][:, half * 4:(half + 1) * 4, :].rearrange("p a b -> p (a b)"), in_=pb[bank][:]))
                 if half == 0 else
                 (lambda e, half=half, bank=bank: e.tensor_copy(
                     xtr[i2][:, half * 4:(half + 1) * 4, :].rearrange("p a b -> p (a b)"), pb[bank][:])),
                 r=[pbB[bank]], w=[xtrB[i2]])
        P.dma("sp", xT_v[:, :, blk * 128:(blk + 1) * 128], xtr[i2][:], r=[xtrB[i2]], w=[xT_b[blk // 4]])

    barrier(P)
    es0.close()
    cur_v, cur_b = xT_v, xT_b

    ctx = dict(P=P, nc=nc, S=S, L=L, NT=NT, NBLK=NBLK, Wd=Wd, Cd=Cd, pb=pb, pbB=pbB, ident=ident, identb=identb,
               ones_bf=ones_bf, B_const=B_const, condT=condT, condTb=condTb, condB=condB, transpose_to=transpose_to,
               load_cols=load_cols, xT_v=xT_v, xT_b=xT_b, x1T_v=x1T_v, x1T_b=x1T_b, tapset=tapset,
               tap_out=tap_out, sb=sb, do_mixer=do_mixer, do_moe=do_moe)
    ctx.update(extra)
    if L > 0 and run_layers:
        cur_v, cur_b = build_layers(ctx)

    xt = [sb("fx%d" % i, [128, NCH, 512], F32) for i in range(2)]
    xtB = [Buf("fx%d" % i) for i in range(2)]
    sq = [sb("fsq%d" % i, [128, 512], BF16) for i in range(2)]
    sqB = [Buf("fsq%d" % i) for i in range(2)]
    rs = sb("frs", [128, 512], F32)
    rsB = Buf("frs")
    yo = [sb("fyo%d" % i, [128, D], F32) for i in range(2)]
    yoB = [Buf("fyo%d" % i) for i in range(2)]
    for s in range(NT):
        i2 = s % 2
        X, XB = xt[i2], xtB[i2]
        P.dma("sp", X[:], cur_v[:, :, s * 512:(s + 1) * 512], r=[cur_b[s]], w=[XB])
        rms_stats(P, pb, pbB, 0, ones_bf, B_const, X, XB, sq, sqB, rs, rsB)
        for k in range(NCH):
            P.op("dve", lambda e, k=k: e.scalar_tensor_tensor(
                out=X[:, k, :], in0=X[:, k, :], scalar=fgT[:, k:k + 1], in1=rs[:], op0=ALU.mult, op1=ALU.mult),
                r=[XB, rsB, fgB], w=[XB])
        for t in range(4):
            blk = s * 4 + t
            o2 = blk % 2
            for half in range(2):
                bank = 1 + (blk * 2 + half) % 2
                for k4 in range(4):
                    k = half * 4 + k4
                    P.op("pe", lambda e, k=k, k4=k4, bank=bank, t=t: e.transpose(
                        pb[bank][:, k4 * 128:(k4 + 1) * 128], X[:, k, t * 128:(t + 1) * 128], ident[:]),
                        r=[XB, B_const], w=[pbB[bank]], inc=(k4 == 3))
                if half == 0:
                    P.op("act", lambda e, bank=bank, o2=o2: e.copy(out=yo[o2][:, 0:512], in_=pb[bank][:]),
                         r=[pbB[bank]], w=[yoB[o2]])
                else:
                    P.op("dve", lambda e, bank=bank, o2=o2: e.tensor_copy(yo[o2][:, 512:1024], pb[bank][:]),
                         r=[pbB[bank]], w=[yoB[o2]])
            P.dma("sp", y_d.ap()[blk * 128:(blk + 1) * 128, :], yo[o2][:], r=[yoB[o2]], w=[y_b])
    P.finish([y_b])
    return P, tap_d


def rms_stats(P, pb, pbB, bank, ones_bf, B_const, X, XB, sq, sqB, rs, rsB):
    for k in range(NCH):
        P.op("act", lambda e, k=k: e.activation(out=sq[k % 2][:], in_=X[:, k, :], func=AF.Square),
             r=[XB], w=[sqB[k % 2]])
        P.op("pe", lambda e, k=k: e.matmul(pb[bank][:], lhsT=ones_bf[:], rhs=sq[k % 2][:], start=(k == 0),
                                           stop=(k == NCH - 1)),
             r=[sqB[k % 2], B_const], w=[pbB[bank]], inc=True)
    P.op("act", lambda e: e.activation(out=rs[:], in_=pb[bank][:], func=AF.Sqrt, bias=EPS, scale=1.0 / D),
         r=[pbB[bank]], w=[rsB])
    P.op("dve", lambda e: e.reciprocal(rs[:], rs[:]), r=[rsB], w=[rsB])


def barrier(P):
    for e in ("pe", "act", "dve", "pool", "sp"):
        P._wait(e, {k: c for k, c in P.cnt.items() if c > 0})


def build_layers(ctx):
    from contextlib import ExitStack
    P = ctx["P"]; nc = ctx["nc"]; S = ctx["S"]; L = ctx["L"]; NT = ctx["NT"]
    Wd = ctx["Wd"]; Cd = ctx["Cd"]; pb = ctx["pb"]; pbB = ctx["pbB"]; sb = ctx["sb"]
    ident = ctx["ident"]; identb = ctx["identb"]; ones_bf = ctx["ones_bf"]; B_const = ctx["B_const"]
    condT = ctx["condT"]; condTb = ctx["condTb"]; condB = ctx["condB"]; transpose_to = ctx["transpose_to"]
    xT_v = ctx["xT_v"]; xT_b = ctx["xT_b"]; x1T_v = ctx["x1T_v"]; x1T_b = ctx["x1T_b"]
    tapset = ctx["tapset"]; tap_out = ctx["tap_out"]
    do_attn = ctx.get("do_attn", True); do_ret = ctx.get("do_ret", True); do_s5 = ctx.get("do_s5", True); RS = ctx.get("ret_stage", 99)

    pb7b = pb[7][:].bitcast(BF16)

    stA = sb("stA", [80, 128], F32); stAB = Buf("stA")
    vecT = sb("vecT", [128, 80], F32); vecB = Buf("vecT")
    st_ld = sb("st_ld", [8, 2], F32); st2 = sb("st2", [8, 128], F32); st2B = Buf("st2")
    ldtT = sb("ldtT", [128, 8], F32); ldtB = Buf("ldtT")
    st3 = sb("st3", [4, 128], F32); st3B = Buf("st3")
    sdT = sb("sdT", [128, 4], F32); sdB = Buf("sdT")
    modT = sb("modT", [128, 48], F32); modB = Buf("modT")
    modN = sb("modN", [128, 48], F32); modNB = Buf("modN")
    gm = sb("gm", [128, 16], F32); gmB = Buf("gm")
    tri_sb = sb("tri", [128, 128], F32)
    retG_sb = sb("retG", [128, 128], F32)
    amask_sb = sb("amask", [128, 640], BF16)
    P.dma("sp", tri_sb[:], Cd["tri"].ap(), w=[B_const])
    P.dma("sp", retG_sb[:], Cd["retG"].ap(), w=[B_const])
    P.dma("pool", amask_sb[:], Cd["amask"].ap(), w=[B_const])
    Fd = nc.dram_tensor("F_scr", [L, 8, 768], F32, kind="Internal")
    FdB = Buf("Fd")

    C_BADA, C_N1, C_N2, C_ARE, C_AIM = 0, 48, 56, 64, 72

    TS = 128
    s5 = {}
    TWO_PI = 2.0 * math.pi

    def s5_setup(l, es, a):
        def sba(name, shape, dt):
            return es.enter_context(nc.sbuf_tensor("S5_%d_%s" % (l, name), list(shape), dt))
        vecT, vecB, ldtT, ldtB = a["vecT"], a["vecB"], a["ldtT"], a["ldtB"]
        Ec = sba("Ec", [128, 8, TS], F32); Es = sba("Es", [128, 8, TS], F32); EB = Buf("E")
        WB = sba("WB", [128, 16, 128], BF16); WBB = Buf("WB")
        WC = sba("WC", [128, 24, 128], BF16); WCB = Buf("WC")
        E16 = sba("E16", [128, 2, 8, TS], BF16); E16B = Buf("E16")
        wglu = sba("wglu", [128, 2, 256], BF16); wgluB = Buf("wglu")
        sm = sba("sm", [128, 24, 8], F32); smB = Buf("sm")
        smi = sba("smi", [128, 8], I32)
        Braw = sba("Braw", [128, 2, 8, 16], F32); BrawB = Buf("Braw")
        Bbar = sba("Bbar", [128, 2, 8, 16], F32); BbarB = Buf("Bbar")
        Bt = sba("Bt", [128, 2, 8, 16], F32); BtB = Buf("Bt")
        Bx = sba("Bx", [128, 128], F32); BxB = Buf("Bx")
        Cx = sba("Cx", [128, 128], F32); CxB = Buf("Cx")
        Xst = sba("Xst", [128, 2, 8], F32); XstB = Buf("Xst")
        ct = sba("ct", [128, 4], F32); ctB = Buf("ct")
        mt, mtB = a["mt"], a["mtB"]
        zb = sba("zb", [128, 2, 2, 256], BF16); zbB = [[Buf("zb%d_%d" % (s_, i)) for i in range(2)] for s_ in range(2)]
        pr = sba("pr", [128, 2, 4, 256], BF16); prB = [[Buf("pr%d_%d" % (s_, i)) for i in range(4)] for s_ in range(2)]
        glb = sba("glb", [128, 2, 512], BF16); glB = Buf("gl")
        s5.update(Ec=Ec, Es=Es, EB=EB, WB=WB, WBB=WBB, WC=WC, WCB=WCB, wglu=wglu, wgluB=wgluB, sm=sm, smB=smB,
                  Xst=Xst, XstB=XstB, ct=ct, ctB=ctB, mt=mt, mtB=mtB, zb=zb, zbB=zbB, pr=pr, prB=prB, glb=glb,
                  glB=glB, a=a, E16=E16, E16B=E16B)
        V = lambda i: sm[:, i, :]
        are, aim = vecT[:, C_ARE:C_ARE + 8], vecT[:, C_AIM:C_AIM + 8]
        DT, T1, MAG, TH, YV, KF, FR, G1, SIN, COS, AR, AI, NR, DEN, CR, CI, T2, T3 = range(18)

        def dve(fn, r=(), w=()):
            P.op("dve", fn, r=list(r) + [smB], w=list(w) + [smB])

        def act(fn, r=(), w=()):
            P.op("act", fn, r=list(r) + [smB], w=list(w) + [smB])

        P.dma("pool", wglu[:], Wd["ssm_w_glu"].ap()[l].rearrange("(k p) n -> p k n", p=128), w=[wgluB])
        act(lambda e: e.activation(out=V(DT), in_=ldtT[:], func=AF.Exp), r=[ldtB])
        dve(lambda e: e.tensor_tensor(out=V(T1), in0=are, in1=V(DT), op=ALU.mult), r=[vecB])
        act(lambda e: e.activation(out=V(MAG), in_=V(T1), func=AF.Exp))
        dve(lambda e: e.tensor_tensor(out=V(TH), in0=aim, in1=V(DT), op=ALU.mult), r=[vecB])

        def sin_of(dst, shift):
            dve(lambda e: e.tensor_scalar(out=V(YV), in0=V(TH), scalar1=1.0 / TWO_PI, scalar2=shift, op0=ALU.mult,
                                          op1=ALU.add))
            dve(lambda e: e.tensor_copy(smi[:], V(YV)))
            dve(lambda e: e.tensor_copy(V(KF), smi[:]))
            dve(lambda e: e.tensor_tensor(out=V(FR), in0=V(YV), in1=V(KF), op=ALU.subtract))
            dve(lambda e: e.tensor_single_scalar(out=V(G1), in_=V(FR), scalar=0.5, op=ALU.is_gt))
            dve(lambda e: e.tensor_tensor(out=V(FR), in0=V(FR), in1=V(G1), op=ALU.subtract))
            dve(lambda e: e.tensor_single_scalar(out=V(G1), in_=V(FR), scalar=-0.5, op=ALU.is_lt))
            dve(lambda e: e.tensor_tensor(out=V(FR), in0=V(FR), in1=V(G1), op=ALU.add))
            act(lambda e: e.activation(out=V(dst), in_=V(FR), func=AF.Sin, scale=TWO_PI))

        sin_of(SIN, 0.0)
        sin_of(COS, 0.25)
        dve(lambda e: e.tensor_tensor(out=V(AR), in0=V(MAG), in1=V(COS), op=ALU.mult))
        dve(lambda e: e.tensor_tensor(out=V(AI), in0=V(MAG), in1=V(SIN), op=ALU.mult))
        dve(lambda e: e.tensor_scalar(out=V(NR), in0=V(AR), scalar1=-1.0, scalar2=None, op0=ALU.add))
        dve(lambda e: e.tensor_tensor(out=V(DEN), in0=are, in1=are, op=ALU.mult), r=[vecB])
        dve(lambda e: e.tensor_tensor(out=V(T2), in0=aim, in1=aim, op=ALU.mult), r=[vecB])
        dve(lambda e: e.tensor_tensor(out=V(DEN), in0=V(DEN), in1=V(T2), op=ALU.add))
        dve(lambda e: e.reciprocal(V(DEN), V(DEN)))
        dve(lambda e: e.tensor_tensor(out=V(T2), in0=V(NR), in1=are, op=ALU.mult), r=[vecB])
        dve(lambda e: e.tensor_tensor(out=V(T3), in0=V(AI), in1=aim, op=ALU.mult), r=[vecB])
        dve(lambda e: e.tensor_tensor(out=V(T2), in0=V(T2), in1=V(T3), op=ALU.add))
        dve(lambda e: e.tensor_tensor(out=V(CR), in0=V(T2), in1=V(DEN), op=ALU.mult))
        dve(lambda e: e.tensor_tensor(out=V(T2), in0=V(AI), in1=are, op=ALU.mult), r=[vecB])
        dve(lambda e: e.tensor_tensor(out=V(T3), in0=V(NR), in1=aim, op=ALU.mult), r=[vecB])
        dve(lambda e: e.tensor_tensor(out=V(T2), in0=V(T2), in1=V(T3), op=ALU.subtract))
        dve(lambda e: e.tensor_tensor(out=V(CI), in0=V(T2), in1=V(DEN), op=ALU.mult))
        for ri, nm in ((0, "ssm_b_re"), (1, "ssm_b_im")):
            src = Wd[nm].ap()[l].rearrange("(cb j) p c -> (j p) cb c", j=2)
            P.dma("sp", Braw[:, ri, :, :], src, w=[BrawB])
        bc = lambda i: sm[:, i, :, None].to_broadcast([128, 8, 16])
        P.op("dve", lambda e: e.tensor_tensor(out=Bbar[:, 0], in0=Braw[:, 0], in1=bc(CR), op=ALU.mult), r=[BrawB, smB], w=[BbarB])
        P.op("dve", lambda e: e.tensor_tensor(out=Bt[:, 0], in0=Braw[:, 1], in1=bc(CI), op=ALU.mult), r=[BrawB, smB], w=[BtB])
        P.op("dve", lambda e: e.tensor_tensor(out=Bbar[:, 0], in0=Bbar[:, 0], in1=Bt[:, 0], op=ALU.subtract), r=[BbarB, BtB], w=[BbarB])
        P.op("dve", lambda e: e.tensor_tensor(out=Bbar[:, 1], in0=Braw[:, 1], in1=bc(CR), op=ALU.mult), r=[BrawB, smB], w=[BbarB])
        P.op("dve", lambda e: e.tensor_tensor(out=Bt[:, 1], in0=Braw[:, 0], in1=bc(CI), op=ALU.mult), r=[BrawB, smB], w=[BtB])
        P.op("dve", lambda e: e.tensor_tensor(out=Bbar[:, 1], in0=Bbar[:, 1], in1=Bt[:, 1], op=ALU.add), r=[BbarB, BtB], w=[BbarB])
        for cb in range(8):
            q = cb % 4
            for ri in range(2):
                P.op("dve", lambda e: e.memset(Bx[:], 0.0), w=[BxB])
                for j in range(2):
                    P.op("dve", lambda e, j=j, q=q, ri=ri, cb=cb: e.tensor_copy(
                        Bx[64 * j:64 * j + 64, 32 * q + 16 * j:32 * q + 16 * j + 16], Bbar[64 * j:64 * j + 64, ri, cb, :]),
                        r=[BbarB], w=[BxB])
                P.op("pe", lambda e: e.transpose(pb[6][:, 0:128], Bx[:], ident[:]), r=[BxB, B_const], w=[pbB[6]])
                P.op("act", lambda e, cb=cb, ri=ri: e.copy(out=WB[:, cb * 2 + ri, :], in_=pb[6][:, 0:128]),
                     r=[pbB[6]], w=[WBB])
        P.op("dve", lambda e: e.memset(WC[:].rearrange("p a b -> p (a b)"), 0.0), w=[WCB])
        for ri, nm in ((0, "ssm_c_re"), (1, "ssm_c_im")):
            for hh in range(2):
                src = Wd[nm].ap()[l][8 * hh:8 * hh + 8].rearrange("g c p -> (g c) p")
                P.dma("sp", Cx[:, 0:64], src, w=[CxB])
                P.dma("sp", Cx[:, 64:128], src, w=[CxB])
                P.op("pe", lambda e: e.transpose(pb[6][:, 0:128], Cx[:], ident[:]), r=[CxB, B_const], w=[pbB[6]])
                P.op("act", lambda e: e.copy(out=Bx[:], in_=pb[6][:, 0:128]), r=[pbB[6]], w=[BxB])
                for q in range(4):
                    cb = 4 * hh + q
                    for j in range(2):
                        cols = slice(32 * q + 16 * j, 32 * q + 16 * j + 16)
                        for (slot_, sgn) in (((0, 1.0), (2, -1.0)) if ri == 0 else ((1, -1.0),)):
                            P.op("dve", lambda e, cb=cb, slot_=slot_, sgn=sgn, j=j, cols=cols: e.tensor_scalar(
                                out=WC[64 * j:64 * j + 64, cb * 3 + slot_, cols], in0=Bx[64 * j:64 * j + 64, cols],
                                scalar1=sgn, scalar2=None, op0=ALU.mult), r=[BxB], w=[WCB])
        P.op("dve", lambda e: e.tensor_copy(Ec[:, :, 0:1], sm[:, COS, :, None]), r=[smB], w=[EB])
        P.op("dve", lambda e: e.tensor_copy(Es[:, :, 0:1], sm[:, SIN, :, None]), r=[smB], w=[EB])
        n = 1
        tA = mt[0][:].rearrange("p (a b) -> p a b", a=8)
        tB_ = mt[1][:].rearrange("p (a b) -> p a b", a=8)
        while n < TS:
            cc = Ec[:, :, n - 1:n].to_broadcast([128, 8, n])
            ss = Es[:, :, n - 1:n].to_broadcast([128, 8, n])
            P.op("dve", lambda e, n=n, cc=cc: e.tensor_tensor(out=tA[:, :, 0:n], in0=Ec[:, :, 0:n], in1=cc, op=ALU.mult), r=[EB], w=[mtB[0]])
            P.op("dve", lambda e, n=n, ss=ss: e.tensor_tensor(out=tB_[:, :, 0:n], in0=Es[:, :, 0:n], in1=ss, op=ALU.mult), r=[EB], w=[mtB[1]])
            P.op("dve", lambda e, n=n: e.tensor_tensor(out=Ec[:, :, n:2 * n], in0=tA[:, :, 0:n], in1=tB_[:, :, 0:n], op=ALU.subtract), r=[mtB[0], mtB[1]], w=[EB])
            P.op("dve", lambda e, n=n, ss=ss: e.tensor_tensor(out=tA[:, :, 0:n], in0=Ec[:, :, 0:n], in1=ss, op=ALU.mult), r=[EB], w=[mtB[0]])
            P.op("dve", lambda e, n=n, cc=cc: e.tensor_tensor(out=tB_[:, :, 0:n], in0=Es[:, :, 0:n], in1=cc, op=ALU.mult), r=[EB], w=[mtB[1]])
            P.op("dve", lambda e, n=n: e.tensor_tensor(out=Es[:, :, n:2 * n], in0=tA[:, :, 0:n], in1=tB_[:, :, 0:n], op=ALU.add), r=[mtB[0], mtB[1]], w=[EB])
            n *= 2
        P.op("dve", lambda e: e.memset(Xst[:].rearrange("p a b -> p (a b)"), 0.0), w=[XstB])
        P.op("act", lambda e: e.copy(out=E16[:, 0].rearrange("p a b -> p (a b)"), in_=Ec[:].rearrange("p a b -> p (a b)")), r=[EB], w=[E16B])
        P.op("act", lambda e: e.copy(out=E16[:, 1].rearrange("p a b -> p (a b)"), in_=Es[:].rearrange("p a b -> p (a b)")), r=[EB], w=[E16B])
        s5["MAG"] = MAG

    def s5_gen(s, P):
        a = s5["a"]
        uT32, uTb, uTB, YT, YTB, sdT, sdB = a["uT32"], a["uTb"], a["uTB"], a["YT"], a["YTB"], a["sdT"], a["sdB"]
        Ec, Es, EB, WB, WBB, WC, WCB = s5["Ec"], s5["Es"], s5["EB"], s5["WB"], s5["WBB"], s5["WC"], s5["WCB"]
        E16, E16B = s5["E16"], s5["E16B"]
        sm, smB, Xst, XstB, ct, ctB = s5["sm"], s5["smB"], s5["Xst"], s5["XstB"], s5["ct"], s5["ctB"]
        mt, mtB = s5["mt"], s5["mtB"]
        zb, zbB, pr, prB = s5["zb"], s5["zbB"], s5["pr"], s5["prB"]
        glb, glB, wglu, wgluB = s5["glb"], s5["glB"], s5["wglu"], s5["wgluB"]
        MAG = s5["MAG"]
        HW = 256
        v2 = lambda ap: ap.rearrange("p (n t) -> p n t", n=2)
        unit = 0
        for half in range(2):
            tcols = slice(half * HW, (half + 1) * HW)
            for cb in range(8):
                st_ = unit % 2
                unit += 1
                kc = cb // 4
                T = [mt[i][:, st_ * HW:(st_ + 1) * HW] for i in range(4)]
                TB = [mtB[i][st_] for i in range(4)]
                ZB, ZBB = zb[:, st_], zbB[st_]
                PR, PRB = pr[:, st_], prB[st_]
                bank = st_
                Ecb = Ec[:, cb, None, :].to_broadcast([128, 2, TS])
                Esb = Es[:, cb, None, :].to_broadcast([128, 2, TS])
                Ec16 = E16[:, 0, cb, None, :].to_broadcast([128, 2, TS])
                Es16 = E16[:, 1, cb, None, :].to_broadcast([128, 2, TS])
                P.op("pe", lambda e: e.matmul(pb[bank][:, 0:HW], lhsT=WB[:, cb * 2, :], rhs=uTb[:, kc, tcols], start=True, stop=True),
                     r=[WBB, uTB], w=[pbB[bank]], inc=False)
                P.op("pe", lambda e: e.matmul(pb[bank][:, HW:2 * HW], lhsT=WB[:, cb * 2 + 1, :], rhs=uTb[:, kc, tcols], start=True, stop=True),
                     r=[WBB, uTB], w=[pbB[bank]])
                bur, bui = v2(pb[bank][:, 0:HW]), v2(pb[bank][:, HW:2 * HW])
                P.op("dve", lambda e: e.tensor_tensor(out=v2(T[0]), in0=bur, in1=Ecb, op=ALU.mult), r=[pbB[bank], EB], w=[TB[0]])
                P.op("dve", lambda e: e.tensor_tensor(out=v2(T[1]), in0=bui, in1=Esb, op=ALU.mult), r=[pbB[bank], EB], w=[TB[1]])
                P.op("dve", lambda e: e.tensor_tensor(out=v2(T[2]), in0=bui, in1=Ecb, op=ALU.mult), r=[pbB[bank], EB], w=[TB[2]])
                P.op("dve", lambda e: e.tensor_tensor(out=v2(T[3]), in0=bur, in1=Esb, op=ALU.mult), r=[pbB[bank], EB], w=[TB[3]])
                P.op("pool", lambda e: e.tensor_tensor(out=T[0], in0=T[0], in1=T[1], op=ALU.add), r=[TB[0], TB[1]], w=[TB[0]])
                P.op("pool", lambda e: e.tensor_tensor(out=T[2], in0=T[2], in1=T[3], op=ALU.subtract), r=[TB[2], TB[3]], w=[TB[2]])
                yield
                magb = sm[:, MAG, cb:cb + 1].to_broadcast([128, TS])
                for n in range(2):
                    cs_ = slice(n * TS, (n + 1) * TS)
                    P.op("dve", lambda e: e.tensor_tensor_scan(
                        out=T[1][:, cs_], data0=magb, data1=T[0][:, cs_], initial=Xst[:, 0, cb:cb + 1], op0=ALU.mult, op1=ALU.add),
                        r=[TB[0], XstB, smB], w=[TB[1]])
                    P.op("dve", lambda e: e.tensor_tensor_scan(
                        out=T[3][:, cs_], data0=magb, data1=T[2][:, cs_], initial=Xst[:, 1, cb:cb + 1], op0=ALU.mult, op1=ALU.add),
                        r=[TB[2], XstB, smB], w=[TB[3]])
                    last = (n + 1) * TS - 1
                    zrl, zil = T[1][:, last:last + 1], T[3][:, last:last + 1]
                    ecl, esl = Ec[:, cb, TS - 1:TS], Es[:, cb, TS - 1:TS]
                    P.op("dve", lambda e: e.tensor_tensor(out=ct[:, 0:1], in0=zil, in1=esl, op=ALU.mult), r=[TB[3], EB], w=[ctB])
                    P.op("dve", lambda e: e.tensor_tensor(out=ct[:, 1:2], in0=zil, in1=ecl, op=ALU.mult), r=[TB[3], EB], w=[ctB])
                    P.op("dve", lambda e: e.scalar_tensor_tensor(
                        out=Xst[:, 0, cb:cb + 1], in0=zrl, scalar=ecl, in1=ct[:, 0:1], op0=ALU.mult, op1=ALU.subtract),
                        r=[TB[1], EB, ctB], w=[XstB])
                    P.op("dve", lambda e: e.scalar_tensor_tensor(
                        out=Xst[:, 1, cb:cb + 1], in0=zrl, scalar=esl, in1=ct[:, 1:2], op0=ALU.mult, op1=ALU.add),
                        r=[TB[1], EB, ctB], w=[XstB])
                P.op("act", lambda e: e.copy(out=ZB[:, 0, :], in_=T[1]), r=[TB[1]], w=[ZBB[0]])
                P.op("act", lambda e: e.copy(out=ZB[:, 1, :], in_=T[3]), r=[TB[3]], w=[ZBB[1]])
                yield
                for (i_, zi_, tab) in ((0, 0, Ec16), (1, 1, Es16), (2, 0, Es16), (3, 1, Ec16)):
                    P.op("dve", lambda e, i_=i_, zi_=zi_, tab=tab: e.tensor_tensor(
                        out=v2(PR[:, i_, :]), in0=v2(ZB[:, zi_, :]), in1=tab, op=ALU.mult), r=[ZBB[zi_], E16B], w=[PRB[i_]])
                for (i_, wsel) in ((0, 0), (1, 2), (2, 1), (3, 1)):
                    P.op("pe", lambda e, i_=i_, wsel=wsel: e.matmul(
                        pb[6][:, 0:HW], lhsT=WC[:, cb * 3 + wsel, :], rhs=PR[:, i_, :], start=(cb % 4 == 0 and i_ == 0),
                        stop=(cb % 4 == 3 and i_ == 3)), r=[WCB, PRB[i_]], w=[pbB[6]], inc=(i_ == 3))
                if cb % 4 == 3:
                    mc = cb // 4
                    P.op("dve", lambda e: e.scalar_tensor_tensor(
                        out=T[0], in0=uT32[:, mc, tcols], scalar=sdT[:, mc:mc + 1], in1=pb[6][:, 0:HW], op0=ALU.mult, op1=ALU.add),
                        r=[uTB, sdB, pbB[6]], w=[TB[0]])
                    P.op("act", lambda e: e.activation(out=glb[:, mc, tcols], in_=T[0], func=AF.Gelu_apprx_tanh),
                         r=[TB[0]], w=[glB])
                yield
        for m in range(2):
            P.begin()
            for k in range(2):
                P.op("pe", lambda e, m=m, k=k: e.matmul(pb[7][:], lhsT=wglu[:, k, m * 128:(m + 1) * 128], rhs=glb[:, k, :],
                                                        start=(k == 0), stop=(k == 1)), r=[wgluB, glB], w=[pbB[7]], inc=(k == 1))
            P.op("act", lambda e, m=m: e.activation(out=mt[m][:], in_=pb[7][:], func=AF.Sigmoid, bias=sdT[:, 2 + m:3 + m], scale=1.0),
                 r=[pbB[7], sdB], w=[mtB[m]])
            P.end()
            P.op("dve", lambda e, m=m: e.tensor_tensor(out=YT[:, 4 + m, :], in0=glb[:, m, :], in1=mt[m][:], op=ALU.mult),
                 r=[glB, mtB[m]], w=[YTB[4 + m]])
        yield

    ctx["s5_setup"] = s5_setup
    ctx["s5_gen"] = s5_gen

    HS = min(S, 2048)
    NHALF = S // HS
    NST = HS // 512
    BIG = 1.0e30

    def moe_phase(l, a):
        sh2, g2, gm2, modB, gmB = a["sh2"], a["g2"], a["gm2"], a["modB"], a["gmB"]
        with ExitStack() as es:
            def sba(name, shape, dt):
                return es.enter_context(nc.sbuf_tensor("B%d_%s" % (l, name), list(shape), dt))
            acc = sba("acc", [128, 8, HS], F32); accB = [Buf("acc%d" % i) for i in range(NST)]
            h2T = sba("h2T", [128, 8, HS], BF16); h2B = [Buf("h2T%d" % i) for i in range(NST)]
            h32 = sba("h32", [128, 8, 512], F32); h32B = Buf("h32")
            sq = [sba("sq%d" % i, [128, 512], BF16) for i in range(2)]; sqB = [Buf("sq%d" % i) for i in range(2)]
            rs = sba("rs", [128, 512], F32); rsB = Buf("rs")
            tmp = [sba("tmp0", [128, 512], F32)] * 2; tmpB = [Buf("tmp0")] * 2
            wr_sb = sba("wr", [128, 8, 36], F32); wrB = Buf("wr")
            rb_bc = sba("rb", [128, 36], F32); rbB = Buf("rb")
            combT = sba("combT", [128, HS], BF16); combB = [Buf("combT%d" % i) for i in range(NST)]
            sel = sba("sel", [128, 32, 128], BF16); selB = Buf("sel")
            wgu = [[sba("wgu%d_%d" % (i, j), [128, 8, 512], BF16) for j in range(2)] for i in range(2)]
            wguB = [[Buf("wgu%d_%d" % (i, j)) for j in range(2)] for i in range(2)]
            wd = [[sba("wd%d_%d" % (i, j), [128, 2, 1024], BF16) for j in range(2)] for i in range(2)]
            wdB = [[Buf("wd%d_%d" % (i, j)) for j in range(2)] for i in range(2)]
            sgt = sba("sgt", [128, 2, 512], BF16); sgB = [Buf("sg0"), Buf("sg1")]
            tt = sba("tt", [128, 2, 512], BF16); ttB = [Buf("tt0"), Buf("tt1")]
            actT = [sba("actT%d" % i, [128, 2, 2, 512], BF16) for i in range(2)]
            actB = [[[Buf("act%d_%d_%d" % (i, j, f)) for f in range(2)] for j in range(2)] for i in range(2)]
            wa2 = [sba("wa2_%d" % i, [128, 8, 128], BF16) for i in range(2)]; wa2B = [Buf("wa2_%d" % i) for i in range(2)]
            cbs = [sba("cbs%d" % i, [128, 2, 512], BF16) for i in range(2)]
            cbsB = [[Buf("cbs%d_%d" % (i, j)) for j in range(2)] for i in range(2)]
            rt = actT[0][:].rearrange("p a b c -> p (a b c)").bitcast(F32); rtB = Buf("rt")
            cB = Buf("comb")

            P.op("dve", lambda e: e.memset(sel[:].rearrange("p a b -> p (a b)"), 0.0), w=[selB])
            P.op("dve", lambda e: e.tensor_copy(sel[0:32, :, :], identb[0:32, 0:32, None].to_broadcast([32, 32, 128])),
                 r=[B_const], w=[selB])
            P.op("dve", lambda e: e.memset(combT[:], 0.0), w=combB)
            P.dma("sp", wr_sb[:, :, 0:4], Wd["moe_w_group"].ap()[l].rearrange("(k p) n -> p k n", p=128), w=[wrB])
            P.dma("sp", wr_sb[:, :, 4:36], Wd["moe_w_expert"].ap()[l].rearrange("(k p) n -> p k n", p=128), w=[wrB])
            P.dma("sp", rb_bc[:, 0:4], Wd["moe_b_group"].ap()[l:l + 1, :].to_broadcast([128, 4]), w=[rbB])
            P.dma("sp", rb_bc[:, 4:36], Wd["moe_b_expert"].ap()[l:l + 1, :].to_broadcast([128, 32]), w=[rbB])

            def load_pair(p):
                for j in range(2):
                    e = 2 * p + j
                    W_, WB_ = wgu[p % 2][j], wguB[p % 2][j]
                    P.dma("pool", W_[:, :, 0:256], Wd["moe_w_gate"].ap()[l, e].rearrange("(k p) f -> p k f", p=128), w=[WB_])
                    P.dma("pool", W_[:, :, 256:512], Wd["moe_w_up"].ap()[l, e].rearrange("(k p) f -> p k f", p=128), w=[WB_])
                    P.dma("pool", wd[p % 2][j][:], Wd["moe_w_down"].ap()[l, e].rearrange("(k p) d -> p k d", p=128),
                          w=[wdB[p % 2][j]])

            for hf in range(NHALF):
                t0 = hf * HS
                def norm_part(PP, st):
                    cols = slice(st * 512, (st + 1) * 512)
                    gs = (t0 // 512) + st
                    X = acc[:, :, cols]
                    PP.dma("sp", X, x1T_v[:, :, t0 + st * 512:t0 + (st + 1) * 512], r=[x1T_b[gs]], w=[accB[st]])
                    rms_stats(PP, pb, pbB, st % 2, ones_bf, B_const, X, accB[st], sq, sqB, rs, rsB)
                    for k in range(8):
                        T_, TB_ = tmp[k % 2], tmpB[k % 2]
                        PP.op("dve", lambda e, k=k, T_=T_, X=X: e.scalar_tensor_tensor(
                            out=T_[:], in0=X[:, k, :], scalar=gm2(k), in1=rs[:], op0=ALU.mult, op1=ALU.mult),
                            r=[accB[st], rsB, gmB], w=[TB_])
                        PP.op("act", lambda e, k=k, T_=T_: e.activation(out=h32[:, k, :], in_=T_[:], func=AF.Identity,
                                                                        bias=sh2(k), scale=1.0), r=[TB_, modB], w=[h32B])
                    PP.op("pool", lambda e, cols=cols: e.tensor_copy(h2T[:, :, cols], h32[:]), r=[h32B], w=[h2B[st]])

                def router_mm(st):
                    bk = 2 + st % 2
                    for t in range(4):
                        tok = slice(t * 128, (t + 1) * 128)
                        for k in range(8):
                            P.op("pe", lambda e, k=k, bk=bk, tok=tok, t=t: e.matmul(
                                pb[bk][:, t * 36:(t + 1) * 36], lhsT=h32[:, k, tok], rhs=wr_sb[:, k, :], start=(k == 0),
                                stop=(k == 7)), r=[h32B, wrB], w=[pbB[bk]], inc=(k == 7))

                def router_chain(PP, st):
                    bk = 2 + st % 2
                    LGS = rt[:, 0:144].rearrange("p (t x) -> p t x", t=4)
                    off = [144]

                    def alloc(n):
                        o = off[0]; off[0] += 4 * n
                        return rt[:, o:o + 4 * n].rearrange("p (t x) -> p t x", t=4)
                    GM, GD, GOH, GEX, GS, PEN = alloc(1), alloc(4), alloc(4), alloc(4), alloc(1), alloc(4)
                    MK, M1, D1, OH1, MK2, M2, OH2 = alloc(32), alloc(1), alloc(32), alloc(32), alloc(32), alloc(1), alloc(32)
                    DD, W1, W2, CMB = alloc(1), alloc(1), alloc(1), alloc(32)
                    bc = lambda ap, n: ap.to_broadcast([128, 4, n])

                    def dv(fn, extra_r=()):
                        PP.op("dve", fn, r=[rtB] + list(extra_r), w=[rtB])

                    PP.op("dve", lambda e, bk=bk: e.tensor_tensor(
                        out=LGS, in0=pb[bk][:, 0:144].rearrange("p (t x) -> p t x", t=4),
                        in1=rb_bc[:, None, :].to_broadcast([128, 4, 36]), op=ALU.add), r=[pbB[bk], rbB, rtB], w=[rtB])
                    dv(lambda e: e.tensor_reduce(out=GM, in_=LGS[:, :, 0:4], axis=AX.X, op=ALU.max))
                    dv(lambda e: e.tensor_tensor(out=GD, in0=LGS[:, :, 0:4], in1=bc(GM, 4), op=ALU.subtract))
                    dv(lambda e: e.tensor_single_scalar(out=GOH, in_=GD, scalar=0.0, op=ALU.is_equal))
                    PP.op("act", lambda e: e.activation(out=GEX, in_=GD, func=AF.Exp), r=[rtB], w=[rtB])
                    dv(lambda e: e.tensor_reduce(out=GS, in_=GEX, axis=AX.X, op=ALU.add))
                    dv(lambda e: e.reciprocal(GS, GS))
                    dv(lambda e: e.tensor_scalar(out=PEN, in0=GOH, scalar1=-1.0, scalar2=BIG, op0=ALU.add, op1=ALU.mult))
                    for t in range(4):
                        mk3 = MK[:, t, :].rearrange("p (g x) -> p g x", g=4)
                        el3 = LGS[:, t, 4:36].rearrange("p (g x) -> p g x", g=4)
                        dv(lambda e, mk3=mk3, el3=el3, t=t: e.tensor_tensor(
                            out=mk3, in0=el3, in1=GOH[:, t, :, None].to_broadcast([128, 4, 8]), op=ALU.mult))
                        dv(lambda e, mk3=mk3, t=t: e.tensor_tensor(
                            out=mk3, in0=mk3, in1=PEN[:, t, :, None].to_broadcast([128, 4, 8]), op=ALU.add))
                    dv(lambda e: e.tensor_reduce(out=M1, in_=MK, axis=AX.X, op=ALU.max))
                    dv(lambda e: e.tensor_tensor(out=D1, in0=MK, in1=bc(M1, 32), op=ALU.subtract))
                    dv(lambda e: e.tensor_single_scalar(out=OH1, in_=D1, scalar=0.0, op=ALU.is_equal))
                    dv(lambda e: e.scalar_tensor_tensor(out=MK2, in0=OH1, scalar=-BIG, in1=MK, op0=ALU.mult, op1=ALU.add))
                    dv(lambda e: e.tensor_reduce(out=M2, in_=MK2, axis=AX.X, op=ALU.max))
                    dv(lambda e: e.tensor_tensor(out=D1, in0=MK2, in1=bc(M2, 32), op=ALU.subtract))
                    dv(lambda e: e.tensor_single_scalar(out=OH2, in_=D1, scalar=0.0, op=ALU.is_equal))
                    dv(lambda e: e.tensor_tensor(out=DD, in0=M2, in1=M1, op=ALU.subtract))
                    PP.op("act", lambda e: e.activation(out=DD, in_=DD, func=AF.Exp), r=[rtB], w=[rtB])
                    dv(lambda e: e.tensor_scalar(out=W1, in0=DD, scalar1=1.0, scalar2=None, op0=ALU.add))
                    dv(lambda e: e.reciprocal(W1, W1))
                    dv(lambda e: e.tensor_tensor(out=W2, in0=DD, in1=W1, op=ALU.mult))
                    dv(lambda e: e.tensor_tensor(out=W1, in0=W1, in1=GS, op=ALU.mult))
                    dv(lambda e: e.tensor_tensor(out=W2, in0=W2, in1=GS, op=ALU.mult))
                    dv(lambda e: e.tensor_tensor(out=OH1, in0=OH1, in1=bc(W1, 32), op=ALU.mult))
                    dv(lambda e: e.tensor_tensor(out=OH2, in0=OH2, in1=bc(W2, 32), op=ALU.mult))
                    PP.op("dve", lambda e: e.tensor_tensor(out=CMB, in0=OH1, in1=OH2, op=ALU.add), r=[rtB], w=[rtB, cB])
                    for t in range(4):
                        PP.op("pe", lambda e, t=t: e.transpose(pb[4][0:32, t * 128:(t + 1) * 128], CMB[:, t, :], ident[:]),
                             r=[rtB, cB, B_const], w=[pbB[4]], inc=(t == 3))
                    PP.op("act", lambda e, st=st: e.copy(out=combT[0:32, st * 512:(st + 1) * 512], in_=pb[4][0:32, :]),
                         r=[pbB[4]], w=[combB[st]])
                    if "comb" in tapset:
                        if "tapcomb" not in ctx:
                            ctx["tapcomb"] = tap_out("comb", [L, S, 32]); ctx["tapcombB"] = Buf("tapcomb")
                        for t in range(4):
                            g0 = t0 + st * 512 + t * 128
                            PP.dma("sp", ctx["tapcomb"].ap()[l][g0:g0 + 128, :], CMB[:, t, :], r=[rtB, cB], w=[ctx["tapcombB"]])

                barrier(P)
                norm_part(P, 0)
                for st in range(NST):
                    router_mm(st)
                    d1 = Deferred()
                    router_chain(d1, st)
                    ds = [d1]
                    if st + 1 < NST:
                        d2 = Deferred()
                        norm_part(d2, st + 1)
                        ds.append(d2)
                    run_interleaved(P, ds)
                barrier(P)
                units = [(p, st) for p in range(N_EXP // 2) for st in range(NST)]
                dcnt = [0]

                def emit_expert(i, j, mid=None):
                    p, st = units[i]
                    e = 2 * p + j
                    cols = slice(st * 512, (st + 1) * 512)
                    W_, WB_ = wgu[p % 2][j], wguB[p % 2][j]
                    CB, CBB = cbs[i % 2], cbsB[i % 2][j]
                    P.op("pe", lambda ee: ee.matmul(pb[4][:], lhsT=sel[:, e, :], rhs=combT[:, cols], start=True, stop=True),
                         r=[selB, combB[st]], w=[pbB[4]])
                    P.op("act", lambda ee: ee.copy(out=CB[:, j, :], in_=pb[4][:]), r=[pbB[4]], w=[CBB])
                    for f in range(2):
                        if f == 1 and mid is not None:
                            mid()
                        for (bank, col0) in ((0 + f, f * 128), (2 + f, 256 + f * 128)):
                            for k in range(8):
                                P.op("pe", lambda ee, k=k, bank=bank, col0=col0: ee.matmul(
                                    pb[bank][:], lhsT=W_[:, k, col0:col0 + 128], rhs=h2T[:, k, cols], start=(k == 0),
                                    stop=(k == 7)), r=[WB_, h2B[st]], w=[pbB[bank]], inc=(k == 7))
                        P.op("act", lambda ee, f=f: ee.activation(out=sgt[:, f, :], in_=pb[f][:], func=AF.Silu),
                             r=[pbB[f]], w=[sgB[f]])
                        P.op("dve", lambda ee, f=f: ee.tensor_tensor(out=tt[:, f, :], in0=pb[2 + f][:], in1=sgt[:, f, :],
                                                                     op=ALU.mult), r=[pbB[2 + f], sgB[f]], w=[ttB[f]])
                        P.op("dve", lambda ee, f=f: ee.tensor_tensor(out=actT[i % 2][:, j, f, :], in0=tt[:, f, :],
                                                                     in1=CB[:, j, :], op=ALU.mult),
                             r=[ttB[f], CBB], w=[actB[i % 2][j][f]])

                def emit_down(i):
                    p, st = units[i]
                    cols = slice(st * 512, (st + 1) * 512)
                    for d in range(8):
                        bank = 5 + dcnt[0] % 3
                        dcnt[0] += 1
                        n = 0
                        for j in range(2):
                            for f in range(2):
                                P.op("pe", lambda ee, d=d, f=f, j=j, bank=bank, n=n: ee.matmul(
                                    pb[bank][:], lhsT=wd[p % 2][j][:, f, d * 128:(d + 1) * 128], rhs=actT[i % 2][:, j, f, :],
                                    start=(n == 0), stop=(n == 3)), r=[wdB[p % 2][j], actB[i % 2][j][f]], w=[pbB[bank]],
                                    inc=(n == 3))
                                n += 1
                        P.op("dve", lambda ee, d=d, bank=bank: ee.scalar_tensor_tensor(
                            out=acc[:, d, cols], in0=pb[bank][:], scalar=g2(d), in1=acc[:, d, cols], op0=ALU.mult, op1=ALU.add),
                            r=[pbB[bank], accB[st], modB], w=[accB[st]])

                load_pair(0)
                pre = (hf == NHALF - 1 and l + 1 < L)
                if pre:
                    wavn = Wd["w_ada"].ap()[l + 1].rearrange("(k p) n -> p k n", p=128)
                    NPIECE = 48
                    every = max(1, (len(units) - 4) // NPIECE)

                    def mod_dma(j):
                        P.dma("pool", wa2[j % 2][:], wavn[:, :, j * 128:(j + 1) * 128], w=[wa2B[j % 2]])

                    def mod_piece(j):
                        W_, WB_ = wa2[j % 2], wa2B[j % 2]
                        for k in range(8):
                            P.op("pe", lambda e, k=k: e.matmul(
                                pb[4][:, 0:1], lhsT=W_[:, k, :], rhs=condTb[:, k:k + 1],
                                start=(k == 0), stop=(k == 7)), r=[WB_, condB], w=[pbB[4]], inc=(k == 7))
                        P.op("act", lambda e: e.copy(out=modN[:, j:j + 1], in_=pb[4][:, 0:1]), r=[pbB[4]], w=[modNB])
                    mod_dma(0)
                    mod_dma(1)
                for i in range(len(units)):
                    p, st = units[i]
                    if pre and i % every == 0 and i // every < NPIECE:
                        j = i // every
                        mod_piece(j)
                        if j + 2 < NPIECE:
                            mod_dma(j + 2)
                    emit_expert(i, 0)

                    def mid(i=i, p=p, st=st):
                        if i >= 1:
                            emit_down(i - 1)
                        if st == 0 and p + 1 < N_EXP // 2:
                            load_pair(p + 1)
                    emit_expert(i, 1, mid=mid)
                emit_down(len(units) - 1)
                if pre:
                    for j in range(min(NPIECE, (len(units) + every - 1) // every), NPIECE):
                        mod_piece(j)
                        if j + 2 < NPIECE:
                            mod_dma(j + 2)
                for st in range(NST):
                    gs = (t0 // 512) + st
                    P.dma("sp", xT_v[:, :, t0 + st * 512:t0 + (st + 1) * 512], acc[:, :, st * 512:(st + 1) * 512],
                          r=[accB[st]], w=[xT_b[gs]])

    ctx["moe_phase"] = moe_phase

    if "YT" in tapset:
        ctx["tapYT"] = tap_out("YT", [L, D, S])
        ctx["tapYTB"] = Buf("tapYT")
        ctx["tapst"] = sb("tapst", [128, 512], F32)
        ctx["tapstB"] = Buf("tapst")
    if not ctx["do_moe"]:
        xt0 = sb("cpx", [128, 8, 32], F32); xt0B = Buf("cp")

    for l in range(L):
        P.dma("sp", stA[0:48, :], Wd["b_ada"].ap()[l].rearrange("(r p) -> r p", p=128), w=[stAB])
        P.dma("sp", stA[48:56, :], Wd["norm1_g"].ap()[l].rearrange("(r p) -> r p", p=128), w=[stAB])
        P.dma("sp", stA[56:64, :], Wd["norm2_g"].ap()[l].rearrange("(r p) -> r p", p=128), w=[stAB])
        P.dma("sp", stA[64:72, :], Wd["ssm_a_re"].ap()[l].rearrange("(cb j) p -> cb (j p)", j=2), w=[stAB])
        P.dma("sp", stA[72:80, :], Wd["ssm_a_im"].ap()[l].rearrange("(cb j) p -> cb (j p)", j=2), w=[stAB])
        transpose_to(vecT[:], vecB, stA[:], stAB, 80)
        P.dma("sp", st_ld[:], Wd["ssm_log_dt"].ap()[l].rearrange("(cb j) -> cb j", j=2), w=[st2B])
        P.op("dve", lambda e: e.tensor_copy(st2[:].rearrange("p (j q) -> p j q", j=2),
                                            st_ld[:, :, None].to_broadcast([8, 2, 64])), r=[st2B], w=[st2B])
        transpose_to(ldtT[:], ldtB, st2[:], st2B, 8)
        P.dma("sp", st3[0:2, :], Wd["ssm_d"].ap()[l].rearrange("(r p) -> r p", p=128), w=[st3B])
        P.dma("sp", st3[2:4, :], Wd["ssm_b_glu"].ap()[l].rearrange("(r p) -> r p", p=128), w=[st3B])
        transpose_to(sdT[:], sdB, st3[:], st3B, 4)
        es1 = ExitStack()
        if l == 0 or not ctx["do_moe"]:
            wav = Wd["w_ada"].ap()[l].rearrange("(k p) n -> p k n", p=128)
            wa = [es1.enter_context(nc.sbuf_tensor("wa%d_%d" % (l, i), [128, 8, 512], BF16)) for i in range(2)]
            waB = [Buf("wa%d" % i) for i in range(2)]
            for j in range(12):
                W_, WB_ = wa[j % 2], waB[j % 2]
                P.dma("pool", W_[:], wav[:, :, j * 512:(j + 1) * 512], w=[WB_])
                for m in range(4):
                    col = 4 * j + m
                    for k in range(8):
                        P.op("pe", lambda e, W_=W_, m=m, k=k, col=col: e.matmul(
                            pb[6][:, col:col + 1], lhsT=W_[:, k, m * 128:(m + 1) * 128], rhs=condTb[:, k:k + 1],
                            start=(k == 0), stop=(k == 7)), r=[WB_, condB], w=[pbB[6]], inc=(k == 7))

            P.op("act", lambda e: e.copy(out=modN[:], in_=pb[6][:, 0:48]), r=[pbB[6]], w=[modNB])
        P.op("dve", lambda e: e.tensor_tensor(out=modT[:], in0=modN[:], in1=vecT[:, C_BADA:C_BADA + 48],
                                              op=ALU.add), r=[modNB, vecB], w=[modB])
        P.op("dve", lambda e: e.scalar_tensor_tensor(out=gm[:, 0:8], in0=modT[:, 8:16], scalar=1.0,
                                                     in1=vecT[:, C_N1:C_N1 + 8], op0=ALU.add, op1=ALU.mult),
             r=[modB, vecB], w=[gmB])
        P.op("dve", lambda e: e.scalar_tensor_tensor(out=gm[:, 8:16], in0=modT[:, 32:40], scalar=1.0,
                                                     in1=vecT[:, C_N2:C_N2 + 8], op0=ALU.add, op1=ALU.mult),
             r=[modB, vecB], w=[gmB])
        sh1 = lambda k: modT[:, k:k + 1]
        g1 = lambda k: modT[:, 16 + k:17 + k]
        sh2 = lambda k: modT[:, 24 + k:25 + k]
        g2 = lambda k: modT[:, 40 + k:41 + k]
        gm1 = lambda k: gm[:, k:k + 1]
        gm2 = lambda k: gm[:, 8 + k:9 + k]

        barrier(P)
        es1.close()
        with ExitStack() as es:
            def sba(name, shape, dt):
                return es.enter_context(nc.sbuf_tensor("A%d_%s" % (l, name), list(shape), dt))
            w_in_sb = sba("w_in", [128, 8, IN_W], BF16); winB = Buf("w_in")
            w_out_sb = sba("w_out", [128, 8, D], BF16); woutB = Buf("w_out")
            wiv = Wd["w_in"].ap()[l].rearrange("(k p) n -> p k n", p=128)
            wov = Wd["w_out"].ap()[l].rearrange("(k p) n -> p k n", p=128)
            for k in range(8):
                for hh in range(2):
                    P.dma("pool", w_in_sb[:, k, hh * 1408:(hh + 1) * 1408], wiv[:, k, hh * 1408:(hh + 1) * 1408],
                          w=[winB])
            for k in range(8):
                P.dma("pool", w_out_sb[:, k, :], wov[:, k, :], w=[woutB])
            xt = [sba("xt%d" % i, [128, 8, 512], F32) for i in range(1)]
            xtB = [Buf("xt%d" % i) for i in range(1)]
            sq = [sba("sq%d" % i, [128, 512], BF16) for i in range(2)]; sqB = [Buf("sq%d" % i) for i in range(2)]
            rs = sba("rs", [128, 512], F32); rsB = Buf("rs")
            mt = [sba("mt%d" % i, [128, 512], F32) for i in range(4)]
            mtB = [[Buf("mt%dA" % i), Buf("mt%dB" % i)] for i in range(4)]
            tmp = mt[0:2]; tmpB = mtB[0:2]
            hT = sba("hT", [128, 8, 512], BF16); hTB = Buf("hT")
            qT = sba("qT", [128, 2, 4, 512], BF16); qTB = Buf("qT")
            kT = sba("kT", [128, 4, 1024], BF16); kTB = [Buf("kT%d" % i) for i in range(8)]
            Vr = sba("Vr", [128, 8, 8, 65], BF16); VrB = [Buf("Vr%d" % i) for i in range(8)]
            uT32 = sba("uT32", [128, 2, 512], F32); uTb = sba("uTb", [128, 2, 512], BF16); uTB = Buf("uT")
            YT = sba("YT", [128, 8, 512], BF16)
            YTB = [Buf("YT%d" % i) for i in range(8)]
            expB = sba("expB", [128, 8, 5, 128], BF16); expBB = Buf("expB")
            Pt = [sba("Pt%d" % i, [128, 5, 128], BF16) for i in range(3)]
            PtB = [Buf("Pt%d" % i) for i in range(3)]
            trB = Buf("tr4")
            pb4b = pb[4][:].bitcast(BF16)
            ya = sba("ya", [128, 8, 64], BF16); yaB = Buf("ya")
            rec = sba("rec", [128, 8], F32); recB = Buf("rec")
            tailB = [Buf("tail%d" % i) for i in range(4)]
            cs = [sba("cs%d" % i, [128, 512], F32) for i in range(1)] * 2; csB = [Buf("cs0")] * 2
            rmt = sba("rmt", [128, 2, 256], F32)
            rm = [rmt[:, i % 2, :].rearrange("p (h f) -> p h f", h=8) for i in range(4)]
            rmB = [Buf("rm0"), Buf("rm1")] * 2
            qkrot = sba("qkrot", [128, 8, 64], BF16); qkrotB = Buf("qkrot")
            qTr = sba("qTr", [128, 2, 2, 128], BF16); kTr = sba("kTr", [128, 2, 128], BF16); qkTB = Buf("qkT")
            vr = sba("vr", [128, 256], BF16); vrB = Buf("vr")
            sg = sba("sg", [128, 256], F32); sgB = Buf("sg")
            Am = sba("Am", [128, 4, 128], BF16); AmB = Buf("Am")
            Sst = sba("Sst", [128, 2, 64], F32); Sbf = sba("Sbf", [128, 2, 64], BF16); SstB = Buf("Sst"); SbfB = Buf("Sbf")
            ysb = sba("ysb", [128, 256], F32); ysbB = Buf("ysb")
            ysq = sba("ysq", [128, 256], F32); ysqB = Buf("ysq")
            gst = sba("gst", [128, 16], F32); gstB = Buf("gst")
            gn_bc = sba("gn_bc", [128, 256], F32); gnB = Buf("gn_bc")
            yr = sba("yr", [128, 256], BF16); yrB = Buf("yr")

            P.op("dve", lambda e: e.memset(Vr[:].rearrange("p a b c -> p (a b c)"), 1.0), w=VrB)
            P.op("dve", lambda e: e.memset(Sst[:].rearrange("p a b -> p (a b)"), 0.0), w=[SstB])
            P.op("dve", lambda e: e.memset(Sbf[:].rearrange("p a b -> p (a b)"), 0.0), w=[SbfB])
            P.op("dve", lambda e: e.memset(YT[:].rearrange("p a b -> p (a b)"), 0.0), w=YTB)
            P.op("dve", lambda e: e.memset(qT[:].rearrange("p a b c -> p (a b c)"), 0.0), w=[qTB])
            P.op("dve", lambda e: e.memset(qTr[:].rearrange("p a b c -> p (a b c)"), 0.0), w=[qkTB])
            P.dma("sp", gn_bc[:], Wd["ret_gn_g"].ap()[l:l + 1, :].to_broadcast([128, 256]), w=[gnB])
            es2 = ExitStack()
            Hk = es2.enter_context(nc.sbuf_tensor("Hk%d" % l, [128, 5, 128], F32)); HkB = Buf("Hk")
            Fsb = es2.enter_context(nc.sbuf_tensor("Fsb%d" % l, [8, 768], F32)); FsbB = Buf("Fsb")
            P.dma("sp", Fsb[:, 511:703], Wd["attn_rel_bias"].ap()[l], w=[FsbB])
            P.op("dve", lambda e: e.tensor_copy(Fsb[:, 0:511], Fsb[:, 511:512].to_broadcast([8, 511])),
                 r=[FsbB], w=[FsbB])
            P.op("dve", lambda e: e.tensor_copy(Fsb[:, 703:768], Fsb[:, 702:703].to_broadcast([8, 65])),
                 r=[FsbB], w=[FsbB])
            P.dma("sp", Fd.ap()[l], Fsb[:], r=[FsbB], w=[FdB])
            for h in range(8):
                src = bass.AP(Fd, (l * 8 + h) * 768, [[1, 128], [128, 5], [1, 128]])
                P.dma("sp", Hk[:], src, r=[FdB], w=[HkB])
                P.op("act", lambda e, h=h: e.activation(out=expB[:, h, :, :], in_=_rev_last(Hk[:], 128), func=AF.Exp),
                     r=[HkB], w=[expBB])
            P.op("dve", lambda e: e.tensor_tensor(
                out=expB[:], in0=expB[:],
                in1=amask_sb[:].rearrange("p (b q) -> p b q", b=5)[:, None, :, :].to_broadcast([128, 8, 5, 128]),
                op=ALU.mult), r=[expBB, B_const], w=[expBB])

            barrier(P)
            es2.close()
            if do_s5:
                ctx["s5_setup"](l, es, dict(vecT=vecT, vecB=vecB, ldtT=ldtT, ldtB=ldtB, sdT=sdT, sdB=sdB, uT32=uT32,
                                            uTb=uTb, uTB=uTB, YT=YT, YTB=YTB, mt=mt, mtB=mtB))
            unit = [0]
            sbank = [0]

            def evac(i, out_ap, in_ap, r, w):
                if i % 2 == 0:
                    P.op("act", lambda e: e.copy(out=out_ap, in_=in_ap), r=r, w=w)
                else:
                    P.op("dve", lambda e: e.tensor_copy(out_ap, in_ap), r=r, w=w)

            mmc = [0]

            def mmbank():
                mmc[0] += 1
                return mmc[0] % 2

            for s in range(NT):
                X, XB = xt[0], xtB[0]
                P.dma("sp", X[:], xT_v[:, :, s * 512:(s + 1) * 512], r=[xT_b[s]], w=[XB])
                rms_stats(P, pb, pbB, mmbank(), ones_bf, B_const, X, XB, sq, sqB, rs, rsB)
                for k in range(8):
                    T_, TB_ = tmp[k % 2], tmpB[k % 2]
                    P.op("dve", lambda e, k=k, T_=T_: e.scalar_tensor_tensor(
                        out=T_[:], in0=X[:, k, :], scalar=gm1(k), in1=rs[:], op0=ALU.mult, op1=ALU.mult),
                        r=[XB, rsB, gmB], w=[TB_])
                    P.op("act", lambda e, k=k, T_=T_: e.activation(out=hT[:, k, :], in_=T_[:], func=AF.Identity,
                                                                    bias=sh1(k), scale=1.0),
                         r=[TB_, modB], w=[hTB])
                ring0 = (4 * s) % 8
                fm = [("q", c, c * 128) for c in range(4)] + [("k", c, 512 + c * 128) for c in range(4)] + \
                     [("u", c, 1536 + c * 128) for c in range(2)]
                for i, (kind, c, col) in enumerate(fm):
                    bk = mmbank()
                    for kk in range(8):
                        P.op("pe", lambda e, kk=kk, col=col, bk=bk: e.matmul(
                            pb[bk][:], lhsT=w_in_sb[:, kk, col:col + 128], rhs=hT[:, kk, :], start=(kk == 0),
                            stop=(kk == 7)), r=[winB, hTB], w=[pbB[bk]], inc=(kk == 7))
                    if kind == "q":
                        evac(0, qT[0:64, 0, c, :], pb[bk][0:64, :], [pbB[bk]], [qTB])
                        evac(1, qT[64:128, 1, c, :], pb[bk][64:128, :], [pbB[bk]], [qTB])
                    elif kind == "k":
                        evac(i, kT[:, c, ring0 * 128:ring0 * 128 + 512], pb[bk][:], [pbB[bk]], kTB[ring0:ring0 + 4])
                    else:
                        P.op("act", lambda e, c=c, bk=bk: e.copy(out=uT32[:, c, :], in_=pb[bk][:]), r=[pbB[bk]], w=[uTB])
                        P.op("dve", lambda e, c=c, bk=bk: e.tensor_copy(uTb[:, c, :], pb[bk][:]), r=[pbB[bk]], w=[uTB])
                for t in range(4):
                    gb = 4 * s + t
                    slot = gb % 8
                    tok = slice(t * 128, (t + 1) * 128)
                    bk = mmbank()
                    for kk in range(8):
                        P.op("pe", lambda e, kk=kk, bk=bk, tok=tok: e.matmul(
                            pb[bk][:], lhsT=hT[:, kk, tok], rhs=w_in_sb[:, kk, 1024:1536], start=(kk == 0),
                            stop=(kk == 7)), r=[winB, hTB], w=[pbB[bk]], inc=(kk == 7))
                    P.op("act", lambda e, bk=bk, slot=slot: e.copy(
                        out=Vr[:, slot, :, 0:64], in_=pb[bk][:].rearrange("p (h d) -> p h d", h=8)),
                        r=[pbB[bk]], w=[VrB[slot]])

                def attn_gen(P):
                    for t in range(4):
                        gb = 4 * s + t
                        tok = slice(t * 128, (t + 1) * 128)
                        nbk = min(5, gb + 1)
                        b0 = 5 - nbk
                        info = {}

                        def stage_a(h, gb=gb, tok=tok, nbk=nbk, b0=b0, info=info):
                            c = h // 2
                            u = unit[0]; unit[0] += 1
                            PT, PTB = Pt[u % 3], PtB[u % 3]
                            info[h] = (PT, PTB)
                            pieces = ([(b0, 4)] if nbk > 1 else []) + [(4, 5)]
                            for (ba_, bb_) in pieces:
                                bank = 2 + sbank[0] % 2
                                sbank[0] += 1
                                for b in range(ba_, bb_):
                                    kslot = (gb - 4 + b) % 8
                                    P.op("pe", lambda e, b=b, kslot=kslot, c=c, h=h, bank=bank, ba_=ba_: e.matmul(
                                        pb[bank][:, (b - ba_) * 128:(b - ba_ + 1) * 128],
                                        lhsT=kT[:, c, kslot * 128:(kslot + 1) * 128],
                                        rhs=qT[:, h % 2, c, tok], start=True, stop=True),
                                        r=[kTB[kslot], qTB], w=[pbB[bank]], inc=(b == bb_ - 1))
                                P.op("act", lambda e, bank=bank, PT=PT, ba_=ba_, bb_=bb_: e.activation(
                                    out=PT[:, ba_:bb_, :].rearrange("p a b -> p (a b)"),
                                    in_=pb[bank][:, 0:(bb_ - ba_) * 128], func=AF.Exp, scale=0.125),
                                    r=[pbB[bank]], w=[PTB])
                            P.op("dve", lambda e, PT=PT, h=h: e.tensor_tensor(
                                out=PT[:, b0:5, :], in0=PT[:, b0:5, :], in1=expB[:, h, b0:5, :], op=ALU.mult),
                                r=[PTB, expBB], w=[PTB])

                        def stage_b(h, gb=gb, b0=b0, info=info):
                            PT, PTB = info[h]
                            for b in range(b0, 5):
                                kslot = (gb - 4 + b) % 8
                                P.op("pe", lambda e, b=b, kslot=kslot, h=h, PT=PT: e.matmul(
                                    pb[5][:, (h % 4) * 65:(h % 4) * 65 + 65], lhsT=PT[:, b, :],
                                    rhs=Vr[:, kslot, h, :], start=(b == b0), stop=(b == 4)),
                                    r=[PTB, VrB[kslot]], w=[pbB[5]], inc=(b == 4))
                            if h % 4 == 3:
                                hh = h // 4
                                pvv = pb[5][:, 0:260].rearrange("p (h d) -> p h d", h=4)
                                P.op("dve", lambda e, pvv=pvv, hh=hh: e.reciprocal(
                                    rec[:, hh * 4:hh * 4 + 4], pvv[:, :, 64]), r=[pbB[5]], w=[recB])
                                P.op("dve", lambda e, pvv=pvv, hh=hh: e.tensor_tensor(
                                    out=ya[:, hh * 4:hh * 4 + 4, :], in0=pvv[:, :, 0:64],
                                    in1=rec[:, hh * 4:hh * 4 + 4, None].to_broadcast([128, 4, 64]), op=ALU.mult),
                                    r=[pbB[5], recB], w=[yaB])

                        stage_a(0)
                        yield
                        for h in range(1, 8):
                            stage_a(h)
                            stage_b(h - 1)
                            yield
                        stage_b(7)
                        P.begin()
                        for c in range(4):
                            P.op("pe", lambda e, c=c: e.transpose(
                                pb7b[:, c * 128:(c + 1) * 128],
                                ya[:, 2 * c:2 * c + 2, :].rearrange("p a b -> p (a b)"), identb[:]),
                                r=[yaB, B_const], w=[pbB[7]], inc=(c == 3))
                        P.op("act", lambda e, tok=tok: e.copy(out=YT[:, 0:4, tok],
                                                              in_=pb7b[:, 0:512].rearrange("p (c q) -> p c q", c=4)),
                             r=[pbB[7]], w=YTB[0:4])
                        P.end()
                        yield

                def ret_gen(P):
                    for t in range(4):
                        gb = 4 * s + t
                        slot = gb % 8
                        tok = slice(t * 128, (t + 1) * 128)
                        if do_ret:
                            bq = 4
                            for kk in range(8):
                                P.op("pe", lambda e, kk=kk, bq=bq: e.matmul(
                                    pb[bq][:], lhsT=hT[:, kk, tok], rhs=w_in_sb[:, kk, 1792:2304], start=(kk == 0),
                                    stop=(kk == 7)), r=[winB, hTB], w=[pbB[bq]], inc=(kk == 7))
                            CS, CSB = cs[gb % 2], csB[gb % 2]
                            P.dma("sp", CS[:], Cd["retcs"].ap()[gb * 128:(gb + 1) * 128, :], w=[CSB])
                            srcv = pb[bq][:].rearrange("p (h two f) -> p h two f", h=8, two=2)
                            x1 = srcv[:, :, 0, :]
                            x2 = srcv[:, :, 1, :]
                            cosv = CS[:, 0:256].rearrange("p (h f) -> p h f", h=8)
                            sinv = CS[:, 256:512].rearrange("p (h f) -> p h f", h=8)
                            for (lo_, a1, b1, a2, b2, op_) in ((0, x1, cosv, x2, sinv, ALU.subtract), (32, x1, sinv, x2, cosv, ALU.add)):
                                P.op("dve", lambda e: e.tensor_tensor(out=rm[0], in0=a1, in1=b1, op=ALU.mult),
                                     r=[pbB[bq], CSB], w=[rmB[0]])
                                P.op("dve", lambda e: e.tensor_tensor(out=rm[1], in0=a2, in1=b2, op=ALU.mult),
                                     r=[pbB[bq], CSB], w=[rmB[1]])
                                P.op("dve", lambda e: e.tensor_tensor(out=qkrot[:, :, lo_:lo_ + 32], in0=rm[0], in1=rm[1], op=op_),
                                     r=[rmB[0], rmB[1]], w=[qkrotB])
                            yield
                            if RS >= 2:
                                bv = 4
                                for kk in range(8):
                                    P.op("pe", lambda e, kk=kk, bv=bv: e.matmul(
                                        pb[bv][:], lhsT=hT[:, kk, tok], rhs=w_in_sb[:, kk, 2304:2816], start=(kk == 0),
                                        stop=(kk == 7)), r=[winB, hTB], w=[pbB[bv]], inc=(kk == 7))
                                P.op("act", lambda e, bv=bv: e.copy(out=vr[:], in_=pb[bv][:, 0:256]), r=[pbB[bv]], w=[vrB])
                                P.op("act", lambda e, bv=bv: e.activation(out=sg[:], in_=pb[bv][:, 256:512], func=AF.Silu),
                                 r=[pbB[bv]], w=[sgB])
                        if do_ret:
                            yield
                            if RS >= 3:
                                P.begin()
                                for c in range(4):
                                    P.op("pe", lambda e, c=c: e.transpose(
                                        pb7b[:, 512 + c * 128:512 + (c + 1) * 128],
                                        qkrot[:, 2 * c:2 * c + 2, :].rearrange("p a b -> p (a b)"), identb[:]),
                                        r=[qkrotB, B_const], w=[pbB[7]], inc=(c == 3))
                                P.op("act", lambda e: e.copy(out=qTr[0:64, 0, :, :].rearrange("p a b -> p (a b)"),
                                                             in_=pb7b[0:64, 512:768]), r=[pbB[7]], w=[qkTB])
                                P.op("act", lambda e: e.copy(out=qTr[64:128, 1, :, :].rearrange("p a b -> p (a b)"),
                                                             in_=pb7b[64:128, 512:768]), r=[pbB[7]], w=[qkTB])
                                P.op("dve", lambda e: e.tensor_copy(kTr[:].rearrange("p a b -> p (a b)"), pb7b[:, 768:1024]),
                                     r=[pbB[7]], w=[qkTB])
                                P.end()
                            yield
                            if RS >= 4:
                                ba = 4
                                for h in range(4):
                                    c, pbase = h // 2, 64 * (h % 2)
                                    P.op("pe", lambda e, h=h, c=c, pbase=pbase, ba=ba: e.matmul(
                                        pb[ba][:, h * 128:(h + 1) * 128], lhsT=kTr[:, c, :],
                                        rhs=qTr[:, h % 2, c, :], start=True, stop=True),
                                        r=[qkTB], w=[pbB[ba]], inc=(h == 3))
                                if RS >= 4.5: P.op("dve", lambda e, ba=ba: e.tensor_tensor(
                                    out=Am[:], in0=pb[ba][:].rearrange("p (h i) -> p h i", h=4),
                                    in1=tri_sb[:, None, :].to_broadcast([128, 4, 128]), op=ALU.mult),
                                    r=[pbB[ba], B_const], w=[AmB])
                            yield
                            if RS >= 5:
                                by = 4
                                for h in range(4):
                                    c, pbase = h // 2, 64 * (h % 2)
                                    P.op("pe", lambda e, h=h, by=by: e.matmul(
                                        pb[by][:, h * 64:(h + 1) * 64], lhsT=Am[:, h, :], rhs=vr[:, h * 64:(h + 1) * 64],
                                        start=True, stop=False), r=[AmB, vrB], w=[pbB[by]], inc=False)
                                    P.op("pe", lambda e, h=h, c=c, pbase=pbase, by=by: e.matmul(
                                        pb[by][:, h * 64:(h + 1) * 64], lhsT=qTr[:, h % 2, c, :],
                                        rhs=Sbf[:, c, :], start=False, stop=True),
                                        r=[qkTB, SbfB], w=[pbB[by]], inc=(h == 3))
                                for c in range(2):
                                    P.op("pe", lambda e, c=c, by=by: e.matmul(
                                        pb[by][:, 256 + c * 128:256 + (c + 1) * 128],
                                        lhsT=qkrot[:, 4 + 2 * c:6 + 2 * c, :].rearrange("p a b -> p (a b)"),
                                        rhs=vr[:, c * 128:(c + 1) * 128], start=True, stop=True),
                                        r=[qkrotB, vrB], w=[pbB[by]], inc=(c == 1))
                            if RS >= 5.2:
                                P.op("act", lambda e, by=by: e.copy(out=ysb[:], in_=pb[by][:, 0:256]), r=[pbB[by]], w=[ysbB])
                                for hh in range(2):
                                    ps_ = slice(64 * hh, 64 * hh + 64)
                                    if hh == 0:
                                        P.op("act", lambda e, by=by: e.copy(out=ysq[:], in_=pb[by][:, 256:512]),
                                             r=[pbB[by]], w=[ysqB])
                                    kvv = ysq[ps_, :].rearrange("p (c x) -> p c x", c=2)[:, :, 64 * hh:64 * hh + 64]
                                    if RS >= 5.4 + 0.2 * hh: P.op("dve", lambda e, ps_=ps_, kvv=kvv: e.tensor_tensor(
                                        out=Sst[ps_, :, :], in0=kvv, in1=Sst[ps_, :, :], op=ALU.add),
                                        r=[ysqB, SstB], w=[SstB])
                                    if RS >= 5.5 + 0.2 * hh: P.op("dve", lambda e, ps_=ps_: e.tensor_tensor(
                                        out=Sst[ps_, :, :], in0=Sst[ps_, :, :],
                                        in1=retG_sb[ps_, :].rearrange("p (c x) -> p c x", c=2), op=ALU.mult),
                                        r=[SstB, B_const], w=[SstB])
                                P.op("act", lambda e: e.copy(out=Sbf[:].rearrange("p a b -> p (a b)"),
                                                             in_=Sst[:].rearrange("p a b -> p (a b)")), r=[SstB], w=[SbfB])
                            yield
                            if RS >= 7:
                                y3 = ysb[:].rearrange("p (h x) -> p h x", h=4)
                                P.op("dve", lambda e: e.tensor_reduce(out=gst[:, 0:4], in_=y3, axis=AX.X, op=ALU.add),
                                     r=[ysbB], w=[gstB])
                                P.op("act", lambda e: e.activation(out=ysq[:], in_=ysb[:], func=AF.Square), r=[ysbB], w=[ysqB])
                                P.op("dve", lambda e: e.tensor_reduce(out=gst[:, 4:8], in_=ysq[:].rearrange("p (h x) -> p h x", h=4),
                                                                      axis=AX.X, op=ALU.add), r=[ysqB], w=[gstB])
                                P.op("dve", lambda e: e.tensor_scalar(out=gst[:, 0:4], in0=gst[:, 0:4], scalar1=1.0 / 64, scalar2=None,
                                                                      op0=ALU.mult), r=[gstB], w=[gstB])
                                P.op("dve", lambda e: e.tensor_tensor(out=gst[:, 8:12], in0=gst[:, 0:4], in1=gst[:, 0:4],
                                                                      op=ALU.mult), r=[gstB], w=[gstB])
                                P.op("dve", lambda e: e.scalar_tensor_tensor(out=gst[:, 12:16], in0=gst[:, 4:8], scalar=1.0 / 64,
                                                                             in1=gst[:, 8:12], op0=ALU.mult, op1=ALU.subtract),
                                     r=[gstB], w=[gstB])
                                P.op("act", lambda e: e.activation(out=gst[:, 12:16], in_=gst[:, 12:16], func=AF.Sqrt, bias=EPS,
                                                                   scale=1.0), r=[gstB], w=[gstB])
                                P.op("dve", lambda e: e.reciprocal(gst[:, 12:16], gst[:, 12:16]), r=[gstB], w=[gstB])
                                P.op("dve", lambda e: e.tensor_tensor(out=y3, in0=y3,
                                                                      in1=gst[:, 0:4, None].to_broadcast([128, 4, 64]),
                                                                      op=ALU.subtract), r=[ysbB, gstB], w=[ysbB])
                                P.op("dve", lambda e: e.tensor_tensor(out=y3, in0=y3,
                                                                      in1=gst[:, 12:16, None].to_broadcast([128, 4, 64]),
                                                                      op=ALU.mult), r=[ysbB, gstB], w=[ysbB])
                                P.op("dve", lambda e: e.tensor_tensor(out=ysb[:], in0=ysb[:], in1=gn_bc[:], op=ALU.mult),
                                     r=[ysbB, gnB], w=[ysbB])
                                P.op("dve", lambda e: e.tensor_tensor(out=yr[:], in0=ysb[:], in1=sg[:], op=ALU.mult),
                                     r=[ysbB, sgB], w=[yrB])
                            yield
                            if RS >= 8:
                                P.begin()
                                for c in range(2):
                                    P.op("pe", lambda e, c=c: e.transpose(
                                        pb7b[:, c * 128:(c + 1) * 128], yr[:, c * 128:(c + 1) * 128], identb[:]),
                                        r=[yrB, B_const], w=[pbB[7]], inc=(c == 1))
                                P.op("act", lambda e: e.copy(out=YT[:, 6:8, tok],
                                                             in_=pb7b[:, 0:256].rearrange("p (c q) -> p c q", c=2)),
                                     r=[pbB[7]], w=YTB[6:8])
                                P.end()

                        yield
                recs = []
                for mk in ((attn_gen if do_attn else None), ((lambda PP: ctx["s5_gen"](s, PP)) if do_s5 else None),
                           (ret_gen if do_ret else None)):
                    if mk is not None:
                        d = Deferred()
                        for _ in mk(d):
                            pass
                        recs.append(d)
                run_interleaved(P, recs)
                if "YT" in tapset:
                    for kk in range(8):
                        P.op("dve", lambda e, kk=kk: e.tensor_copy(ctx["tapst"][:], YT[:, kk, :]), r=[YTB[kk]],
                             w=[ctx["tapstB"]])
                        P.dma("sp", ctx["tapYT"].ap()[l][kk * 128:(kk + 1) * 128, s * 512:(s + 1) * 512],
                              ctx["tapst"][:], r=[ctx["tapstB"]], w=[ctx["tapYTB"]])
                for d in range(8):
                    bk = mmbank()
                    for kk in range(8):
                        P.op("pe", lambda e, kk=kk, d=d, bk=bk: e.matmul(
                            pb[bk][:], lhsT=w_out_sb[:, kk, d * 128:(d + 1) * 128], rhs=YT[:, kk, :], start=(kk == 0),
                            stop=(kk == 7)), r=[woutB, YTB[kk]], w=[pbB[bk]], inc=(kk == 7))
                    P.op("dve", lambda e, d=d, bk=bk: e.scalar_tensor_tensor(
                        out=X[:, d, :], in0=pb[bk][:], scalar=g1(d), in1=X[:, d, :], op0=ALU.mult, op1=ALU.add),
                        r=[pbB[bk], XB, modB], w=[XB])
                P.dma("sp", x1T_v[:, :, s * 512:(s + 1) * 512], X[:], r=[XB], w=[x1T_b[s]])
        barrier(P)
        if ctx["do_moe"]:
            ctx["moe_phase"](l, dict(sh2=sh2, g2=g2, gm2=gm2, modB=modB, gmB=gmB))
        else:
            for s in range(16 * NT):
                P.dma("sp", xt0[:], x1T_v[:, :, s * 32:(s + 1) * 32], r=[x1T_b[s // 16]], w=[xt0B])
                P.dma("sp", xT_v[:, :, s * 32:(s + 1) * 32], xt0[:], r=[xt0B], w=[xT_b[s // 16]])
        barrier(P)
    return xT_v, xT_b


_CACHE = {}


def _get_program(S, L):
    key = (S, L)
    if key not in _CACHE:
        nc = bass.Bass("TRN2", target_bir_lowering=False)
        P, _ = build_program(nc, S, L)
        _CACHE[key] = nc
    return _CACHE[key]


def kernel(**inputs):
    x = np.asarray(inputs["x"], dtype=np.float32)
    B, S, _ = x.shape
    L = int(np.asarray(inputs["w_in"]).shape[0])
    nc = _get_program(S, L)
    cst = host_constants(S)
    shared = {k: np.ascontiguousarray(np.asarray(inputs[k], dtype=np.float32)) for k in WEIGHT_SHAPES(L)}
    shared.update(cst)
    c = np.asarray(inputs["c"], dtype=np.float32)
    in_maps = []
    for b in range(B):
        m = dict(shared)
        m["x"] = np.ascontiguousarray(x[b])
        m["c"] = np.ascontiguousarray(c[b].reshape(8, 128))
        in_maps.append(m)
    res = run_bass_kernel_spmd(nc, in_maps, core_ids=list(range(B)))
    return np.stack([np.asarray(r["y"], dtype=np.float32) for r in res.results], axis=0)
```

```python
import math
import numpy as np
import concourse.bass as bass
import concourse.mybir as mybir
from concourse.bass_utils import run_bass_kernel_spmd

F32 = mybir.dt.float32
BF16 = mybir.dt.bfloat16
I32 = mybir.dt.int32
AF = mybir.ActivationFunctionType
ALU = mybir.AluOpType
AX = mybir.AxisListType

D = 1024
NCH = 8
IN_W = 2816
EPS = 1e-6
N_EXP = 32
FF = 256


class Buf:
    __slots__ = ("w", "rs", "name", "excl")

    def __init__(self, name="", excl=False):
        self.w = None
        self.rs = {}
        self.name = name
        self.excl = excl


class Prog:
    NRING = 8

    def __init__(self, nc):
        self.nc = nc
        self.E = {"pe": nc.tensor, "act": nc.scalar, "dve": nc.vector, "pool": nc.gpsimd, "sp": nc.sync}
        self.sem = {}
        self.cnt = {}
        for e in ("pe", "act", "dve", "pool"):
            self.sem[e] = nc.alloc_semaphore("s_" + e)
            self.cnt[e] = 0
        for q in ("sp", "act", "pool"):
            for i in range(self.NRING):
                k = "d_%s%d" % (q, i)
                self.sem[k] = nc.alloc_semaphore(k)
                self.cnt[k] = 0
        self.dma_i = {"sp": 0, "act": 0, "pool": 0}
        self.seen = {e: {} for e in ("pe", "act", "dve", "pool", "sp")}
        self.n_ins = 0

    def _deps(self, r, w, eng=None):
        deps = {}
        for b in r:
            if b.w is not None and deps.get(b.w[0], 0) < b.w[1]:
                deps[b.w[0]] = b.w[1]
            if b.excl:
                for e, c in b.rs.items():
                    if e != eng and deps.get(e, 0) < c:
                        deps[e] = c
        for b in w:
            if b.w is not None and deps.get(b.w[0], 0) < b.w[1]:
                deps[b.w[0]] = b.w[1]
            for e, c in b.rs.items():
                if deps.get(e, 0) < c:
                    deps[e] = c
        return deps

    def _wait(self, eng, deps):
        for e2, c in deps.items():
            if c <= 0:
                continue
            if e2 == eng and eng == "pe":
                continue
            if self.seen[eng].get(e2, 0) >= c:
                continue
            self.E[eng].wait_ge(self.sem[e2], c)
            self.seen[eng][e2] = c

    @staticmethod
    def _flat(bs):
        out = []
        for b in bs:
            if isinstance(b, (list, tuple)):
                out.extend(Prog._flat(b))
            else:
                out.append(b)
        return out

    def op(self, eng, fn, r=(), w=(), inc=True):
        r = self._flat(r)
        w = self._flat(w)
        self._wait(eng, self._deps(r, w, eng))
        ins = fn(self.E[eng])
        self.n_ins += 1
        if inc:
            ins.then_inc(self.sem[eng], 1)
            self.cnt[eng] += 1
            c = self.cnt[eng]
        else:
            c = self.cnt[eng] + 1
        for b in w:
            b.w = (eng, c)
            b.rs = {}
        for b in r:
            if b.rs.get(eng, 0) < c:
                b.rs[eng] = c
        return ins

    def dma(self, q, out, in_, r=(), w=(), **kw):
        i = self.dma_i[q]
        self.dma_i[q] += 1
        k = "d_%s%d" % (q, i % self.NRING)
        r = self._flat(r)
        w = self._flat(w)
        deps = self._deps(r, w)
        if self.cnt[k] > 0 and deps.get(k, 0) < self.cnt[k]:
            deps[k] = self.cnt[k]
        self._wait(q, deps)
        ins = self.E[q].dma_start(out=out, in_=in_, **kw)
        self.n_ins += 1
        ins.then_inc(self.sem[k], 16)
        self.cnt[k] += 16
        c = self.cnt[k]
        for b in w:
            b.w = (k, c)
            b.rs = {}
        for b in r:
            if b.rs.get(k, 0) < c:
                b.rs[k] = c
        return ins

    def finish(self, bufs):
        deps = {}
        for b in bufs:
            if b.w is not None and deps.get(b.w[0], 0) < b.w[1]:
                deps[b.w[0]] = b.w[1]
        for k, c in self.cnt.items():
            if c > 0 and deps.get(k, 0) < c:
                deps[k] = c
        self._wait("sp", deps)


class _EngProxy:
    def __init__(self):
        self.call = None

    def __getattr__(self, name):
        def f(*a, **k):
            self.call = (name, a, k)
            return self
        return f


class Deferred:
    def __init__(self):
        self.items = []
        self.grp = None

    def _add(self, it):
        (self.grp if self.grp is not None else self.items).append(it)

    def op(self, eng, fn, r=(), w=(), inc=True):
        px = _EngProxy()
        fn(px)
        name, a, k = px.call
        self._add(("op", (eng, (lambda e, name=name, a=a, k=k: getattr(e, name)(*a, **k))),
                   dict(r=list(r), w=list(w), inc=inc)))

    def dma(self, *a, **k):
        self._add(("dma", a, k))

    def begin(self):
        self.grp = []

    def end(self):
        g, self.grp = self.grp, None
        self.items.append(("group", g, None))


def _emit_item(P, it):
    if it[0] == "group":
        for sub in it[1]:
            _emit_item(P, sub)
    else:
        getattr(P, it[0])(*it[1], **it[2])


def run_interleaved(P, ds):
    ds = [d for d in ds if d.items]
    pos = [0] * len(ds)
    while True:
        best, bf = None, None
        for i, d in enumerate(ds):
            if pos[i] < len(d.items):
                f = pos[i] / float(len(d.items))
                if bf is None or f < bf:
                    best, bf = i, f
        if best is None:
            break
        _emit_item(P, ds[best].items[pos[best]])
        pos[best] += 1


def _rev_last(ap, n):
    pat = [list(p) for p in ap.ap]
    st = pat[-1][0]
    pat[-1] = [-st, n]
    return bass.AP(ap.tensor, ap.offset + st * (n - 1), pat)


def host_constants(S):
    cst = {}
    cst["ident"] = np.eye(128, dtype=np.float32)
    j = np.arange(128)[:, None]
    i = np.arange(128)[None, :]
    cst["tri"] = (j <= i).astype(np.float32)
    h = np.arange(4, dtype=np.float64)
    gam = 1.0 - np.exp2(-5.0 - h)
    G = np.zeros((2, 64, 2, 64))
    for c_ in range(2):
        for hh_ in range(2):
            G[hh_, :, c_, :] = gam[2 * c_ + hh_] ** 128
    cst["retG"] = G.reshape(128, 128).astype(np.float32)
    pos = np.arange(S, dtype=np.float64)
    half = 32
    inv_freq = (10000.0 ** (-np.arange(half, dtype=np.float32) / half)).astype(np.float32)
    ang = (pos.astype(np.float32)[:, None] * inv_freq[None, :]).astype(np.float32).astype(np.float64)
    cosv = np.cos(ang)
    sinv = np.sin(ang)
    il = (np.arange(S) % 128).astype(np.float64)
    facq = gam[None, :] ** (il[:, None] + 1.0)
    fack = gam[None, :] ** (-(il[:, None] + 1.0)) / 8.0
    fac = np.concatenate([facq, fack], axis=1)
    tab = np.zeros((S, 2, 8, 32), dtype=np.float64)
    tab[:, 0] = cosv[:, None, :] * fac[:, :, None]
    tab[:, 1] = sinv[:, None, :] * fac[:, :, None]
    cst["retcs"] = tab.reshape(S, 512).astype(np.float32)
    am = np.ones((128, 5, 128), dtype=np.float32)
    kk = np.arange(128)[:, None]
    qi = np.arange(128)[None, :]
    am[:, 4, :] = 1.0 - ((kk >= 64) & (qi < 64)).astype(np.float32)
    am[:, 0, :] = 1.0 - ((kk < 64) & (qi >= 64)).astype(np.float32)
    cst["amask"] = am.reshape(128, 640)
    return cst


CONST_SHAPES = lambda S: {"ident": [128, 128], "tri": [128, 128], "retG": [128, 128],
                          "retcs": [S, 512], "amask": [128, 640]}

WEIGHT_SHAPES = lambda L: {
    "norm1_g": [L, D], "norm2_g": [L, D], "w_ada": [L, D, 6 * D], "b_ada": [L, 6 * D],
    "w_in": [L, D, IN_W], "attn_rel_bias": [L, 8, 192],
    "ssm_a_re": [L, 16, 64], "ssm_a_im": [L, 16, 64], "ssm_log_dt": [L, 16],
    "ssm_b_re": [L, 16, 64, 16], "ssm_b_im": [L, 16, 64, 16],
    "ssm_c_re": [L, 16, 16, 64], "ssm_c_im": [L, 16, 16, 64],
    "ssm_d": [L, 256], "ssm_w_glu": [L, 256, 256], "ssm_b_glu": [L, 256],
    "ret_gn_g": [L, 256], "w_out": [L, D, D],
    "moe_w_group": [L, D, 4], "moe_b_group": [L, 4], "moe_w_expert": [L, D, 32], "moe_b_expert": [L, 32],
    "moe_w_gate": [L, 32, D, FF], "moe_w_up": [L, 32, D, FF], "moe_w_down": [L, 32, FF, D],
    "final_g": [D],
}


def build_program(nc, S, L, taps=(), do_mixer=True, do_moe=True, run_layers=True, **extra):
    P = Prog(nc)
    NT = S // 512
    NBLK = S // 128
    tapset = set(taps)

    def din(name, shape, dt=F32):
        return nc.dram_tensor(name, list(shape), dt, kind="ExternalInput")

    x_d = din("x", [S, D])
    c_d = din("c", [8, 128])
    Wd = {k: din(k, shp) for k, shp in WEIGHT_SHAPES(L).items()}
    Cd = {k: din(k, shp) for k, shp in CONST_SHAPES(S).items()}
    y_d = nc.dram_tensor("y", [S, D], F32, kind="ExternalOutput")
    tap_d = {}

    def tap_out(name, shape):
        tap_d[name] = nc.dram_tensor("tap_" + name, list(shape), F32, kind="ExternalOutput")
        return tap_d[name]

    xT_d = nc.dram_tensor("xT_scr", [D, S], F32, kind="Internal")
    x1T_d = nc.dram_tensor("x1T_scr", [D, S], F32, kind="Internal")
    xT_b = [Buf("xT%d" % i) for i in range(NT)]
    x1T_b = [Buf("x1T%d" % i) for i in range(NT)]
    xT_v = xT_d.ap().rearrange("(c p) t -> p c t", p=128)
    x1T_v = x1T_d.ap().rearrange("(c p) t -> p c t", p=128)
    y_b = Buf("y")

    def sb(name, shape, dt):
        return nc.alloc_sbuf_tensor("sb_" + name, list(shape), dt)

    pb = [nc.alloc_psum_tensor("pb%d" % i, [128, 512], F32) for i in range(8)]
    pbB = [Buf("pb%d" % i, excl=True) for i in range(8)]

    ident = sb("ident", [128, 128], F32)
    identb = sb("identb", [128, 128], BF16)
    ones_bf = sb("ones_bf", [128, 128], BF16)
    B_const = Buf("const")
    P.dma("sp", ident[:], Cd["ident"].ap(), w=[B_const])
    P.op("dve", lambda e: e.tensor_copy(identb[:], ident[:]), r=[B_const], w=[B_const])
    P.op("dve", lambda e: e.memset(ones_bf[:], 1.0), w=[B_const])

    def load_cols(name, rows_ap, R, tag):
        st = sb("st_" + tag, [R, 128], F32)
        stB = Buf("st_" + tag)
        P.dma("sp", st[:], rows_ap, w=[stB])
        return st, stB

    def transpose_to(dst_ap, dstB, src_ap, srcB, R, bank=7):
        P.op("pe", lambda e: e.transpose(pb[bank][:, 0:R], src_ap, ident[0:R, 0:R]),
             r=[srcB, B_const], w=[pbB[bank]])
        P.op("dve", lambda e: e.tensor_copy(dst_ap, pb[bank][:, 0:R]), r=[pbB[bank]], w=[dstB])

    condT = sb("condT", [128, 8], F32)
    condB = Buf("condT")
    st_c, st_cB = load_cols("c", c_d.ap(), 8, "c")
    transpose_to(condT[:], condB, st_c[:], st_cB, 8)
    P.op("act", lambda e: e.activation(out=condT[:], in_=condT[:], func=AF.Silu), r=[condB], w=[condB])
    condTb = sb("condTb", [128, 8], BF16)
    P.op("dve", lambda e: e.tensor_copy(condTb[:], condT[:]), r=[condB], w=[condB])
    fgT = sb("fgT", [128, 8], F32)
    fgB = Buf("fgT")
    st_f, st_fB = load_cols("fg", Wd["final_g"].ap().rearrange("(r p) -> r p", p=128), 8, "fg")
    transpose_to(fgT[:], fgB, st_f[:], st_fB, 8)

    from contextlib import ExitStack
    es0 = ExitStack()
    xin = [es0.enter_context(nc.sbuf_tensor("T0_xin%d" % i, [128, D], F32)) for i in range(2)]
    xinB = [Buf("xin%d" % i) for i in range(2)]
    xtr = [es0.enter_context(nc.sbuf_tensor("T0_xtr%d" % i, [128, NCH, 128], F32)) for i in range(2)]
    xtrB = [Buf("xtr%d" % i) for i in range(2)]
    for blk in range(NBLK):
        i2 = blk % 2
        P.dma("sp", xin[i2][:], x_d.ap()[blk * 128:(blk + 1) * 128, :], w=[xinB[i2]])
        for half in range(2):
            bank = (blk * 2 + half) % 2
            for k4 in range(4):
                k = half * 4 + k4
                P.op("pe", lambda e, k=k, k4=k4, bank=bank: e.transpose(
                    pb[bank][:, k4 * 128:(k4 + 1) * 128], xin[i2][:, k * 128:(k + 1) * 128], ident[:]),
                    r=[xinB[i2], B_const], w=[pbB[bank]], inc=(k4 == 3))
            P.op("act" if half == 0 else "dve",
                 (lambda e, half=half, bank=bank: e.copy(
                     out=xtr[i2][:, half * 4:(half + 1) * 4, :].rearrange("p a b -> p (a b)"), in_=pb[bank][:]))
                 if half == 0 else
                 (lambda e, half=half, bank=bank: e.tensor_copy(
                     xtr[i2][:, half * 4:(half + 1) * 4, :].rearrange("p a b -> p (a b)"), pb[bank][:])),
                 r=[pbB[bank]], w=[xtrB[i2]])
        P.dma("sp", xT_v[:, :, blk * 128:(blk + 1) * 128], xtr[i2][:], r=[xtrB[i2]], w=[xT_b[blk // 4]])

    barrier(P)
    es0.close()
    cur_v, cur_b = xT_v, xT_b

    ctx = dict(P=P, nc=nc, S=S, L=L, NT=NT, NBLK=NBLK, Wd=Wd, Cd=Cd, pb=pb, pbB=pbB, ident=ident, identb=identb,
               ones_bf=ones_bf, B_const=B_const, condT=condT, condTb=condTb, condB=condB, transpose_to=transpose_to,
               load_cols=load_cols, xT_v=xT_v, xT_b=xT_b, x1T_v=x1T_v, x1T_b=x1T_b, tapset=tapset,
               tap_out=tap_out, sb=sb, do_mixer=do_mixer, do_moe=do_moe)
    ctx.update(extra)
    if L > 0 and run_layers:
        cur_v, cur_b = build_layers(ctx)

    xt = [sb("fx%d" % i, [128, NCH, 512], F32) for i in range(2)]
    xtB = [Buf("fx%d" % i) for i in range(2)]
    sq = [sb("fsq%d" % i, [128, 512], BF16) for i in range(2)]
    sqB = [Buf("fsq%d" % i) for i in range(2)]
    rs = sb("frs", [128, 512], F32)
    rsB = Buf("frs")
    yo = [sb("fyo%d" % i, [128, D], F32) for i in range(2)]
    yoB = [Buf("fyo%d" % i) for i in range(2)]
    for s in range(NT):
        i2 = s % 2
        X, XB = xt[i2], xtB[i2]
        P.dma("sp", X[:], cur_v[:, :, s * 512:(s + 1) * 512], r=[cur_b[s]], w=[XB])
        rms_stats(P, pb, pbB, 0, ones_bf, B_const, X, XB, sq, sqB, rs, rsB)
        for k in range(NCH):
            P.op("dve", lambda e, k=k: e.scalar_tensor_tensor(
                out=X[:, k, :], in0=X[:, k, :], scalar=fgT[:, k:k + 1], in1=rs[:], op0=ALU.mult, op1=ALU.mult),
                r=[XB, rsB, fgB], w=[XB])
        for t in range(4):
            blk = s * 4 + t
            o2 = blk % 2
            for half in range(2):
                bank = 1 + (blk * 2 + half) % 2
                for k4 in range(4):
                    k = half * 4 + k4
                    P.op("pe", lambda e, k=k, k4=k4, bank=bank, t=t: e.transpose(
                        pb[bank][:, k4 * 128:(k4 + 1) * 128], X[:, k, t * 128:(t + 1) * 128], ident[:]),
                        r=[XB, B_const], w=[pbB[bank]], inc=(k4 == 3))
                if half == 0:
                    P.op("act", lambda e, bank=bank, o2=o2: e.copy(out=yo[o2][:, 0:512], in_=pb[bank][:]),
                         r=[pbB[bank]], w=[yoB[o2]])
                else:
                    P.op("dve", lambda e, bank=bank, o2=o2: e.tensor_copy(yo[o2][:, 512:1024], pb[bank][:]),
                         r=[pbB[bank]], w=[yoB[o2]])
            P.dma("sp", y_d.ap()[blk * 128:(blk + 1) * 128, :], yo[o2][:], r=[yoB[o2]], w=[y_b])
    P.finish([y_b])
    return P, tap_d


def rms_stats(P, pb, pbB, bank, ones_bf, B_const, X, XB, sq, sqB, rs, rsB):
    for k in range(NCH):
        P.op("act", lambda e, k=k: e.activation(out=sq[k % 2][:], in_=X[:, k, :], func=AF.Square),
             r=[XB], w=[sqB[k % 2]])
        P.op("pe", lambda e, k=k: e.matmul(pb[bank][:], lhsT=ones_bf[:], rhs=sq[k % 2][:], start=(k == 0),
                                           stop=(k == NCH - 1)),
             r=[sqB[k % 2], B_const], w=[pbB[bank]], inc=True)
    P.op("act", lambda e: e.activation(out=rs[:], in_=pb[bank][:], func=AF.Sqrt, bias=EPS, scale=1.0 / D),
         r=[pbB[bank]], w=[rsB])
    P.op("dve", lambda e: e.reciprocal(rs[:], rs[:]), r=[rsB], w=[rsB])


def barrier(P):
    for e in ("pe", "act", "dve", "pool", "sp"):
        P._wait(e, {k: c for k, c in P.cnt.items() if c > 0})


def build_layers(ctx):
    from contextlib import ExitStack
    P = ctx["P"]; nc = ctx["nc"]; S = ctx["S"]; L = ctx["L"]; NT = ctx["NT"]
    Wd = ctx["Wd"]; Cd = ctx["Cd"]; pb = ctx["pb"]; pbB = ctx["pbB"]; sb = ctx["sb"]
    ident = ctx["ident"]; identb = ctx["identb"]; ones_bf = ctx["ones_bf"]; B_const = ctx["B_const"]
    condT = ctx["condT"]; condTb = ctx["condTb"]; condB = ctx["condB"]; transpose_to = ctx["transpose_to"]
    xT_v = ctx["xT_v"]; xT_b = ctx["xT_b"]; x1T_v = ctx["x1T_v"]; x1T_b = ctx["x1T_b"]
    tapset = ctx["tapset"]; tap_out = ctx["tap_out"]
    do_attn = ctx.get("do_attn", True); do_ret = ctx.get("do_ret", True); do_s5 = ctx.get("do_s5", True); RS = ctx.get("ret_stage", 99)

    pb7b = pb[7][:].bitcast(BF16)

    stA = sb("stA", [80, 128], F32); stAB = Buf("stA")
    vecT = sb("vecT", [128, 80], F32); vecB = Buf("vecT")
    st_ld = sb("st_ld", [8, 2], F32); st2 = sb("st2", [8, 128], F32); st2B = Buf("st2")
    ldtT = sb("ldtT", [128, 8], F32); ldtB = Buf("ldtT")
    st3 = sb("st3", [4, 128], F32); st3B = Buf("st3")
    sdT = sb("sdT", [128, 4], F32); sdB = Buf("sdT")
    modT = sb("modT", [128, 48], F32); modB = Buf("modT")
    modN = sb("modN", [128, 48], F32); modNB = Buf("modN")
    gm = sb("gm", [128, 16], F32); gmB = Buf("gm")
    tri_sb = sb("tri", [128, 128], F32)
    retG_sb = sb("retG", [128, 128], F32)
    amask_sb = sb("amask", [128, 640], BF16)
    P.dma("sp", tri_sb[:], Cd["tri"].ap(), w=[B_const])
    P.dma("sp", retG_sb[:], Cd["retG"].ap(), w=[B_const])
    P.dma("pool", amask_sb[:], Cd["amask"].ap(), w=[B_const])
    Fd = nc.dram_tensor("F_scr", [L, 8, 768], F32, kind="Internal")
    FdB = Buf("Fd")

    C_BADA, C_N1, C_N2, C_ARE, C_AIM = 0, 48, 56, 64, 72

    TS = 128
    s5 = {}
    TWO_PI = 2.0 * math.pi

    def s5_setup(l, es, a):
        def sba(name, shape, dt):
            return es.enter_context(nc.sbuf_tensor("S5_%d_%s" % (l, name), list(shape), dt))
        vecT, vecB, ldtT, ldtB = a["vecT"], a["vecB"], a["ldtT"], a["ldtB"]
        Ec = sba("Ec", [128, 8, TS], F32); Es = sba("Es", [128, 8, TS], F32); EB = Buf("E")
        WB = sba("WB", [128, 16, 128], BF16); WBB = Buf("WB")
        WC = sba("WC", [128, 24, 128], BF16); WCB = Buf("WC")
        E16 = sba("E16", [128, 2, 8, TS], BF16); E16B = Buf("E16")
        wglu = sba("wglu", [128, 2, 256], BF16); wgluB = Buf("wglu")
        sm = sba("sm", [128, 24, 8], F32); smB = Buf("sm")
        smi = sba("smi", [128, 8], I32)
        Braw = sba("Braw", [128, 2, 8, 16], F32); BrawB = Buf("Braw")
        Bbar = sba("Bbar", [128, 2, 8, 16], F32); BbarB = Buf("Bbar")
        Bt = sba("Bt", [128, 2, 8, 16], F32); BtB = Buf("Bt")
        Bx = sba("Bx", [128, 128], F32); BxB = Buf("Bx")
        Cx = sba("Cx", [128, 128], F32); CxB = Buf("Cx")
        Xst = sba("Xst", [128, 2, 8], F32); XstB = Buf("Xst")
        ct = sba("ct", [128, 4], F32); ctB = Buf("ct")
        mt, mtB = a["mt"], a["mtB"]
        zb = sba("zb", [128, 2, 2, 256], BF16); zbB = [[Buf("zb%d_%d" % (s_, i)) for i in range(2)] for s_ in range(2)]
        pr = sba("pr", [128, 2, 4, 256], BF16); prB = [[Buf("pr%d_%d" % (s_, i)) for i in range(4)] for s_ in range(2)]
        glb = sba("glb", [128, 2, 512], BF16); glB = Buf("gl")
        s5.update(Ec=Ec, Es=Es, EB=EB, WB=WB, WBB=WBB, WC=WC, WCB=WCB, wglu=wglu, wgluB=wgluB, sm=sm, smB=smB,
                  Xst=Xst, XstB=XstB, ct=ct, ctB=ctB, mt=mt, mtB=mtB, zb=zb, zbB=zbB, pr=pr, prB=prB, glb=glb,
                  glB=glB, a=a, E16=E16, E16B=E16B)
        V = lambda i: sm[:, i, :]
        are, aim = vecT[:, C_ARE:C_ARE + 8], vecT[:, C_AIM:C_AIM + 8]
        DT, T1, MAG, TH, YV, KF, FR, G1, SIN, COS, AR, AI, NR, DEN, CR, CI, T2, T3 = range(18)

        def dve(fn, r=(), w=()):
            P.op("dve", fn, r=list(r) + [smB], w=list(w) + [smB])

        def act(fn, r=(), w=()):
            P.op("act", fn, r=list(r) + [smB], w=list(w) + [smB])

        P.dma("pool", wglu[:], Wd["ssm_w_glu"].ap()[l].rearrange("(k p) n -> p k n", p=128), w=[wgluB])
        act(lambda e: e.activation(out=V(DT), in_=ldtT[:], func=AF.Exp), r=[ldtB])
        dve(lambda e: e.tensor_tensor(out=V(T1), in0=are, in1=V(DT), op=ALU.mult), r=[vecB])
        act(lambda e: e.activation(out=V(MAG), in_=V(T1), func=AF.Exp))
        dve(lambda e: e.tensor_tensor(out=V(TH), in0=aim, in1=V(DT), op=ALU.mult), r=[vecB])

        def sin_of(dst, shift):
            dve(lambda e: e.tensor_scalar(out=V(YV), in0=V(TH), scalar1=1.0 / TWO_PI, scalar2=shift, op0=ALU.mult,
                                          op1=ALU.add))
            dve(lambda e: e.tensor_copy(smi[:], V(YV)))
            dve(lambda e: e.tensor_copy(V(KF), smi[:]))
            dve(lambda e: e.tensor_tensor(out=V(FR), in0=V(YV), in1=V(KF), op=ALU.subtract))
            dve(lambda e: e.tensor_single_scalar(out=V(G1), in_=V(FR), scalar=0.5, op=ALU.is_gt))
            dve(lambda e: e.tensor_tensor(out=V(FR), in0=V(FR), in1=V(G1), op=ALU.subtract))
            dve(lambda e: e.tensor_single_scalar(out=V(G1), in_=V(FR), scalar=-0.5, op=ALU.is_lt))
            dve(lambda e: e.tensor_tensor(out=V(FR), in0=V(FR), in1=V(G1), op=ALU.add))
            act(lambda e: e.activation(out=V(dst), in_=V(FR), func=AF.Sin, scale=TWO_PI))

        sin_of(SIN, 0.0)
        sin_of(COS, 0.25)
        dve(lambda e: e.tensor_tensor(out=V(AR), in0=V(MAG), in1=V(COS), op=ALU.mult))
        dve(lambda e: e.tensor_tensor(out=V(AI), in0=V(MAG), in1=V(SIN), op=ALU.mult))
        dve(lambda e: e.tensor_scalar(out=V(NR), in0=V(AR), scalar1=-1.0, scalar2=None, op0=ALU.add))
        dve(lambda e: e.tensor_tensor(out=V(DEN), in0=are, in1=are, op=ALU.mult), r=[vecB])
        dve(lambda e: e.tensor_tensor(out=V(T2), in0=aim, in1=aim, op=ALU.mult), r=[vecB])
        dve(lambda e: e.tensor_tensor(out=V(DEN), in0=V(DEN), in1=V(T2), op=ALU.add))
        dve(lambda e: e.reciprocal(V(DEN), V(DEN)))
        dve(lambda e: e.tensor_tensor(out=V(T2), in0=V(NR), in1=are, op=ALU.mult), r=[vecB])
        dve(lambda e: e.tensor_tensor(out=V(T3), in0=V(AI), in1=aim, op=ALU.mult), r=[vecB])
        dve(lambda e: e.tensor_tensor(out=V(T2), in0=V(T2), in1=V(T3), op=ALU.add))
        dve(lambda e: e.tensor_tensor(out=V(CR), in0=V(T2), in1=V(DEN), op=ALU.mult))
        dve(lambda e: e.tensor_tensor(out=V(T2), in0=V(AI), in1=are, op=ALU.mult), r=[vecB])
        dve(lambda e: e.tensor_tensor(out=V(T3), in0=V(NR), in1=aim, op=ALU.mult), r=[vecB])
        dve(lambda e: e.tensor_tensor(out=V(T2), in0=V(T2), in1=V(T3), op=ALU.subtract))
        dve(lambda e: e.tensor_tensor(out=V(CI), in0=V(T2), in1=V(DEN), op=ALU.mult))
        for ri, nm in ((0, "ssm_b_re"), (1, "ssm_b_im")):
            src = Wd[nm].ap()[l].rearrange("(cb j) p c -> (j p) cb c", j=2)
            P.dma("sp", Braw[:, ri, :, :], src, w=[BrawB])
        bc = lambda i: sm[:, i, :, None].to_broadcast([128, 8, 16])
        P.op("dve", lambda e: e.tensor_tensor(out=Bbar[:, 0], in0=Braw[:, 0], in1=bc(CR), op=ALU.mult), r=[BrawB, smB], w=[BbarB])
        P.op("dve", lambda e: e.tensor_tensor(out=Bt[:, 0], in0=Braw[:, 1], in1=bc(CI), op=ALU.mult), r=[BrawB, smB], w=[BtB])
        P.op("dve", lambda e: e.tensor_tensor(out=Bbar[:, 0], in0=Bbar[:, 0], in1=Bt[:, 0], op=ALU.subtract), r=[BbarB, BtB], w=[BbarB])
        P.op("dve", lambda e: e.tensor_tensor(out=Bbar[:, 1], in0=Braw[:, 1], in1=bc(CR), op=ALU.mult), r=[BrawB, smB], w=[BbarB])
        P.op("dve", lambda e: e.tensor_tensor(out=Bt[:, 1], in0=Braw[:, 0], in1=bc(CI), op=ALU.mult), r=[BrawB, smB], w=[BtB])
        P.op("dve", lambda e: e.tensor_tensor(out=Bbar[:, 1], in0=Bbar[:, 1], in1=Bt[:, 1], op=ALU.add), r=[BbarB, BtB], w=[BbarB])
        for cb in range(8):
            q = cb % 4
            for ri in range(2):
                P.op("dve", lambda e: e.memset(Bx[:], 0.0), w=[BxB])
                for j in range(2):
                    P.op("dve", lambda e, j=j, q=q, ri=ri, cb=cb: e.tensor_copy(
                        Bx[64 * j:64 * j + 64, 32 * q + 16 * j:32 * q + 16 * j + 16], Bbar[64 * j:64 * j + 64, ri, cb, :]),
                        r=[BbarB], w=[BxB])
                P.op("pe", lambda e: e.transpose(pb[6][:, 0:128], Bx[:], ident[:]), r=[BxB, B_const], w=[pbB[6]])
                P.op("act", lambda e, cb=cb, ri=ri: e.copy(out=WB[:, cb * 2 + ri, :], in_=pb[6][:, 0:128]),
                     r=[pbB[6]], w=[WBB])
        P.op("dve", lambda e: e.memset(WC[:].rearrange("p a b -> p (a b)"), 0.0), w=[WCB])
        for ri, nm in ((0, "ssm_c_re"), (1, "ssm_c_im")):
            for hh in range(2):
                src = Wd[nm].ap()[l][8 * hh:8 * hh + 8].rearrange("g c p -> (g c) p")
                P.dma("sp", Cx[:, 0:64], src, w=[CxB])
                P.dma("sp", Cx[:, 64:128], src, w=[CxB])
                P.op("pe", lambda e: e.transpose(pb[6][:, 0:128], Cx[:], ident[:]), r=[CxB, B_const], w=[pbB[6]])
                P.op("act", lambda e: e.copy(out=Bx[:], in_=pb[6][:, 0:128]), r=[pbB[6]], w=[BxB])
                for q in range(4):
                    cb = 4 * hh + q
                    for j in range(2):
                        cols = slice(32 * q + 16 * j, 32 * q + 16 * j + 16)
                        for (slot_, sgn) in (((0, 1.0), (2, -1.0)) if ri == 0 else ((1, -1.0),)):
                            P.op("dve", lambda e, cb=cb, slot_=slot_, sgn=sgn, j=j, cols=cols: e.tensor_scalar(
                                out=WC[64 * j:64 * j + 64, cb * 3 + slot_, cols], in0=Bx[64 * j:64 * j + 64, cols],
                                scalar1=sgn, scalar2=None, op0=ALU.mult), r=[BxB], w=[WCB])
        P.op("dve", lambda e: e.tensor_copy(Ec[:, :, 0:1], sm[:, COS, :, None]), r=[smB], w=[EB])
        P.op("dve", lambda e: e.tensor_copy(Es[:, :, 0:1], sm[:, SIN, :, None]), r=[smB], w=[EB])
        n = 1
        tA = mt[0][:].rearrange("p (a b) -> p a b", a=8)
        tB_ = mt[1][:].rearrange("p (a b) -> p a b", a=8)
        while n < TS:
            cc = Ec[:, :, n - 1:n].to_broadcast([128, 8, n])
            ss = Es[:, :, n - 1:n].to_broadcast([128, 8, n])
            P.op("dve", lambda e, n=n, cc=cc: e.tensor_tensor(out=tA[:, :, 0:n], in0=Ec[:, :, 0:n], in1=cc, op=ALU.mult), r=[EB], w=[mtB[0]])
            P.op("dve", lambda e, n=n, ss=ss: e.tensor_tensor(out=tB_[:, :, 0:n], in0=Es[:, :, 0:n], in1=ss, op=ALU.mult), r=[EB], w=[mtB[1]])
            P.op("dve", lambda e, n=n: e.tensor_tensor(out=Ec[:, :, n:2 * n], in0=tA[:, :, 0:n], in1=tB_[:, :, 0:n], op=ALU.subtract), r=[mtB[0], mtB[1]], w=[EB])
            P.op("dve", lambda e, n=n, ss=ss: e.tensor_tensor(out=tA[:, :, 0:n], in0=Ec[:, :, 0:n], in1=ss, op=ALU.mult), r=[EB], w=[mtB[0]])
            P.op("dve", lambda e, n=n, cc=cc: e.tensor_tensor(out=tB_[:, :, 0:n], in0=Es[:, :, 0:n], in1=cc, op=ALU.mult), r=[EB], w=[mtB[1]])
            P.op("dve", lambda e, n=n: e.tensor_tensor(out=Es[:, :, n:2 * n], in0=tA[:, :, 0:n], in1=tB_[:, :, 0:n], op=ALU.add), r=[mtB[0], mtB[1]], w=[EB])
            n *= 2
        P.op("dve", lambda e: e.memset(Xst[:].rearrange("p a b -> p (a b)"), 0.0), w=[XstB])
        P.op("act", lambda e: e.copy(out=E16[:, 0].rearrange("p a b -> p (a b)"), in_=Ec[:].rearrange("p a b -> p (a b)")), r=[EB], w=[E16B])
        P.op("act", lambda e: e.copy(out=E16[:, 1].rearrange("p a b -> p (a b)"), in_=Es[:].rearrange("p a b -> p (a b)")), r=[EB], w=[E16B])
        s5["MAG"] = MAG

    def s5_gen(s, P):
        a = s5["a"]
        uT32, uTb, uTB, YT, YTB, sdT, sdB = a["uT32"], a["uTb"], a["uTB"], a["YT"], a["YTB"], a["sdT"], a["sdB"]
        Ec, Es, EB, WB, WBB, WC, WCB = s5["Ec"], s5["Es"], s5["EB"], s5["WB"], s5["WBB"], s5["WC"], s5["WCB"]
        E16, E16B = s5["E16"], s5["E16B"]
        sm, smB, Xst, XstB, ct, ctB = s5["sm"], s5["smB"], s5["Xst"], s5["XstB"], s5["ct"], s5["ctB"]
        mt, mtB = s5["mt"], s5["mtB"]
        zb, zbB, pr, prB = s5["zb"], s5["zbB"], s5["pr"], s5["prB"]
        glb, glB, wglu, wgluB = s5["glb"], s5["glB"], s5["wglu"], s5["wgluB"]
        MAG = s5["MAG"]
        HW = 256
        v2 = lambda ap: ap.rearrange("p (n t) -> p n t", n=2)
        unit = 0
        for half in range(2):
            tcols = slice(half * HW, (half + 1) * HW)
            for cb in range(8):
                st_ = unit % 2
                unit += 1
                kc = cb // 4
                T = [mt[i][:, st_ * HW:(st_ + 1) * HW] for i in range(4)]
                TB = [mtB[i][st_] for i in range(4)]
                ZB, ZBB = zb[:, st_], zbB[st_]
                PR, PRB = pr[:, st_], prB[st_]
                bank = st_
                Ecb = Ec[:, cb, None, :].to_broadcast([128, 2, TS])
                Esb = Es[:, cb, None, :].to_broadcast([128, 2, TS])
                Ec16 = E16[:, 0, cb, None, :].to_broadcast([128, 2, TS])
                Es16 = E16[:, 1, cb, None, :].to_broadcast([128, 2, TS])
                P.op("pe", lambda e: e.matmul(pb[bank][:, 0:HW], lhsT=WB[:, cb * 2, :], rhs=uTb[:, kc, tcols], start=True, stop=True),
                     r=[WBB, uTB], w=[pbB[bank]], inc=False)
                P.op("pe", lambda e: e.matmul(pb[bank][:, HW:2 * HW], lhsT=WB[:, cb * 2 + 1, :], rhs=uTb[:, kc, tcols], start=True, stop=True),
                     r=[WBB, uTB], w=[pbB[bank]])
                bur, bui = v2(pb[bank][:, 0:HW]), v2(pb[bank][:, HW:2 * HW])
                P.op("dve", lambda e: e.tensor_tensor(out=v2(T[0]), in0=bur, in1=Ecb, op=ALU.mult), r=[pbB[bank], EB], w=[TB[0]])
                P.op("dve", lambda e: e.tensor_tensor(out=v2(T[1]), in0=bui, in1=Esb, op=ALU.mult), r=[pbB[bank], EB], w=[TB[1]])
                P.op("dve", lambda e: e.tensor_tensor(out=v2(T[2]), in0=bui, in1=Ecb, op=ALU.mult), r=[pbB[bank], EB], w=[TB[2]])
                P.op("dve", lambda e: e.tensor_tensor(out=v2(T[3]), in0=bur, in1=Esb, op=ALU.mult), r=[pbB[bank], EB], w=[TB[3]])
                P.op("pool", lambda e: e.tensor_tensor(out=T[0], in0=T[0], in1=T[1], op=ALU.add), r=[TB[0], TB[1]], w=[TB[0]])
                P.op("pool", lambda e: e.tensor_tensor(out=T[2], in0=T[2], in1=T[3], op=ALU.subtract), r=[TB[2], TB[3]], w=[TB[2]])
                yield
                magb = sm[:, MAG, cb:cb + 1].to_broadcast([128, TS])
                for n in range(2):
                    cs_ = slice(n * TS, (n + 1) * TS)
                    P.op("dve", lambda e: e.tensor_tensor_scan(
                        out=T[1][:, cs_], data0=magb, data1=T[0][:, cs_], initial=Xst[:, 0, cb:cb + 1], op0=ALU.mult, op1=ALU.add),
                        r=[TB[0], XstB, smB], w=[TB[1]])
                    P.op("dve", lambda e: e.tensor_tensor_scan(
                        out=T[3][:, cs_], data0=magb, data1=T[2][:, cs_], initial=Xst[:, 1, cb:cb + 1], op0=ALU.mult, op1=ALU.add),
                        r=[TB[2], XstB, smB], w=[TB[3]])
                    last = (n + 1) * TS - 1
                    zrl, zil = T[1][:, last:last + 1], T[3][:, last:last + 1]
                    ecl, esl = Ec[:, cb, TS - 1:TS], Es[:, cb, TS - 1:TS]
                    P.op("dve", lambda e: e.tensor_tensor(out=ct[:, 0:1], in0=zil, in1=esl, op=ALU.mult), r=[TB[3], EB], w=[ctB])
                    P.op("dve", lambda e: e.tensor_tensor(out=ct[:, 1:2], in0=zil, in1=ecl, op=ALU.mult), r=[TB[3], EB], w=[ctB])
                    P.op("dve", lambda e: e.scalar_tensor_tensor(
                        out=Xst[:, 0, cb:cb + 1], in0=zrl, scalar=ecl, in1=ct[:, 0:1], op0=ALU.mult, op1=ALU.subtract),
                        r=[TB[1], EB, ctB], w=[XstB])
                    P.op("dve", lambda e: e.scalar_tensor_tensor(
                        out=Xst[:, 1, cb:cb + 1], in0=zrl, scalar=esl, in1=ct[:, 1:2], op0=ALU.mult, op1=ALU.add),
                        r=[TB[1], EB, ctB], w=[XstB])
                P.op("act", lambda e: e.copy(out=ZB[:, 0, :], in_=T[1]), r=[TB[1]], w=[ZBB[0]])
                P.op("act", lambda e: e.copy(out=ZB[:, 1, :], in_=T[3]), r=[TB[3]], w=[ZBB[1]])
                yield
                for (i_, zi_, tab) in ((0, 0, Ec16), (1, 1, Es16), (2, 0, Es16), (3, 1, Ec16)):
                    P.op("dve", lambda e, i_=i_, zi_=zi_, tab=tab: e.tensor_tensor(
                        out=v2(PR[:, i_, :]), in0=v2(ZB[:, zi_, :]), in1=tab, op=ALU.mult), r=[ZBB[zi_], E16B], w=[PRB[i_]])
                for (i_, wsel) in ((0, 0), (1, 2), (2, 1), (3, 1)):
                    P.op("pe", lambda e, i_=i_, wsel=wsel: e.matmul(
                        pb[6][:, 0:HW], lhsT=WC[:, cb * 3 + wsel, :], rhs=PR[:, i_, :], start=(cb % 4 == 0 and i_ == 0),
                        stop=(cb % 4 == 3 and i_ == 3)), r=[WCB, PRB[i_]], w=[pbB[6]], inc=(i_ == 3))
                if cb % 4 == 3:
                    mc = cb // 4
                    P.op("dve", lambda e: e.scalar_tensor_tensor(
                        out=T[0], in0=uT32[:, mc, tcols], scalar=sdT[:, mc:mc + 1], in1=pb[6][:, 0:HW], op0=ALU.mult, op1=ALU.add),
                        r=[uTB, sdB, pbB[6]], w=[TB[0]])
                    P.op("act", lambda e: e.activation(out=glb[:, mc, tcols], in_=T[0], func=AF.Gelu_apprx_tanh),
                         r=[TB[0]], w=[glB])
                yield
        for m in range(2):
            P.begin()
            for k in range(2):
                P.op("pe", lambda e, m=m, k=k: e.matmul(pb[7][:], lhsT=wglu[:, k, m * 128:(m + 1) * 128], rhs=glb[:, k, :],
                                                        start=(k == 0), stop=(k == 1)), r=[wgluB, glB], w=[pbB[7]], inc=(k == 1))
            P.op("act", lambda e, m=m: e.activation(out=mt[m][:], in_=pb[7][:], func=AF.Sigmoid, bias=sdT[:, 2 + m:3 + m], scale=1.0),
                 r=[pbB[7], sdB], w=[mtB[m]])
            P.end()
            P.op("dve", lambda e, m=m: e.tensor_tensor(out=YT[:, 4 + m, :], in0=glb[:, m, :], in1=mt[m][:], op=ALU.mult),
                 r=[glB, mtB[m]], w=[YTB[4 + m]])
        yield

    ctx["s5_setup"] = s5_setup
    ctx["s5_gen"] = s5_gen

    HS = min(S, 2048)
    NHALF = S // HS
    NST = HS // 512
    BIG = 1.0e30

    def moe_phase(l, a):
        sh2, g2, gm2, modB, gmB = a["sh2"], a["g2"], a["gm2"], a["modB"], a["gmB"]
        with ExitStack() as es:
            def sba(name, shape, dt):
                return es.enter_context(nc.sbuf_tensor("B%d_%s" % (l, name), list(shape), dt))
            acc = sba("acc", [128, 8, HS], F32); accB = [Buf("acc%d" % i) for i in range(NST)]
            h2T = sba("h2T", [128, 8, HS], BF16); h2B = [Buf("h2T%d" % i) for i in range(NST)]
            h32 = sba("h32", [128, 8, 512], F32); h32B = Buf("h32")
            sq = [sba("sq%d" % i, [128, 512], BF16) for i in range(2)]; sqB = [Buf("sq%d" % i) for i in range(2)]
            rs = sba("rs", [128, 512], F32); rsB = Buf("rs")
            tmp = [sba("tmp0", [128, 512], F32)] * 2; tmpB = [Buf("tmp0")] * 2
            wr_sb = sba("wr", [128, 8, 36], F32); wrB = Buf("wr")
            rb_bc = sba("rb", [128, 36], F32); rbB = Buf("rb")
            combT = sba("combT", [128, HS], BF16); combB = [Buf("combT%d" % i) for i in range(NST)]
            sel = sba("sel", [128, 32, 128], BF16); selB = Buf("sel")
            wgu = [[sba("wgu%d_%d" % (i, j), [128, 8, 512], BF16) for j in range(2)] for i in range(2)]
            wguB = [[Buf("wgu%d_%d" % (i, j)) for j in range(2)] for i in range(2)]
            wd = [[sba("wd%d_%d" % (i, j), [128, 2, 1024], BF16) for j in range(2)] for i in range(2)]
            wdB = [[Buf("wd%d_%d" % (i, j)) for j in range(2)] for i in range(2)]
            sgt = sba("sgt", [128, 2, 512], BF16); sgB = [Buf("sg0"), Buf("sg1")]
            tt = sba("tt", [128, 2, 512], BF16); ttB = [Buf("tt0"), Buf("tt1")]
            actT = [sba("actT%d" % i, [128, 2, 2, 512], BF16) for i in range(2)]
            actB = [[[Buf("act%d_%d_%d" % (i, j, f)) for f in range(2)] for j in range(2)] for i in range(2)]
            wa2 = [sba("wa2_%d" % i, [128, 8, 128], BF16) for i in range(2)]; wa2B = [Buf("wa2_%d" % i) for i in range(2)]
            cbs = [sba("cbs%d" % i, [128, 2, 512], BF16) for i in range(2)]
            cbsB = [[Buf("cbs%d_%d" % (i, j)) for j in range(2)] for i in range(2)]
            rt = actT[0][:].rearrange("p a b c -> p (a b c)").bitcast(F32); rtB = Buf("rt")
            cB = Buf("comb")

            P.op("dve", lambda e: e.memset(sel[:].rearrange("p a b -> p (a b)"), 0.0), w=[selB])
            P.op("dve", lambda e: e.tensor_copy(sel[0:32, :, :], identb[0:32, 0:32, None].to_broadcast([32, 32, 128])),
                 r=[B_const], w=[selB])
            P.op("dve", lambda e: e.memset(combT[:], 0.0), w=combB)
            P.dma("sp", wr_sb[:, :, 0:4], Wd["moe_w_group"].ap()[l].rearrange("(k p) n -> p k n", p=128), w=[wrB])
            P.dma("sp", wr_sb[:, :, 4:36], Wd["moe_w_expert"].ap()[l].rearrange("(k p) n -> p k n", p=128), w=[wrB])
            P.dma("sp", rb_bc[:, 0:4], Wd["moe_b_group"].ap()[l:l + 1, :].to_broadcast([128, 4]), w=[rbB])
            P.dma("sp", rb_bc[:, 4:36], Wd["moe_b_expert"].ap()[l:l + 1, :].to_broadcast([128, 32]), w=[rbB])

            def load_pair(p):
                for j in range(2):
                    e = 2 * p + j
                    W_, WB_ = wgu[p % 2][j], wguB[p % 2][j]
                    P.dma("pool", W_[:, :, 0:256], Wd["moe_w_gate"].ap()[l, e].rearrange("(k p) f -> p k f", p=128), w=[WB_])
                    P.dma("pool", W_[:, :, 256:512], Wd["moe_w_up"].ap()[l, e].rearrange("(k p) f -> p k f", p=128), w=[WB_])
                    P.dma("pool", wd[p % 2][j][:], Wd["moe_w_down"].ap()[l, e].rearrange("(k p) d -> p k d", p=128),
                          w=[wdB[p % 2][j]])

            for hf in range(NHALF):
                t0 = hf * HS
                def norm_part(PP, st):
                    cols = slice(st * 512, (st + 1) * 512)
                    gs = (t0 // 512) + st
                    X = acc[:, :, cols]
                    PP.dma("sp", X, x1T_v[:, :, t0 + st * 512:t0 + (st + 1) * 512], r=[x1T_b[gs]], w=[accB[st]])
                    rms_stats(PP, pb, pbB, st % 2, ones_bf, B_const, X, accB[st], sq, sqB, rs, rsB)
                    for k in range(8):
                        T_, TB_ = tmp[k % 2], tmpB[k % 2]
                        PP.op("dve", lambda e, k=k, T_=T_, X=X: e.scalar_tensor_tensor(
                            out=T_[:], in0=X[:, k, :], scalar=gm2(k), in1=rs[:], op0=ALU.mult, op1=ALU.mult),
                            r=[accB[st], rsB, gmB], w=[TB_])
                        PP.op("act", lambda e, k=k, T_=T_: e.activation(out=h32[:, k, :], in_=T_[:], func=AF.Identity,
                                                                        bias=sh2(k), scale=1.0), r=[TB_, modB], w=[h32B])
                    PP.op("pool", lambda e, cols=cols: e.tensor_copy(h2T[:, :, cols], h32[:]), r=[h32B], w=[h2B[st]])

                def router_mm(st):
                    bk = 2 + st % 2
                    for t in range(4):
                        tok = slice(t * 128, (t + 1) * 128)
                        for k in range(8):
                            P.op("pe", lambda e, k=k, bk=bk, tok=tok, t=t: e.matmul(
                                pb[bk][:, t * 36:(t + 1) * 36], lhsT=h32[:, k, tok], rhs=wr_sb[:, k, :], start=(k == 0),
                                stop=(k == 7)), r=[h32B, wrB], w=[pbB[bk]], inc=(k == 7))

                def router_chain(PP, st):
                    bk = 2 + st % 2
                    LGS = rt[:, 0:144].rearrange("p (t x) -> p t x", t=4)
                    off = [144]

                    def alloc(n):
                        o = off[0]; off[0] += 4 * n
                        return rt[:, o:o + 4 * n].rearrange("p (t x) -> p t x", t=4)
                    GM, GD, GOH, GEX, GS, PEN = alloc(1), alloc(4), alloc(4), alloc(4), alloc(1), alloc(4)
                    MK, M1, D1, OH1, MK2, M2, OH2 = alloc(32), alloc(1), alloc(32), alloc(32), alloc(32), alloc(1), alloc(32)
                    DD, W1, W2, CMB = alloc(1), alloc(1), alloc(1), alloc(32)
                    bc = lambda ap, n: ap.to_broadcast([128, 4, n])

                    def dv(fn, extra_r=()):
                        PP.op("dve", fn, r=[rtB] + list(extra_r), w=[rtB])

                    PP.op("dve", lambda e, bk=bk: e.tensor_tensor(
                        out=LGS, in0=pb[bk][:, 0:144].rearrange("p (t x) -> p t x", t=4),
                        in1=rb_bc[:, None, :].to_broadcast([128, 4, 36]), op=ALU.add), r=[pbB[bk], rbB, rtB], w=[rtB])
                    dv(lambda e: e.tensor_reduce(out=GM, in_=LGS[:, :, 0:4], axis=AX.X, op=ALU.max))
                    dv(lambda e: e.tensor_tensor(out=GD, in0=LGS[:, :, 0:4], in1=bc(GM, 4), op=ALU.subtract))
                    dv(lambda e: e.tensor_single_scalar(out=GOH, in_=GD, scalar=0.0, op=ALU.is_equal))
                    PP.op("act", lambda e: e.activation(out=GEX, in_=GD, func=AF.Exp), r=[rtB], w=[rtB])
                    dv(lambda e: e.tensor_reduce(out=GS, in_=GEX, axis=AX.X, op=ALU.add))
                    dv(lambda e: e.reciprocal(GS, GS))
                    dv(lambda e: e.tensor_scalar(out=PEN, in0=GOH, scalar1=-1.0, scalar2=BIG, op0=ALU.add, op1=ALU.mult))
                    for t in range(4):
                        mk3 = MK[:, t, :].rearrange("p (g x) -> p g x", g=4)
                        el3 = LGS[:, t, 4:36].rearrange("p (g x) -> p g x", g=4)
                        dv(lambda e, mk3=mk3, el3=el3, t=t: e.tensor_tensor(
                            out=mk3, in0=el3, in1=GOH[:, t, :, None].to_broadcast([128, 4, 8]), op=ALU.mult))
                        dv(lambda e, mk3=mk3, t=t: e.tensor_tensor(
                            out=mk3, in0=mk3, in1=PEN[:, t, :, None].to_broadcast([128, 4, 8]), op=ALU.add))
                    dv(lambda e: e.tensor_reduce(out=M1, in_=MK, axis=AX.X, op=ALU.max))
                    dv(lambda e: e.tensor_tensor(out=D1, in0=MK, in1=bc(M1, 32), op=ALU.subtract))
                    dv(lambda e: e.tensor_single_scalar(out=OH1, in_=D1, scalar=0.0, op=ALU.is_equal))
                    dv(lambda e: e.scalar_tensor_tensor(out=MK2, in0=OH1, scalar=-BIG, in1=MK, op0=ALU.mult, op1=ALU.add))
                    dv(lambda e: e.tensor_reduce(out=M2, in_=MK2, axis=AX.X, op=ALU.max))
                    dv(lambda e: e.tensor_tensor(out=D1, in0=MK2, in1=bc(M2, 32), op=ALU.subtract))
                    dv(lambda e: e.tensor_single_scalar(out=OH2, in_=D1, scalar=0.0, op=ALU.is_equal))
                    dv(lambda e: e.tensor_tensor(out=DD, in0=M2, in1=M1, op=ALU.subtract))
                    PP.op("act", lambda e: e.activation(out=DD, in_=DD, func=AF.Exp), r=[rtB], w=[rtB])
                    dv(lambda e: e.tensor_scalar(out=W1, in0=DD, scalar1=1.0, scalar2=None, op0=ALU.add))
                    dv(lambda e: e.reciprocal(W1, W1))
                    dv(lambda e: e.tensor_tensor(out=W2, in0=DD, in1=W1, op=ALU.mult))
                    dv(lambda e: e.tensor_tensor(out=W1, in0=W1, in1=GS, op=ALU.mult))
                    dv(lambda e: e.tensor_tensor(out=W2, in0=W2, in1=GS, op=ALU.mult))
                    dv(lambda e: e.tensor_tensor(out=OH1, in0=OH1, in1=bc(W1, 32), op=ALU.mult))
                    dv(lambda e: e.tensor_tensor(out=OH2, in0=OH2, in1=bc(W2, 32), op=ALU.mult))
                    PP.op("dve", lambda e: e.tensor_tensor(out=CMB, in0=OH1, in1=OH2, op=ALU.add), r=[rtB], w=[rtB, cB])
                    for t in range(4):
                        PP.op("pe", lambda e, t=t: e.transpose(pb[4][0:32, t * 128:(t + 1) * 128], CMB[:, t, :], ident[:]),
                             r=[rtB, cB, B_const], w=[pbB[4]], inc=(t == 3))
                    PP.op("act", lambda e, st=st: e.copy(out=combT[0:32, st * 512:(st + 1) * 512], in_=pb[4][0:32, :]),
                         r=[pbB[4]], w=[combB[st]])
                    if "comb" in tapset:
                        if "tapcomb" not in ctx:
                            ctx["tapcomb"] = tap_out("comb", [L, S, 32]); ctx["tapcombB"] = Buf("tapcomb")
                        for t in range(4):
                            g0 = t0 + st * 512 + t * 128
                            PP.dma("sp", ctx["tapcomb"].ap()[l][g0:g0 + 128, :], CMB[:, t, :], r=[rtB, cB], w=[ctx["tapcombB"]])

                barrier(P)
                norm_part(P, 0)
                for st in range(NST):
                    router_mm(st)
                    d1 = Deferred()
                    router_chain(d1, st)
                    ds = [d1]
                    if st + 1 < NST:
                        d2 = Deferred()
                        norm_part(d2, st + 1)
                        ds.append(d2)
                    run_interleaved(P, ds)
                barrier(P)
                units = [(p, st) for p in range(N_EXP // 2) for st in range(NST)]
                dcnt = [0]

                def emit_expert(i, j, mid=None):
                    p, st = units[i]
                    e = 2 * p + j
                    cols = slice(st * 512, (st + 1) * 512)
                    W_, WB_ = wgu[p % 2][j], wguB[p % 2][j]
                    CB, CBB = cbs[i % 2], cbsB[i % 2][j]
                    P.op("pe", lambda ee: ee.matmul(pb[4][:], lhsT=sel[:, e, :], rhs=combT[:, cols], start=True, stop=True),
                         r=[selB, combB[st]], w=[pbB[4]])
                    P.op("act", lambda ee: ee.copy(out=CB[:, j, :], in_=pb[4][:]), r=[pbB[4]], w=[CBB])
                    for f in range(2):
                        if f == 1 and mid is not None:
                            mid()
                        for (bank, col0) in ((0 + f, f * 128), (2 + f, 256 + f * 128)):
                            for k in range(8):
                                P.op("pe", lambda ee, k=k, bank=bank, col0=col0: ee.matmul(
                                    pb[bank][:], lhsT=W_[:, k, col0:col0 + 128], rhs=h2T[:, k, cols], start=(k == 0),
                                    stop=(k == 7)), r=[WB_, h2B[st]], w=[pbB[bank]], inc=(k == 7))
                        P.op("act", lambda ee, f=f: ee.activation(out=sgt[:, f, :], in_=pb[f][:], func=AF.Silu),
                             r=[pbB[f]], w=[sgB[f]])
                        P.op("dve", lambda ee, f=f: ee.tensor_tensor(out=tt[:, f, :], in0=pb[2 + f][:], in1=sgt[:, f, :],
                                                                     op=ALU.mult), r=[pbB[2 + f], sgB[f]], w=[ttB[f]])
                        P.op("dve", lambda ee, f=f: ee.tensor_tensor(out=actT[i % 2][:, j, f, :], in0=tt[:, f, :],
                                                                     in1=CB[:, j, :], op=ALU.mult),
                             r=[ttB[f], CBB], w=[actB[i % 2][j][f]])

                def emit_down(i):
                    p, st = units[i]
                    cols = slice(st * 512, (st + 1) * 512)
                    for d in range(8):
                        bank = 5 + dcnt[0] % 3
                        dcnt[0] += 1
                        n = 0
                        for j in range(2):
                            for f in range(2):
                                P.op("pe", lambda ee, d=d, f=f, j=j, bank=bank, n=n: ee.matmul(
                                    pb[bank][:], lhsT=wd[p % 2][j][:, f, d * 128:(d + 1) * 128], rhs=actT[i % 2][:, j, f, :],
                                    start=(n == 0), stop=(n == 3)), r=[wdB[p % 2][j], actB[i % 2][j][f]], w=[pbB[bank]],
                                    inc=(n == 3))
                                n += 1
                        P.op("dve", lambda ee, d=d, bank=bank: ee.scalar_tensor_tensor(
                            out=acc[:, d, cols], in0=pb[bank][:], scalar=g2(d), in1=acc[:, d, cols], op0=ALU.mult, op1=ALU.add),
                            r=[pbB[bank], accB[st], modB], w=[accB[st]])

                load_pair(0)
                pre = (hf == NHALF - 1 and l + 1 < L)
                if pre:
                    wavn = Wd["w_ada"].ap()[l + 1].rearrange("(k p) n -> p k n", p=128)
                    NPIECE = 48
                    every = max(1, (len(units) - 4) // NPIECE)

                    def mod_dma(j):
                        P.dma("pool", wa2[j % 2][:], wavn[:, :, j * 128:(j + 1) * 128], w=[wa2B[j % 2]])

                    def mod_piece(j):
                        W_, WB_ = wa2[j % 2], wa2B[j % 2]
                        for k in range(8):
                            P.op("pe", lambda e, k=k: e.matmul(
                                pb[4][:, 0:1], lhsT=W_[:, k, :], rhs=condTb[:, k:k + 1],
                                start=(k == 0), stop=(k == 7)), r=[WB_, condB], w=[pbB[4]], inc=(k == 7))
                        P.op("act", lambda e: e.copy(out=modN[:, j:j + 1], in_=pb[4][:, 0:1]), r=[pbB[4]], w=[modNB])
                    mod_dma(0)
                    mod_dma(1)
                for i in range(len(units)):
                    p, st = units[i]
                    if pre and i % every == 0 and i // every < NPIECE:
                        j = i // every
                        mod_piece(j)
                        if j + 2 < NPIECE:
                            mod_dma(j + 2)
                    emit_expert(i, 0)

                    def mid(i=i, p=p, st=st):
                        if i >= 1:
                            emit_down(i - 1)
                        if st == 0 and p + 1 < N_EXP // 2:
                            load_pair(p + 1)
                    emit_expert(i, 1, mid=mid)
                emit_down(len(units) - 1)
                if pre:
                    for j in range(min(NPIECE, (len(units) + every - 1) // every), NPIECE):
                        mod_piece(j)
                        if j + 2 < NPIECE:
                            mod_dma(j + 2)
                for st in range(NST):
                    gs = (t0 // 512) + st
                    P.dma("sp", xT_v[:, :, t0 + st * 512:t0 + (st + 1) * 512], acc[:, :, st * 512:(st + 1) * 512],
                          r=[accB[st]], w=[xT_b[gs]])

    ctx["moe_phase"] = moe_phase

    if "YT" in tapset:
        ctx["tapYT"] = tap_out("YT", [L, D, S])
        ctx["tapYTB"] = Buf("tapYT")
        ctx["tapst"] = sb("tapst", [128, 512], F32)
        ctx["tapstB"] = Buf("tapst")
    if not ctx["do_moe"]:
        xt0 = sb("cpx", [128, 8, 32], F32); xt0B = Buf("cp")

    for l in range(L):
        P.dma("sp", stA[0:48, :], Wd["b_ada"].ap()[l].rearrange("(r p) -> r p", p=128), w=[stAB])
        P.dma("sp", stA[48:56, :], Wd["norm1_g"].ap()[l].rearrange("(r p) -> r p", p=128), w=[stAB])
        P.dma("sp", stA[56:64, :], Wd["norm2_g"].ap()[l].rearrange("(r p) -> r p", p=128), w=[stAB])
        P.dma("sp", stA[64:72, :], Wd["ssm_a_re"].ap()[l].rearrange("(cb j) p -> cb (j p)", j=2), w=[stAB])
        P.dma("sp", stA[72:80, :], Wd["ssm_a_im"].ap()[l].rearrange("(cb j) p -> cb (j p)", j=2), w=[stAB])
        transpose_to(vecT[:], vecB, stA[:], stAB, 80)
        P.dma("sp", st_ld[:], Wd["ssm_log_dt"].ap()[l].rearrange("(cb j) -> cb j", j=2), w=[st2B])
        P.op("dve", lambda e: e.tensor_copy(st2[:].rearrange("p (j q) -> p j q", j=2),
                                            st_ld[:, :, None].to_broadcast([8, 2, 64])), r=[st2B], w=[st2B])
        transpose_to(ldtT[:], ldtB, st2[:], st2B, 8)
        P.dma("sp", st3[0:2, :], Wd["ssm_d"].ap()[l].rearrange("(r p) -> r p", p=128), w=[st3B])
        P.dma("sp", st3[2:4, :], Wd["ssm_b_glu"].ap()[l].rearrange("(r p) -> r p", p=128), w=[st3B])
        transpose_to(sdT[:], sdB, st3[:], st3B, 4)
        es1 = ExitStack()
        if l == 0 or not ctx["do_moe"]:
            wav = Wd["w_ada"].ap()[l].rearrange("(k p) n -> p k n", p=128)
            wa = [es1.enter_context(nc.sbuf_tensor("wa%d_%d" % (l, i), [128, 8, 512], BF16)) for i in range(2)]
            waB = [Buf("wa%d" % i) for i in range(2)]
            for j in range(12):
                W_, WB_ = wa[j % 2], waB[j % 2]
                P.dma("pool", W_[:], wav[:, :, j * 512:(j + 1) * 512], w=[WB_])
                for m in range(4):
                    col = 4 * j + m
                    for k in range(8):
                        P.op("pe", lambda e, W_=W_, m=m, k=k, col=col: e.matmul(
                            pb[6][:, col:col + 1], lhsT=W_[:, k, m * 128:(m + 1) * 128], rhs=condTb[:, k:k + 1],
                            start=(k == 0), stop=(k == 7)), r=[WB_, condB], w=[pbB[6]], inc=(k == 7))

            P.op("act", lambda e: e.copy(out=modN[:], in_=pb[6][:, 0:48]), r=[pbB[6]], w=[modNB])
        P.op("dve", lambda e: e.tensor_tensor(out=modT[:], in0=modN[:], in1=vecT[:, C_BADA:C_BADA + 48],
                                              op=ALU.add), r=[modNB, vecB], w=[modB])
        P.op("dve", lambda e: e.scalar_tensor_tensor(out=gm[:, 0:8], in0=modT[:, 8:16], scalar=1.0,
                                                     in1=vecT[:, C_N1:C_N1 + 8], op0=ALU.add, op1=ALU.mult),
             r=[modB, vecB], w=[gmB])
        P.op("dve", lambda e: e.scalar_tensor_tensor(out=gm[:, 8:16], in0=modT[:, 32:40], scalar=1.0,
                                                     in1=vecT[:, C_N2:C_N2 + 8], op0=ALU.add, op1=ALU.mult),
             r=[modB, vecB], w=[gmB])
        sh1 = lambda k: modT[:, k:k + 1]
        g1 = lambda k: modT[:, 16 + k:17 + k]
        sh2 = lambda k: modT[:, 24 + k:25 + k]
        g2 = lambda k: modT[:, 40 + k:41 + k]
        gm1 = lambda k: gm[:, k:k + 1]
        gm2 = lambda k: gm[:, 8 + k:9 + k]

        barrier(P)
        es1.close()
        with ExitStack() as es:
            def sba(name, shape, dt):
                return es.enter_context(nc.sbuf_tensor("A%d_%s" % (l, name), list(shape), dt))
            w_in_sb = sba("w_in", [128, 8, IN_W], BF16); winB = Buf("w_in")
            w_out_sb = sba("w_out", [128, 8, D], BF16); woutB = Buf("w_out")
            wiv = Wd["w_in"].ap()[l].rearrange("(k p) n -> p k n", p=128)
            wov = Wd["w_out"].ap()[l].rearrange("(k p) n -> p k n", p=128)
            winB0, winB1 = Buf("w_in0"), Buf("w_in1")
            for hh, wb_ in ((0, winB0), (1, winB1)):
                for k in range(8):
                    P.dma("pool", w_in_sb[:, k, hh * 1408:(hh + 1) * 1408], wiv[:, k, hh * 1408:(hh + 1) * 1408],
                          w=[wb_])
            winB = [winB0, winB1]
            for k in range(8):
                P.dma("pool", w_out_sb[:, k, :], wov[:, k, :], w=[woutB])
            xt = [sba("xt%d" % i, [128, 8, 512], F32) for i in range(1)]
            xtB = [Buf("xt%d" % i) for i in range(1)]
            sq = [sba("sq%d" % i, [128, 512], BF16) for i in range(2)]; sqB = [Buf("sq%d" % i) for i in range(2)]
            rs = sba("rs", [128, 512], F32); rsB = Buf("rs")
            mt = [sba("mt%d" % i, [128, 512], F32) for i in range(4)]
            mtB = [[Buf("mt%dA" % i), Buf("mt%dB" % i)] for i in range(4)]
            tmp = mt[0:2]; tmpB = mtB[0:2]
            hT = sba("hT", [128, 8, 512], BF16); hTB = Buf("hT")
            qT = sba("qT", [128, 2, 4, 512], BF16); qTB = Buf("qT")
            kT = sba("kT", [128, 4, 1024], BF16); kTB = [Buf("kT%d" % i) for i in range(8)]
            Vr = sba("Vr", [128, 8, 8, 65], BF16); VrB = [Buf("Vr%d" % i) for i in range(8)]
            uT32 = sba("uT32", [128, 2, 512], F32); uTb = sba("uTb", [128, 2, 512], BF16); uTB = Buf("uT")
            YT = sba("YT", [128, 8, 512], BF16)
            YTB = [Buf("YT%d" % i) for i in range(8)]
            expB = sba("expB", [128, 8, 5, 128], BF16); expBB = Buf("expB")
            Pt = [sba("Pt%d" % i, [128, 5, 128], BF16) for i in range(3)]
            PtB = [Buf("Pt%d" % i) for i in range(3)]
            trB = Buf("tr4")
            pb4b = pb[4][:].bitcast(BF16)
            ya = sba("ya", [128, 8, 64], BF16); yaB = Buf("ya")
            rec = sba("rec", [128, 8], F32); recB = Buf("rec")
            tailB = [Buf("tail%d" % i) for i in range(4)]
            cs = [sba("cs%d" % i, [128, 512], F32) for i in range(1)] * 2; csB = [Buf("cs0")] * 2
            rmt = sba("rmt", [128, 2, 256], F32)
            rm = [rmt[:, i % 2, :].rearrange("p (h f) -> p h f", h=8) for i in range(4)]
            rmB = [Buf("rm0"), Buf("rm1")] * 2
            qkrot = sba("qkrot", [128, 8, 64], BF16); qkrotB = Buf("qkrot")
            qTr = sba("qTr", [128, 2, 2, 128], BF16); kTr = sba("kTr", [128, 2, 128], BF16); qkTB = Buf("qkT")
            vr = sba("vr", [128, 256], BF16); vrB = Buf("vr")
            sg = sba("sg", [128, 256], F32); sgB = Buf("sg")
            Am = sba("Am", [128, 4, 128], BF16); AmB = Buf("Am")
            Sst = sba("Sst", [128, 2, 64], F32); Sbf = sba("Sbf", [128, 2, 64], BF16); SstB = Buf("Sst"); SbfB = Buf("Sbf")
            ysb = sba("ysb", [128, 256], F32); ysbB = Buf("ysb")
            ysq = sba("ysq", [128, 256], F32); ysqB = Buf("ysq")
            gst = sba("gst", [128, 16], F32); gstB = Buf("gst")
            gn_bc = sba("gn_bc", [128, 256], F32); gnB = Buf("gn_bc")
            yr = sba("yr", [128, 256], BF16); yrB = Buf("yr")

            P.op("dve", lambda e: e.memset(Vr[:].rearrange("p a b c -> p (a b c)"), 1.0), w=VrB)
            P.op("dve", lambda e: e.memset(Sst[:].rearrange("p a b -> p (a b)"), 0.0), w=[SstB])
            P.op("dve", lambda e: e.memset(Sbf[:].rearrange("p a b -> p (a b)"), 0.0), w=[SbfB])
            P.op("dve", lambda e: e.memset(YT[:].rearrange("p a b -> p (a b)"), 0.0), w=YTB)
            P.op("dve", lambda e: e.memset(qT[:].rearrange("p a b c -> p (a b c)"), 0.0), w=[qTB])
            P.op("dve", lambda e: e.memset(qTr[:].rearrange("p a b c -> p (a b c)"), 0.0), w=[qkTB])
            P.dma("sp", gn_bc[:], Wd["ret_gn_g"].ap()[l:l + 1, :].to_broadcast([128, 256]), w=[gnB])
            es2 = ExitStack()
            Hk = es2.enter_context(nc.sbuf_tensor("Hk%d" % l, [128, 5, 128], F32)); HkB = Buf("Hk")
            Fsb = es2.enter_context(nc.sbuf_tensor("Fsb%d" % l, [8, 768], F32)); FsbB = Buf("Fsb")
            P.dma("sp", Fsb[:, 511:703], Wd["attn_rel_bias"].ap()[l], w=[FsbB])
            P.op("dve", lambda e: e.tensor_copy(Fsb[:, 0:511], Fsb[:, 511:512].to_broadcast([8, 511])),
                 r=[FsbB], w=[FsbB])
            P.op("dve", lambda e: e.tensor_copy(Fsb[:, 703:768], Fsb[:, 702:703].to_broadcast([8, 65])),
                 r=[FsbB], w=[FsbB])
            P.dma("sp", Fd.ap()[l], Fsb[:], r=[FsbB], w=[FdB])
            for h in range(8):
                src = bass.AP(Fd, (l * 8 + h) * 768, [[1, 128], [128, 5], [1, 128]])
                P.dma("sp", Hk[:], src, r=[FdB], w=[HkB])
                P.op("act", lambda e, h=h: e.activation(out=expB[:, h, :, :], in_=_rev_last(Hk[:], 128), func=AF.Exp),
                     r=[HkB], w=[expBB])
            P.op("dve", lambda e: e.tensor_tensor(
                out=expB[:], in0=expB[:],
                in1=amask_sb[:].rearrange("p (b q) -> p b q", b=5)[:, None, :, :].to_broadcast([128, 8, 5, 128]),
                op=ALU.mult), r=[expBB, B_const], w=[expBB])

            barrier(P)
            es2.close()
            if do_s5:
                ctx["s5_setup"](l, es, dict(vecT=vecT, vecB=vecB, ldtT=ldtT, ldtB=ldtB, sdT=sdT, sdB=sdB, uT32=uT32,
                                            uTb=uTb, uTB=uTB, YT=YT, YTB=YTB, mt=mt, mtB=mtB))
            unit = [0]
            sbank = [0]

            def evac(i, out_ap, in_ap, r, w):
                if i % 2 == 0:
                    P.op("act", lambda e: e.copy(out=out_ap, in_=in_ap), r=r, w=w)
                else:
                    P.op("dve", lambda e: e.tensor_copy(out_ap, in_ap), r=r, w=w)

            mmc = [0]

            def mmbank():
                mmc[0] += 1
                return mmc[0] % 2

            for s in range(NT):
                X, XB = xt[0], xtB[0]
                P.dma("sp", X[:], xT_v[:, :, s * 512:(s + 1) * 512], r=[xT_b[s]], w=[XB])
                rms_stats(P, pb, pbB, mmbank(), ones_bf, B_const, X, XB, sq, sqB, rs, rsB)
                for k in range(8):
                    T_, TB_ = tmp[k % 2], tmpB[k % 2]
                    P.op("dve", lambda e, k=k, T_=T_: e.scalar_tensor_tensor(
                        out=T_[:], in0=X[:, k, :], scalar=gm1(k), in1=rs[:], op0=ALU.mult, op1=ALU.mult),
                        r=[XB, rsB, gmB], w=[TB_])
                    P.op("act", lambda e, k=k, T_=T_: e.activation(out=hT[:, k, :], in_=T_[:], func=AF.Identity,
                                                                    bias=sh1(k), scale=1.0),
                         r=[TB_, modB], w=[hTB])
                ring0 = (4 * s) % 8
                fm = [("q", c, c * 128) for c in range(4)] + [("k", c, 512 + c * 128) for c in range(4)] + \
                     [("u", c, 1536 + c * 128) for c in range(2)]
                for i, (kind, c, col) in enumerate(fm):
                    bk = mmbank()
                    for kk in range(8):
                        P.op("pe", lambda e, kk=kk, col=col, bk=bk: e.matmul(
                            pb[bk][:], lhsT=w_in_sb[:, kk, col:col + 128], rhs=hT[:, kk, :], start=(kk == 0),
                            stop=(kk == 7)), r=[(winB0 if col < 1280 else winB1), hTB], w=[pbB[bk]], inc=(kk == 7))
                    if kind == "q":
                        evac(0, qT[0:64, 0, c, :], pb[bk][0:64, :], [pbB[bk]], [qTB])
                        evac(1, qT[64:128, 1, c, :], pb[bk][64:128, :], [pbB[bk]], [qTB])
                    elif kind == "k":
                        evac(i, kT[:, c, ring0 * 128:ring0 * 128 + 512], pb[bk][:], [pbB[bk]], kTB[ring0:ring0 + 4])
                    else:
                        P.op("act", lambda e, c=c, bk=bk: e.copy(out=uT32[:, c, :], in_=pb[bk][:]), r=[pbB[bk]], w=[uTB])
                        P.op("dve", lambda e, c=c, bk=bk: e.tensor_copy(uTb[:, c, :], pb[bk][:]), r=[pbB[bk]], w=[uTB])
                for t in range(4):
                    gb = 4 * s + t
                    slot = gb % 8
                    tok = slice(t * 128, (t + 1) * 128)
                    bk = mmbank()
                    for kk in range(8):
                        P.op("pe", lambda e, kk=kk, bk=bk, tok=tok: e.matmul(
                            pb[bk][:], lhsT=hT[:, kk, tok], rhs=w_in_sb[:, kk, 1024:1536], start=(kk == 0),
                            stop=(kk == 7)), r=[winB, hTB], w=[pbB[bk]], inc=(kk == 7))
                    P.op("act", lambda e, bk=bk, slot=slot: e.copy(
                        out=Vr[:, slot, :, 0:64], in_=pb[bk][:].rearrange("p (h d) -> p h d", h=8)),
                        r=[pbB[bk]], w=[VrB[slot]])

                def attn_gen(P):
                    for t in range(4):
                        gb = 4 * s + t
                        tok = slice(t * 128, (t + 1) * 128)
                        nbk = min(5, gb + 1)
                        b0 = 5 - nbk
                        info = {}

                        def stage_a(h, gb=gb, tok=tok, nbk=nbk, b0=b0, info=info):
                            c = h // 2
                            u = unit[0]; unit[0] += 1
                            PT, PTB = Pt[u % 3], PtB[u % 3]
                            info[h] = (PT, PTB)
                            pieces = ([(b0, 4)] if nbk > 1 else []) + [(4, 5)]
                            for (ba_, bb_) in pieces:
                                bank = 2 + sbank[0] % 2
                                sbank[0] += 1
                                for b in range(ba_, bb_):
                                    kslot = (gb - 4 + b) % 8
                                    P.op("pe", lambda e, b=b, kslot=kslot, c=c, h=h, bank=bank, ba_=ba_: e.matmul(
                                        pb[bank][:, (b - ba_) * 128:(b - ba_ + 1) * 128],
                                        lhsT=kT[:, c, kslot * 128:(kslot + 1) * 128],
                                        rhs=qT[:, h % 2, c, tok], start=True, stop=True),
                                        r=[kTB[kslot], qTB], w=[pbB[bank]], inc=(b == bb_ - 1))
                                P.op("act", lambda e, bank=bank, PT=PT, ba_=ba_, bb_=bb_: e.activation(
                                    out=PT[:, ba_:bb_, :].rearrange("p a b -> p (a b)"),
                                    in_=pb[bank][:, 0:(bb_ - ba_) * 128], func=AF.Exp, scale=0.125),
                                    r=[pbB[bank]], w=[PTB])
                            P.op("dve", lambda e, PT=PT, h=h: e.tensor_tensor(
                                out=PT[:, b0:5, :], in0=PT[:, b0:5, :], in1=expB[:, h, b0:5, :], op=ALU.mult),
                                r=[PTB, expBB], w=[PTB])

                        def stage_b(h, gb=gb, b0=b0, info=info):
                            PT, PTB = info[h]
                            for b in range(b0, 5):
                                kslot = (gb - 4 + b) % 8
                                P.op("pe", lambda e, b=b, kslot=kslot, h=h, PT=PT: e.matmul(
                                    pb[5][:, (h % 4) * 65:(h % 4) * 65 + 65], lhsT=PT[:, b, :],
                                    rhs=Vr[:, kslot, h, :], start=(b == b0), stop=(b == 4)),
                                    r=[PTB, VrB[kslot]], w=[pbB[5]], inc=(b == 4))
                            if h % 4 == 3:
                                hh = h // 4
                                pvv = pb[5][:, 0:260].rearrange("p (h d) -> p h d", h=4)
                                P.op("dve", lambda e, pvv=pvv, hh=hh: e.reciprocal(
                                    rec[:, hh * 4:hh * 4 + 4], pvv[:, :, 64]), r=[pbB[5]], w=[recB])
                                P.op("dve", lambda e, pvv=pvv, hh=hh: e.tensor_tensor(
                                    out=ya[:, hh * 4:hh * 4 + 4, :], in0=pvv[:, :, 0:64],
                                    in1=rec[:, hh * 4:hh * 4 + 4, None].to_broadcast([128, 4, 64]), op=ALU.mult),
                                    r=[pbB[5], recB], w=[yaB])

                        stage_a(0)
                        yield
                        for h in range(1, 8):
                            stage_a(h)
                            stage_b(h - 1)
                            yield
                        stage_b(7)
                        P.begin()
                        for c in range(4):
                            P.op("pe", lambda e, c=c: e.transpose(
                                pb7b[:, c * 128:(c + 1) * 128],
                                ya[:, 2 * c:2 * c + 2, :].rearrange("p a b -> p (a b)"), identb[:]),
                                r=[yaB, B_const], w=[pbB[7]], inc=(c == 3))
                        P.op("act", lambda e, tok=tok: e.copy(out=YT[:, 0:4, tok],
                                                              in_=pb7b[:, 0:512].rearrange("p (c q) -> p c q", c=4)),
                             r=[pbB[7]], w=YTB[0:4])
                        P.end()
                        yield

                def ret_gen(P):
                    for t in range(4):
                        gb = 4 * s + t
                        slot = gb % 8
                        tok = slice(t * 128, (t + 1) * 128)
                        if do_ret:
                            bq = 4
                            for kk in range(8):
                                P.op("pe", lambda e, kk=kk, bq=bq: e.matmul(
                                    pb[bq][:], lhsT=hT[:, kk, tok], rhs=w_in_sb[:, kk, 1792:2304], start=(kk == 0),
                                    stop=(kk == 7)), r=[winB, hTB], w=[pbB[bq]], inc=(kk == 7))
                            CS, CSB = cs[gb % 2], csB[gb % 2]
                            P.dma("sp", CS[:], Cd["retcs"].ap()[gb * 128:(gb + 1) * 128, :], w=[CSB])
                            srcv = pb[bq][:].rearrange("p (h two f) -> p h two f", h=8, two=2)
                            x1 = srcv[:, :, 0, :]
                            x2 = srcv[:, :, 1, :]
                            cosv = CS[:, 0:256].rearrange("p (h f) -> p h f", h=8)
                            sinv = CS[:, 256:512].rearrange("p (h f) -> p h f", h=8)
                            for (lo_, a1, b1, a2, b2, op_) in ((0, x1, cosv, x2, sinv, ALU.subtract), (32, x1, sinv, x2, cosv, ALU.add)):
                                P.op("dve", lambda e: e.tensor_tensor(out=rm[0], in0=a1, in1=b1, op=ALU.mult),
                                     r=[pbB[bq], CSB], w=[rmB[0]])
                                P.op("dve", lambda e: e.tensor_tensor(out=rm[1], in0=a2, in1=b2, op=ALU.mult),
                                     r=[pbB[bq], CSB], w=[rmB[1]])
                                P.op("dve", lambda e: e.tensor_tensor(out=qkrot[:, :, lo_:lo_ + 32], in0=rm[0], in1=rm[1], op=op_),
                                     r=[rmB[0], rmB[1]], w=[qkrotB])
                            yield
                            if RS >= 2:
                                bv = 4
                                for kk in range(8):
                                    P.op("pe", lambda e, kk=kk, bv=bv: e.matmul(
                                        pb[bv][:], lhsT=hT[:, kk, tok], rhs=w_in_sb[:, kk, 2304:2816], start=(kk == 0),
                                        stop=(kk == 7)), r=[winB, hTB], w=[pbB[bv]], inc=(kk == 7))
                                P.op("act", lambda e, bv=bv: e.copy(out=vr[:], in_=pb[bv][:, 0:256]), r=[pbB[bv]], w=[vrB])
                                P.op("act", lambda e, bv=bv: e.activation(out=sg[:], in_=pb[bv][:, 256:512], func=AF.Silu),
                                 r=[pbB[bv]], w=[sgB])
                        if do_ret:
                            yield
                            if RS >= 3:
                                P.begin()
                                for c in range(4):
                                    P.op("pe", lambda e, c=c: e.transpose(
                                        pb7b[:, 512 + c * 128:512 + (c + 1) * 128],
                                        qkrot[:, 2 * c:2 * c + 2, :].rearrange("p a b -> p (a b)"), identb[:]),
                                        r=[qkrotB, B_const], w=[pbB[7]], inc=(c == 3))
                                P.op("act", lambda e: e.copy(out=qTr[0:64, 0, :, :].rearrange("p a b -> p (a b)"),
                                                             in_=pb7b[0:64, 512:768]), r=[pbB[7]], w=[qkTB])
                                P.op("act", lambda e: e.copy(out=qTr[64:128, 1, :, :].rearrange("p a b -> p (a b)"),
                                                             in_=pb7b[64:128, 512:768]), r=[pbB[7]], w=[qkTB])
                                P.op("dve", lambda e: e.tensor_copy(kTr[:].rearrange("p a b -> p (a b)"), pb7b[:, 768:1024]),
                                     r=[pbB[7]], w=[qkTB])
                                P.end()
                            yield
                            if RS >= 4:
                                ba = 4
                                for h in range(4):
                                    c, pbase = h // 2, 64 * (h % 2)
                                    P.op("pe", lambda e, h=h, c=c, pbase=pbase, ba=ba: e.matmul(
                                        pb[ba][:, h * 128:(h + 1) * 128], lhsT=kTr[:, c, :],
                                        rhs=qTr[:, h % 2, c, :], start=True, stop=True),
                                        r=[qkTB], w=[pbB[ba]], inc=(h == 3))
                                if RS >= 4.5: P.op("dve", lambda e, ba=ba: e.tensor_tensor(
                                    out=Am[:], in0=pb[ba][:].rearrange("p (h i) -> p h i", h=4),
                                    in1=tri_sb[:, None, :].to_broadcast([128, 4, 128]), op=ALU.mult),
                                    r=[pbB[ba], B_const], w=[AmB])
                            yield
                            if RS >= 5:
                                by = 4
                                for h in range(4):
                                    c, pbase = h // 2, 64 * (h % 2)
                                    P.op("pe", lambda e, h=h, by=by: e.matmul(
                                        pb[by][:, h * 64:(h + 1) * 64], lhsT=Am[:, h, :], rhs=vr[:, h * 64:(h + 1) * 64],
                                        start=True, stop=False), r=[AmB, vrB], w=[pbB[by]], inc=False)
                                    P.op("pe", lambda e, h=h, c=c, pbase=pbase, by=by: e.matmul(
                                        pb[by][:, h * 64:(h + 1) * 64], lhsT=qTr[:, h % 2, c, :],
                                        rhs=Sbf[:, c, :], start=False, stop=True),
                                        r=[qkTB, SbfB], w=[pbB[by]], inc=(h == 3))
                                for c in range(2):
                                    P.op("pe", lambda e, c=c, by=by: e.matmul(
                                        pb[by][:, 256 + c * 128:256 + (c + 1) * 128],
                                        lhsT=qkrot[:, 4 + 2 * c:6 + 2 * c, :].rearrange("p a b -> p (a b)"),
                                        rhs=vr[:, c * 128:(c + 1) * 128], start=True, stop=True),
                                        r=[qkrotB, vrB], w=[pbB[by]], inc=(c == 1))
                            if RS >= 5.2:
                                P.op("act", lambda e, by=by: e.copy(out=ysb[:], in_=pb[by][:, 0:256]), r=[pbB[by]], w=[ysbB])
                                for hh in range(2):
                                    ps_ = slice(64 * hh, 64 * hh + 64)
                                    if hh == 0:
                                        P.op("act", lambda e, by=by: e.copy(out=ysq[:], in_=pb[by][:, 256:512]),
                                             r=[pbB[by]], w=[ysqB])
                                    kvv = ysq[ps_, :].rearrange("p (c x) -> p c x", c=2)[:, :, 64 * hh:64 * hh + 64]
                                    if RS >= 5.4 + 0.2 * hh: P.op("dve", lambda e, ps_=ps_, kvv=kvv: e.tensor_tensor(
                                        out=Sst[ps_, :, :], in0=kvv, in1=Sst[ps_, :, :], op=ALU.add),
                                        r=[ysqB, SstB], w=[SstB])
                                    if RS >= 5.5 + 0.2 * hh: P.op("dve", lambda e, ps_=ps_: e.tensor_tensor(
                                        out=Sst[ps_, :, :], in0=Sst[ps_, :, :],
                                        in1=retG_sb[ps_, :].rearrange("p (c x) -> p c x", c=2), op=ALU.mult),
                                        r=[SstB, B_const], w=[SstB])
                                P.op("act", lambda e: e.copy(out=Sbf[:].rearrange("p a b -> p (a b)"),
                                                             in_=Sst[:].rearrange("p a b -> p (a b)")), r=[SstB], w=[SbfB])
                            yield
                            if RS >= 7:
                                y3 = ysb[:].rearrange("p (h x) -> p h x", h=4)
                                P.op("dve", lambda e: e.tensor_reduce(out=gst[:, 0:4], in_=y3, axis=AX.X, op=ALU.add),
                                     r=[ysbB], w=[gstB])
                                P.op("act", lambda e: e.activation(out=ysq[:], in_=ysb[:], func=AF.Square), r=[ysbB], w=[ysqB])
                                P.op("dve", lambda e: e.tensor_reduce(out=gst[:, 4:8], in_=ysq[:].rearrange("p (h x) -> p h x", h=4),
                                                                      axis=AX.X, op=ALU.add), r=[ysqB], w=[gstB])
                                P.op("dve", lambda e: e.tensor_scalar(out=gst[:, 0:4], in0=gst[:, 0:4], scalar1=1.0 / 64, scalar2=None,
                                                                      op0=ALU.mult), r=[gstB], w=[gstB])
                                P.op("dve", lambda e: e.tensor_tensor(out=gst[:, 8:12], in0=gst[:, 0:4], in1=gst[:, 0:4],
                                                                      op=ALU.mult), r=[gstB], w=[gstB])
                                P.op("dve", lambda e: e.scalar_tensor_tensor(out=gst[:, 12:16], in0=gst[:, 4:8], scalar=1.0 / 64,
                                                                             in1=gst[:, 8:12], op0=ALU.mult, op1=ALU.subtract),
                                     r=[gstB], w=[gstB])
                                P.op("act", lambda e: e.activation(out=gst[:, 12:16], in_=gst[:, 12:16], func=AF.Sqrt, bias=EPS,
                                                                   scale=1.0), r=[gstB], w=[gstB])
                                P.op("dve", lambda e: e.reciprocal(gst[:, 12:16], gst[:, 12:16]), r=[gstB], w=[gstB])
                                P.op("dve", lambda e: e.tensor_tensor(out=y3, in0=y3,
                                                                      in1=gst[:, 0:4, None].to_broadcast([128, 4, 64]),
                                                                      op=ALU.subtract), r=[ysbB, gstB], w=[ysbB])
                                P.op("dve", lambda e: e.tensor_tensor(out=y3, in0=y3,
                                                                      in1=gst[:, 12:16, None].to_broadcast([128, 4, 64]),
                                                                      op=ALU.mult), r=[ysbB, gstB], w=[ysbB])
                                P.op("dve", lambda e: e.tensor_tensor(out=ysb[:], in0=ysb[:], in1=gn_bc[:], op=ALU.mult),
                                     r=[ysbB, gnB], w=[ysbB])
                                P.op("dve", lambda e: e.tensor_tensor(out=yr[:], in0=ysb[:], in1=sg[:], op=ALU.mult),
                                     r=[ysbB, sgB], w=[yrB])
                            yield
                            if RS >= 8:
                                P.begin()
                                for c in range(2):
                                    P.op("pe", lambda e, c=c: e.transpose(
                                        pb7b[:, c * 128:(c + 1) * 128], yr[:, c * 128:(c + 1) * 128], identb[:]),
                                        r=[yrB, B_const], w=[pbB[7]], inc=(c == 1))
                                P.op("act", lambda e: e.copy(out=YT[:, 6:8, tok],
                                                             in_=pb7b[:, 0:256].rearrange("p (c q) -> p c q", c=2)),
                                     r=[pbB[7]], w=YTB[6:8])
                                P.end()

                        yield
                recs = []
                for mk in ((attn_gen if do_attn else None), ((lambda PP: ctx["s5_gen"](s, PP)) if do_s5 else None),
                           (ret_gen if do_ret else None)):
                    if mk is not None:
                        d = Deferred()
                        for _ in mk(d):
                            pass
                        recs.append(d)
                run_interleaved(P, recs)
                if "YT" in tapset:
                    for kk in range(8):
                        P.op("dve", lambda e, kk=kk: e.tensor_copy(ctx["tapst"][:], YT[:, kk, :]), r=[YTB[kk]],
                             w=[ctx["tapstB"]])
                        P.dma("sp", ctx["tapYT"].ap()[l][kk * 128:(kk + 1) * 128, s * 512:(s + 1) * 512],
                              ctx["tapst"][:], r=[ctx["tapstB"]], w=[ctx["tapYTB"]])
                for d in range(8):
                    bk = mmbank()
                    for kk in range(8):
                        P.op("pe", lambda e, kk=kk, d=d, bk=bk: e.matmul(
                            pb[bk][:], lhsT=w_out_sb[:, kk, d * 128:(d + 1) * 128], rhs=YT[:, kk, :], start=(kk == 0),
                            stop=(kk == 7)), r=[woutB, YTB[kk]], w=[pbB[bk]], inc=(kk == 7))
                    P.op("dve", lambda e, d=d, bk=bk: e.scalar_tensor_tensor(
                        out=X[:, d, :], in0=pb[bk][:], scalar=g1(d), in1=X[:, d, :], op0=ALU.mult, op1=ALU.add),
                        r=[pbB[bk], XB, modB], w=[XB])
                P.dma("sp", x1T_v[:, :, s * 512:(s + 1) * 512], X[:], r=[XB], w=[x1T_b[s]])
        barrier(P)
        if ctx["do_moe"]:
            ctx["moe_phase"](l, dict(sh2=sh2, g2=g2, gm2=gm2, modB=modB, gmB=gmB))
        else:
            for s in range(16 * NT):
                P.dma("sp", xt0[:], x1T_v[:, :, s * 32:(s + 1) * 32], r=[x1T_b[s // 16]], w=[xt0B])
                P.dma("sp", xT_v[:, :, s * 32:(s + 1) * 32], xt0[:], r=[xt0B], w=[xT_b[s // 16]])
        barrier(P)
    return xT_v, xT_b


_CACHE = {}


def _get_program(S, L):
    key = (S, L)
    if key not in _CACHE:
        nc = bass.Bass("TRN2", target_bir_lowering=False)
        P, _ = build_program(nc, S, L)
        _CACHE[key] = nc
    return _CACHE[key]


def kernel(**inputs):
    x = np.asarray(inputs["x"], dtype=np.float32)
    B, S, _ = x.shape
    L = int(np.asarray(inputs["w_in"]).shape[0])
    nc = _get_program(S, L)
    cst = host_constants(S)
    shared = {k: np.ascontiguousarray(np.asarray(inputs[k], dtype=np.float32)) for k in WEIGHT_SHAPES(L)}
    shared.update(cst)
    c = np.asarray(inputs["c"], dtype=np.float32)
    in_maps = []
    for b in range(B):
        m = dict(shared)
        m["x"] = np.ascontiguousarray(x[b])
        m["c"] = np.ascontiguousarray(c[b].reshape(8, 128))
        in_maps.append(m)
    res = run_bass_kernel_spmd(nc, in_maps, core_ids=list(range(B)))
    return np.stack([np.asarray(r["y"], dtype=np.float32) for r in res.results], axis=0)
```

```python
import math
import numpy as np
import concourse.bass as bass
import concourse.mybir as mybir
from concourse.bass_utils import run_bass_kernel_spmd

F32 = mybir.dt.float32
BF16 = mybir.dt.bfloat16
I32 = mybir.dt.int32
AF = mybir.ActivationFunctionType
ALU = mybir.AluOpType
AX = mybir.AxisListType

D = 1024
NCH = 8
IN_W = 2816
EPS = 1e-6
N_EXP = 32
FF = 256


class Buf:
    __slots__ = ("w", "rs", "name", "excl")

    def __init__(self, name="", excl=False):
        self.w = None
        self.rs = {}
        self.name = name
        self.excl = excl


class Prog:
    NRING = 8

    def __init__(self, nc):
        self.nc = nc
        self.E = {"pe": nc.tensor, "act": nc.scalar, "dve": nc.vector, "pool": nc.gpsimd, "sp": nc.sync}
        self.sem = {}
        self.cnt = {}
        for e in ("pe", "act", "dve", "pool"):
            self.sem[e] = nc.alloc_semaphore("s_" + e)
            self.cnt[e] = 0
        for q in ("sp", "act", "pool"):
            for i in range(self.NRING):
                k = "d_%s%d" % (q, i)
                self.sem[k] = nc.alloc_semaphore(k)
                self.cnt[k] = 0
        self.dma_i = {"sp": 0, "act": 0, "pool": 0}
        self.seen = {e: {} for e in ("pe", "act", "dve", "pool", "sp")}
        self.n_ins = 0

    def _deps(self, r, w, eng=None):
        deps = {}
        for b in r:
            if b.w is not None and deps.get(b.w[0], 0) < b.w[1]:
                deps[b.w[0]] = b.w[1]
            if b.excl:
                for e, c in b.rs.items():
                    if e != eng and deps.get(e, 0) < c:
                        deps[e] = c
        for b in w:
            if b.w is not None and deps.get(b.w[0], 0) < b.w[1]:
                deps[b.w[0]] = b.w[1]
            for e, c in b.rs.items():
                if deps.get(e, 0) < c:
                    deps[e] = c
        return deps

    def _wait(self, eng, deps):
        for e2, c in deps.items():
            if c <= 0:
                continue
            if e2 == eng and eng == "pe":
                continue
            if self.seen[eng].get(e2, 0) >= c:
                continue
            self.E[eng].wait_ge(self.sem[e2], c)
            self.seen[eng][e2] = c

    @staticmethod
    def _flat(bs):
        out = []
        for b in bs:
            if isinstance(b, (list, tuple)):
                out.extend(Prog._flat(b))
            else:
                out.append(b)
        return out

    def op(self, eng, fn, r=(), w=(), inc=True):
        r = self._flat(r)
        w = self._flat(w)
        self._wait(eng, self._deps(r, w, eng))
        ins = fn(self.E[eng])
        self.n_ins += 1
        if inc:
            ins.then_inc(self.sem[eng], 1)
            self.cnt[eng] += 1
            c = self.cnt[eng]
        else:
            c = self.cnt[eng] + 1
        for b in w:
            b.w = (eng, c)
            b.rs = {}
        for b in r:
            if b.rs.get(eng, 0) < c:
                b.rs[eng] = c
        return ins

    def dma(self, q, out, in_, r=(), w=(), **kw):
        i = self.dma_i[q]
        self.dma_i[q] += 1
        k = "d_%s%d" % (q, i % self.NRING)
        r = self._flat(r)
        w = self._flat(w)
        deps = self._deps(r, w)
        if self.cnt[k] > 0 and deps.get(k, 0) < self.cnt[k]:
            deps[k] = self.cnt[k]
        self._wait(q, deps)
        ins = self.E[q].dma_start(out=out, in_=in_, **kw)
        self.n_ins += 1
        ins.then_inc(self.sem[k], 16)
        self.cnt[k] += 16
        c = self.cnt[k]
        for b in w:
            b.w = (k, c)
            b.rs = {}
        for b in r:
            if b.rs.get(k, 0) < c:
                b.rs[k] = c
        return ins

    def finish(self, bufs):
        deps = {}
        for b in bufs:
            if b.w is not None and deps.get(b.w[0], 0) < b.w[1]:
                deps[b.w[0]] = b.w[1]
        for k, c in self.cnt.items():
            if c > 0 and deps.get(k, 0) < c:
                deps[k] = c
        self._wait("sp", deps)


class _EngProxy:
    def __init__(self):
        self.call = None

    def __getattr__(self, name):
        def f(*a, **k):
            self.call = (name, a, k)
            return self
        return f


class Deferred:
    def __init__(self):
        self.items = []
        self.grp = None

    def _add(self, it):
        (self.grp if self.grp is not None else self.items).append(it)

    def op(self, eng, fn, r=(), w=(), inc=True):
        px = _EngProxy()
        fn(px)
        name, a, k = px.call
        self._add(("op", (eng, (lambda e, name=name, a=a, k=k: getattr(e, name)(*a, **k))),
                   dict(r=list(r), w=list(w), inc=inc)))

    def dma(self, *a, **k):
        self._add(("dma", a, k))

    def begin(self):
        self.grp = []

    def end(self):
        g, self.grp = self.grp, None
        self.items.append(("group", g, None))


def _emit_item(P, it):
    if it[0] == "group":
        for sub in it[1]:
            _emit_item(P, sub)
    else:
        getattr(P, it[0])(*it[1], **it[2])


def run_interleaved(P, ds):
    ds = [d for d in ds if d.items]
    pos = [0] * len(ds)
    while True:
        best, bf = None, None
        for i, d in enumerate(ds):
            if pos[i] < len(d.items):
                f = pos[i] / float(len(d.items))
                if bf is None or f < bf:
                    best, bf = i, f
        if best is None:
            break
        _emit_item(P, ds[best].items[pos[best]])
        pos[best] += 1


def _rev_last(ap, n):
    pat = [list(p) for p in ap.ap]
    st = pat[-1][0]
    pat[-1] = [-st, n]
    return bass.AP(ap.tensor, ap.offset + st * (n - 1), pat)


def host_constants(S):
    cst = {}
    cst["ident"] = np.eye(128, dtype=np.float32)
    j = np.arange(128)[:, None]
    i = np.arange(128)[None, :]
    cst["tri"] = (j <= i).astype(np.float32)
    h = np.arange(4, dtype=np.float64)
    gam = 1.0 - np.exp2(-5.0 - h)
    G = np.zeros((2, 64, 2, 64))
    for c_ in range(2):
        for hh_ in range(2):
            G[hh_, :, c_, :] = gam[2 * c_ + hh_] ** 128
    cst["retG"] = G.reshape(128, 128).astype(np.float32)
    pos = np.arange(S, dtype=np.float64)
    half = 32
    inv_freq = (10000.0 ** (-np.arange(half, dtype=np.float32) / half)).astype(np.float32)
    ang = (pos.astype(np.float32)[:, None] * inv_freq[None, :]).astype(np.float32).astype(np.float64)
    cosv = np.cos(ang)
    sinv = np.sin(ang)
    il = (np.arange(S) % 128).astype(np.float64)
    facq = gam[None, :] ** (il[:, None] + 1.0)
    fack = gam[None, :] ** (-(il[:, None] + 1.0)) / 8.0
    fac = np.concatenate([facq, fack], axis=1)
    tab = np.zeros((S, 2, 8, 32), dtype=np.float64)
    tab[:, 0] = cosv[:, None, :] * fac[:, :, None]
    tab[:, 1] = sinv[:, None, :] * fac[:, :, None]
    cst["retcs"] = tab.reshape(S, 512).astype(np.float32)
    am = np.ones((128, 5, 128), dtype=np.float32)
    kk = np.arange(128)[:, None]
    qi = np.arange(128)[None, :]
    am[:, 4, :] = 1.0 - ((kk >= 64) & (qi < 64)).astype(np.float32)
    am[:, 0, :] = 1.0 - ((kk < 64) & (qi >= 64)).astype(np.float32)
    cst["amask"] = am.reshape(128, 640)
    return cst


CONST_SHAPES = lambda S: {"ident": [128, 128], "tri": [128, 128], "retG": [128, 128],
                          "retcs": [S, 512], "amask": [128, 640]}

WEIGHT_SHAPES = lambda L: {
    "norm1_g": [L, D], "norm2_g": [L, D], "w_ada": [L, D, 6 * D], "b_ada": [L, 6 * D],
    "w_in": [L, D, IN_W], "attn_rel_bias": [L, 8, 192],
    "ssm_a_re": [L, 16, 64], "ssm_a_im": [L, 16, 64], "ssm_log_dt": [L, 16],
    "ssm_b_re": [L, 16, 64, 16], "ssm_b_im": [L, 16, 64, 16],
    "ssm_c_re": [L, 16, 16, 64], "ssm_c_im": [L, 16, 16, 64],
    "ssm_d": [L, 256], "ssm_w_glu": [L, 256, 256], "ssm_b_glu": [L, 256],
    "ret_gn_g": [L, 256], "w_out": [L, D, D],
    "moe_w_group": [L, D, 4], "moe_b_group": [L, 4], "moe_w_expert": [L, D, 32], "moe_b_expert": [L, 32],
    "moe_w_gate": [L, 32, D, FF], "moe_w_up": [L, 32, D, FF], "moe_w_down": [L, 32, FF, D],
    "final_g": [D],
}


def build_program(nc, S, L, taps=(), do_mixer=True, do_moe=True, run_layers=True, **extra):
    P = Prog(nc)
    NT = S // 512
    NBLK = S // 128
    tapset = set(taps)

    def din(name, shape, dt=F32):
        return nc.dram_tensor(name, list(shape), dt, kind="ExternalInput")

    x_d = din("x", [S, D])
    c_d = din("c", [8, 128])
    Wd = {k: din(k, shp) for k, shp in WEIGHT_SHAPES(L).items()}
    Cd = {k: din(k, shp) for k, shp in CONST_SHAPES(S).items()}
    y_d = nc.dram_tensor("y", [S, D], F32, kind="ExternalOutput")
    tap_d = {}

    def tap_out(name, shape):
        tap_d[name] = nc.dram_tensor("tap_" + name, list(shape), F32, kind="ExternalOutput")
        return tap_d[name]

    xT_d = nc.dram_tensor("xT_scr", [D, S], F32, kind="Internal")
    x1T_d = nc.dram_tensor("x1T_scr", [D, S], F32, kind="Internal")
    xT_b = [Buf("xT%d" % i) for i in range(NT)]
    x1T_b = [Buf("x1T%d" % i) for i in range(NT)]
    xT_v = xT_d.ap().rearrange("(c p) t -> p c t", p=128)
    x1T_v = x1T_d.ap().rearrange("(c p) t -> p c t", p=128)
    y_b = Buf("y")

    def sb(name, shape, dt):
        return nc.alloc_sbuf_tensor("sb_" + name, list(shape), dt)

    pb = [nc.alloc_psum_tensor("pb%d" % i, [128, 512], F32) for i in range(8)]
    pbB = [Buf("pb%d" % i, excl=True) for i in range(8)]

    ident = sb("ident", [128, 128], F32)
    identb = sb("identb", [128, 128], BF16)
    ones_bf = sb("ones_bf", [128, 128], BF16)
    B_const = Buf("const")
    P.dma("sp", ident[:], Cd["ident"].ap(), w=[B_const])
    P.op("dve", lambda e: e.tensor_copy(identb[:], ident[:]), r=[B_const], w=[B_const])
    P.op("dve", lambda e: e.memset(ones_bf[:], 1.0), w=[B_const])

    def load_cols(name, rows_ap, R, tag):
        st = sb("st_" + tag, [R, 128], F32)
        stB = Buf("st_" + tag)
        P.dma("sp", st[:], rows_ap, w=[stB])
        return st, stB

    def transpose_to(dst_ap, dstB, src_ap, srcB, R, bank=7):
        P.op("pe", lambda e: e.transpose(pb[bank][:, 0:R], src_ap, ident[0:R, 0:R]),
             r=[srcB, B_const], w=[pbB[bank]])
        P.op("dve", lambda e: e.tensor_copy(dst_ap, pb[bank][:, 0:R]), r=[pbB[bank]], w=[dstB])

    condT = sb("condT", [128, 8], F32)
    condB = Buf("condT")
    st_c, st_cB = load_cols("c", c_d.ap(), 8, "c")
    transpose_to(condT[:], condB, st_c[:], st_cB, 8)
    P.op("act", lambda e: e.activation(out=condT[:], in_=condT[:], func=AF.Silu), r=[condB], w=[condB])
    condTb = sb("condTb", [128, 8], BF16)
    P.op("dve", lambda e: e.tensor_copy(condTb[:], condT[:]), r=[condB], w=[condB])
    fgT = sb("fgT", [128, 8], F32)
    fgB = Buf("fgT")
    st_f, st_fB = load_cols("fg", Wd["final_g"].ap().rearrange("(r p) -> r p", p=128), 8, "fg")
    transpose_to(fgT[:], fgB, st_f[:], st_fB, 8)

    from contextlib import ExitStack
    es0 = ExitStack()
    xin = [es0.enter_context(nc.sbuf_tensor("T0_xin%d" % i, [128, D], F32)) for i in range(2)]
    xinB = [Buf("xin%d" % i) for i in range(2)]
    xtr = [es0.enter_context(nc.sbuf_tensor("T0_xtr%d" % i, [128, NCH, 128], F32)) for i in range(2)]
    xtrB = [Buf("xtr%d" % i) for i in range(2)]
    for blk in range(NBLK):
        i2 = blk % 2
        P.dma("sp", xin[i2][:], x_d.ap()[blk * 128:(blk + 1) * 128, :], w=[xinB[i2]])
        for half in range(2):
            bank = (blk * 2 + half) % 2
            for k4 in range(4):
                k = half * 4 + k4
                P.op("pe", lambda e, k=k, k4=k4, bank=bank: e.transpose(
                    pb[bank][:, k4 * 128:(k4 + 1) * 128], xin[i2][:, k * 128:(k + 1) * 128], ident[:]),
                    r=[xinB[i2], B_const], w=[pbB[bank]], inc=(k4 == 3))
            P.op("act" if half == 0 else "dve",
                 (lambda e, half=half, bank=bank: e.copy(
                     out=xtr[i2][:, half * 4:(half + 1) * 4, :].rearrange("p a b -> p (a b)"), in_=pb[bank][:]))
                 if half == 0 else
                 (lambda e, half=half, bank=bank: e.tensor_copy(
                     xtr[i2][:, half * 4:(half + 1) * 4, :].rearrange("p a b -> p (a b)"), pb[bank][:])),
                 r=[pbB[bank]], w=[xtrB[i2]])
        P.dma("sp", xT_v[:, :, blk * 128:(blk + 1) * 128], xtr[i2][:], r=[xtrB[i2]], w=[xT_b[blk // 4]])

    barrier(P)
    es0.close()
    cur_v, cur_b = xT_v, xT_b

    ctx = dict(P=P, nc=nc, S=S, L=L, NT=NT, NBLK=NBLK, Wd=Wd, Cd=Cd, pb=pb, pbB=pbB, ident=ident, identb=identb,
               ones_bf=ones_bf, B_const=B_const, condT=condT, condTb=condTb, condB=condB, transpose_to=transpose_to,
               load_cols=load_cols, xT_v=xT_v, xT_b=xT_b, x1T_v=x1T_v, x1T_b=x1T_b, tapset=tapset,
               tap_out=tap_out, sb=sb, do_mixer=do_mixer, do_moe=do_moe)
    ctx.update(extra)
    if L > 0 and run_layers:
        cur_v, cur_b = build_layers(ctx)

    xt = [sb("fx%d" % i, [128, NCH, 512], F32) for i in range(2)]
    xtB = [Buf("fx%d" % i) for i in range(2)]
    sq = [sb("fsq%d" % i, [128, 512], BF16) for i in range(2)]
    sqB = [Buf("fsq%d" % i) for i in range(2)]
    rs = sb("frs", [128, 512], F32)
    rsB = Buf("frs")
    yo = [sb("fyo%d" % i, [128, D], F32) for i in range(2)]
    yoB = [Buf("fyo%d" % i) for i in range(2)]
    for s in range(NT):
        i2 = s % 2
        X, XB = xt[i2], xtB[i2]
        P.dma("sp", X[:], cur_v[:, :, s * 512:(s + 1) * 512], r=[cur_b[s]], w=[XB])
        rms_stats(P, pb, pbB, 0, ones_bf, B_const, X, XB, sq, sqB, rs, rsB)
        for k in range(NCH):
            P.op("dve", lambda e, k=k: e.scalar_tensor_tensor(
                out=X[:, k, :], in0=X[:, k, :], scalar=fgT[:, k:k + 1], in1=rs[:], op0=ALU.mult, op1=ALU.mult),
                r=[XB, rsB, fgB], w=[XB])
        for t in range(4):
            blk = s * 4 + t
            o2 = blk % 2
            for half in range(2):
                bank = 1 + (blk * 2 + half) % 2
                for k4 in range(4):
                    k = half * 4 + k4
                    P.op("pe", lambda e, k=k, k4=k4, bank=bank, t=t: e.transpose(
                        pb[bank][:, k4 * 128:(k4 + 1) * 128], X[:, k, t * 128:(t + 1) * 128], ident[:]),
                        r=[XB, B_const], w=[pbB[bank]], inc=(k4 == 3))
                if half == 0:
                    P.op("act", lambda e, bank=bank, o2=o2: e.copy(out=yo[o2][:, 0:512], in_=pb[bank][:]),
                         r=[pbB[bank]], w=[yoB[o2]])
                else:
                    P.op("dve", lambda e, bank=bank, o2=o2: e.tensor_copy(yo[o2][:, 512:1024], pb[bank][:]),
                         r=[pbB[bank]], w=[yoB[o2]])
            P.dma("sp", y_d.ap()[blk * 128:(blk + 1) * 128, :], yo[o2][:], r=[yoB[o2]], w=[y_b])
    P.finish([y_b])
    return P, tap_d


def rms_stats(P, pb, pbB, bank, ones_bf, B_const, X, XB, sq, sqB, rs, rsB):
    for k in range(NCH):
        P.op("act", lambda e, k=k: e.activation(out=sq[k % 2][:], in_=X[:, k, :], func=AF.Square),
             r=[XB], w=[sqB[k % 2]])
        P.op("pe", lambda e, k=k: e.matmul(pb[bank][:], lhsT=ones_bf[:], rhs=sq[k % 2][:], start=(k == 0),
                                           stop=(k == NCH - 1)),
             r=[sqB[k % 2], B_const], w=[pbB[bank]], inc=True)
    P.op("act", lambda e: e.activation(out=rs[:], in_=pb[bank][:], func=AF.Sqrt, bias=EPS, scale=1.0 / D),
         r=[pbB[bank]], w=[rsB])
    P.op("dve", lambda e: e.reciprocal(rs[:], rs[:]), r=[rsB], w=[rsB])


def barrier(P):
    for e in ("pe", "act", "dve", "pool", "sp"):
        P._wait(e, {k: c for k, c in P.cnt.items() if c > 0})


def build_layers(ctx):
    from contextlib import ExitStack
    P = ctx["P"]; nc = ctx["nc"]; S = ctx["S"]; L = ctx["L"]; NT = ctx["NT"]
    Wd = ctx["Wd"]; Cd = ctx["Cd"]; pb = ctx["pb"]; pbB = ctx["pbB"]; sb = ctx["sb"]
    ident = ctx["ident"]; identb = ctx["identb"]; ones_bf = ctx["ones_bf"]; B_const = ctx["B_const"]
    condT = ctx["condT"]; condTb = ctx["condTb"]; condB = ctx["condB"]; transpose_to = ctx["transpose_to"]
    xT_v = ctx["xT_v"]; xT_b = ctx["xT_b"]; x1T_v = ctx["x1T_v"]; x1T_b = ctx["x1T_b"]
    tapset = ctx["tapset"]; tap_out = ctx["tap_out"]
    do_attn = ctx.get("do_attn", True); do_ret = ctx.get("do_ret", True); do_s5 = ctx.get("do_s5", True); RS = ctx.get("ret_stage", 99)

    pb7b = pb[7][:].bitcast(BF16)

    stA = sb("stA", [80, 128], F32); stAB = Buf("stA")
    vecT = sb("vecT", [128, 80], F32); vecB = Buf("vecT")
    st_ld = sb("st_ld", [8, 2], F32); st2 = sb("st2", [8, 128], F32); st2B = Buf("st2")
    ldtT = sb("ldtT", [128, 8], F32); ldtB = Buf("ldtT")
    st3 = sb("st3", [4, 128], F32); st3B = Buf("st3")
    sdT = sb("sdT", [128, 4], F32); sdB = Buf("sdT")
    modT = sb("modT", [128, 48], F32); modB = Buf("modT")
    modN = sb("modN", [128, 48], F32); modNB = Buf("modN")
    gm = sb("gm", [128, 16], F32); gmB = Buf("gm")
    tri_sb = sb("tri", [128, 128], F32)
    retG_sb = sb("retG", [128, 128], F32)
    amask_sb = sb("amask", [128, 640], BF16)
    P.dma("sp", tri_sb[:], Cd["tri"].ap(), w=[B_const])
    P.dma("sp", retG_sb[:], Cd["retG"].ap(), w=[B_const])
    P.dma("pool", amask_sb[:], Cd["amask"].ap(), w=[B_const])
    Fd = nc.dram_tensor("F_scr", [L, 8, 768], F32, kind="Internal")
    FdB = Buf("Fd")

    C_BADA, C_N1, C_N2, C_ARE, C_AIM = 0, 48, 56, 64, 72

    TS = 128
    s5 = {}
    TWO_PI = 2.0 * math.pi

    def s5_setup(l, es, a):
        def sba(name, shape, dt):
            return es.enter_context(nc.sbuf_tensor("S5_%d_%s" % (l, name), list(shape), dt))
        vecT, vecB, ldtT, ldtB = a["vecT"], a["vecB"], a["ldtT"], a["ldtB"]
        Ec = sba("Ec", [128, 8, TS], F32); Es = sba("Es", [128, 8, TS], F32); EB = Buf("E")
        WB = sba("WB", [128, 16, 128], BF16); WBB = Buf("WB")
        WC = sba("WC", [128, 24, 128], BF16); WCB = Buf("WC")
        E16 = sba("E16", [128, 2, 8, TS], BF16); E16B = Buf("E16")
        wglu = sba("wglu", [128, 2, 256], BF16); wgluB = Buf("wglu")
        sm = sba("sm", [128, 24, 8], F32); smB = Buf("sm")
        smi = sba("smi", [128, 8], I32)
        Braw = sba("Braw", [128, 2, 8, 16], F32); BrawB = Buf("Braw")
        Bbar = sba("Bbar", [128, 2, 8, 16], F32); BbarB = Buf("Bbar")
        Bt = sba("Bt", [128, 2, 8, 16], F32); BtB = Buf("Bt")
        Bx = sba("Bx", [128, 128], F32); BxB = Buf("Bx")
        Cx = sba("Cx", [128, 128], F32); CxB = Buf("Cx")
        Xst = sba("Xst", [128, 2, 8], F32); XstB = Buf("Xst")
        ct = sba("ct", [128, 4], F32); ctB = Buf("ct")
        mt, mtB = a["mt"], a["mtB"]
        zb = sba("zb", [128, 2, 2, 256], BF16); zbB = [[Buf("zb%d_%d" % (s_, i)) for i in range(2)] for s_ in range(2)]
        pr = sba("pr", [128, 2, 4, 256], BF16); prB = [[Buf("pr%d_%d" % (s_, i)) for i in range(4)] for s_ in range(2)]
        glb = sba("glb", [128, 2, 512], BF16); glB = Buf("gl")
        s5.update(Ec=Ec, Es=Es, EB=EB, WB=WB, WBB=WBB, WC=WC, WCB=WCB, wglu=wglu, wgluB=wgluB, sm=sm, smB=smB,
                  Xst=Xst, XstB=XstB, ct=ct, ctB=ctB, mt=mt, mtB=mtB, zb=zb, zbB=zbB, pr=pr, prB=prB, glb=glb,
                  glB=glB, a=a, E16=E16, E16B=E16B)
        V = lambda i: sm[:, i, :]
        are, aim = vecT[:, C_ARE:C_ARE + 8], vecT[:, C_AIM:C_AIM + 8]
        DT, T1, MAG, TH, YV, KF, FR, G1, SIN, COS, AR, AI, NR, DEN, CR, CI, T2, T3 = range(18)

        def dve(fn, r=(), w=()):
            P.op("dve", fn, r=list(r) + [smB], w=list(w) + [smB])

        def act(fn, r=(), w=()):
            P.op("act", fn, r=list(r) + [smB], w=list(w) + [smB])

        P.dma("pool", wglu[:], Wd["ssm_w_glu"].ap()[l].rearrange("(k p) n -> p k n", p=128), w=[wgluB])
        act(lambda e: e.activation(out=V(DT), in_=ldtT[:], func=AF.Exp), r=[ldtB])
        dve(lambda e: e.tensor_tensor(out=V(T1), in0=are, in1=V(DT), op=ALU.mult), r=[vecB])
        act(lambda e: e.activation(out=V(MAG), in_=V(T1), func=AF.Exp))
        dve(lambda e: e.tensor_tensor(out=V(TH), in0=aim, in1=V(DT), op=ALU.mult), r=[vecB])

        def sin_of(dst, shift):
            dve(lambda e: e.tensor_scalar(out=V(YV), in0=V(TH), scalar1=1.0 / TWO_PI, scalar2=shift, op0=ALU.mult,
                                          op1=ALU.add))
            dve(lambda e: e.tensor_copy(smi[:], V(YV)))
            dve(lambda e: e.tensor_copy(V(KF), smi[:]))
            dve(lambda e: e.tensor_tensor(out=V(FR), in0=V(YV), in1=V(KF), op=ALU.subtract))
            dve(lambda e: e.tensor_single_scalar(out=V(G1), in_=V(FR), scalar=0.5, op=ALU.is_gt))
            dve(lambda e: e.tensor_tensor(out=V(FR), in0=V(FR), in1=V(G1), op=ALU.subtract))
            dve(lambda e: e.tensor_single_scalar(out=V(G1), in_=V(FR), scalar=-0.5, op=ALU.is_lt))
            dve(lambda e: e.tensor_tensor(out=V(FR), in0=V(FR), in1=V(G1), op=ALU.add))
            act(lambda e: e.activation(out=V(dst), in_=V(FR), func=AF.Sin, scale=TWO_PI))

        sin_of(SIN, 0.0)
        sin_of(COS, 0.25)
        dve(lambda e: e.tensor_tensor(out=V(AR), in0=V(MAG), in1=V(COS), op=ALU.mult))
        dve(lambda e: e.tensor_tensor(out=V(AI), in0=V(MAG), in1=V(SIN), op=ALU.mult))
        dve(lambda e: e.tensor_scalar(out=V(NR), in0=V(AR), scalar1=-1.0, scalar2=None, op0=ALU.add))
        dve(lambda e: e.tensor_tensor(out=V(DEN), in0=are, in1=are, op=ALU.mult), r=[vecB])
        dve(lambda e: e.tensor_tensor(out=V(T2), in0=aim, in1=aim, op=ALU.mult), r=[vecB])
        dve(lambda e: e.tensor_tensor(out=V(DEN), in0=V(DEN), in1=V(T2), op=ALU.add))
        dve(lambda e: e.reciprocal(V(DEN), V(DEN)))
        dve(lambda e: e.tensor_tensor(out=V(T2), in0=V(NR), in1=are, op=ALU.mult), r=[vecB])
        dve(lambda e: e.tensor_tensor(out=V(T3), in0=V(AI), in1=aim, op=ALU.mult), r=[vecB])
        dve(lambda e: e.tensor_tensor(out=V(T2), in0=V(T2), in1=V(T3), op=ALU.add))
        dve(lambda e: e.tensor_tensor(out=V(CR), in0=V(T2), in1=V(DEN), op=ALU.mult))
        dve(lambda e: e.tensor_tensor(out=V(T2), in0=V(AI), in1=are, op=ALU.mult), r=[vecB])
        dve(lambda e: e.tensor_tensor(out=V(T3), in0=V(NR), in1=aim, op=ALU.mult), r=[vecB])
        dve(lambda e: e.tensor_tensor(out=V(T2), in0=V(T2), in1=V(T3), op=ALU.subtract))
        dve(lambda e: e.tensor_tensor(out=V(CI), in0=V(T2), in1=V(DEN), op=ALU.mult))
        for ri, nm in ((0, "ssm_b_re"), (1, "ssm_b_im")):
            src = Wd[nm].ap()[l].rearrange("(cb j) p c -> (j p) cb c", j=2)
            P.dma("sp", Braw[:, ri, :, :], src, w=[BrawB])
        bc = lambda i: sm[:, i, :, None].to_broadcast([128, 8, 16])
        P.op("dve", lambda e: e.tensor_tensor(out=Bbar[:, 0], in0=Braw[:, 0], in1=bc(CR), op=ALU.mult), r=[BrawB, smB], w=[BbarB])
        P.op("dve", lambda e: e.tensor_tensor(out=Bt[:, 0], in0=Braw[:, 1], in1=bc(CI), op=ALU.mult), r=[BrawB, smB], w=[BtB])
        P.op("dve", lambda e: e.tensor_tensor(out=Bbar[:, 0], in0=Bbar[:, 0], in1=Bt[:, 0], op=ALU.subtract), r=[BbarB, BtB], w=[BbarB])
        P.op("dve", lambda e: e.tensor_tensor(out=Bbar[:, 1], in0=Braw[:, 1], in1=bc(CR), op=ALU.mult), r=[BrawB, smB], w=[BbarB])
        P.op("dve", lambda e: e.tensor_tensor(out=Bt[:, 1], in0=Braw[:, 0], in1=bc(CI), op=ALU.mult), r=[BrawB, smB], w=[BtB])
        P.op("dve", lambda e: e.tensor_tensor(out=Bbar[:, 1], in0=Bbar[:, 1], in1=Bt[:, 1], op=ALU.add), r=[BbarB, BtB], w=[BbarB])
        for cb in range(8):
            q = cb % 4
            for ri in range(2):
                P.op("dve", lambda e: e.memset(Bx[:], 0.0), w=[BxB])
                for j in range(2):
                    P.op("dve", lambda e, j=j, q=q, ri=ri, cb=cb: e.tensor_copy(
                        Bx[64 * j:64 * j + 64, 32 * q + 16 * j:32 * q + 16 * j + 16], Bbar[64 * j:64 * j + 64, ri, cb, :]),
                        r=[BbarB], w=[BxB])
                P.op("pe", lambda e: e.transpose(pb[6][:, 0:128], Bx[:], ident[:]), r=[BxB, B_const], w=[pbB[6]])
                P.op("act", lambda e, cb=cb, ri=ri: e.copy(out=WB[:, cb * 2 + ri, :], in_=pb[6][:, 0:128]),
                     r=[pbB[6]], w=[WBB])
        P.op("dve", lambda e: e.memset(WC[:].rearrange("p a b -> p (a b)"), 0.0), w=[WCB])
        for ri, nm in ((0, "ssm_c_re"), (1, "ssm_c_im")):
            for hh in range(2):
                src = Wd[nm].ap()[l][8 * hh:8 * hh + 8].rearrange("g c p -> (g c) p")
                P.dma("sp", Cx[:, 0:64], src, w=[CxB])
                P.dma("sp", Cx[:, 64:128], src, w=[CxB])
                P.op("pe", lambda e: e.transpose(pb[6][:, 0:128], Cx[:], ident[:]), r=[CxB, B_const], w=[pbB[6]])
                P.op("act", lambda e: e.copy(out=Bx[:], in_=pb[6][:, 0:128]), r=[pbB[6]], w=[BxB])
                for q in range(4):
                    cb = 4 * hh + q
                    for j in range(2):
                        cols = slice(32 * q + 16 * j, 32 * q + 16 * j + 16)
                        for (slot_, sgn) in (((0, 1.0), (2, -1.0)) if ri == 0 else ((1, -1.0),)):
                            P.op("dve", lambda e, cb=cb, slot_=slot_, sgn=sgn, j=j, cols=cols: e.tensor_scalar(
                                out=WC[64 * j:64 * j + 64, cb * 3 + slot_, cols], in0=Bx[64 * j:64 * j + 64, cols],
                                scalar1=sgn, scalar2=None, op0=ALU.mult), r=[BxB], w=[WCB])
        P.op("dve", lambda e: e.tensor_copy(Ec[:, :, 0:1], sm[:, COS, :, None]), r=[smB], w=[EB])
        P.op("dve", lambda e: e.tensor_copy(Es[:, :, 0:1], sm[:, SIN, :, None]), r=[smB], w=[EB])
        n = 1
        tA = mt[0][:].rearrange("p (a b) -> p a b", a=8)
        tB_ = mt[1][:].rearrange("p (a b) -> p a b", a=8)
        while n < TS:
            cc = Ec[:, :, n - 1:n].to_broadcast([128, 8, n])
            ss = Es[:, :, n - 1:n].to_broadcast([128, 8, n])
            P.op("dve", lambda e, n=n, cc=cc: e.tensor_tensor(out=tA[:, :, 0:n], in0=Ec[:, :, 0:n], in1=cc, op=ALU.mult), r=[EB], w=[mtB[0]])
            P.op("dve", lambda e, n=n, ss=ss: e.tensor_tensor(out=tB_[:, :, 0:n], in0=Es[:, :, 0:n], in1=ss, op=ALU.mult), r=[EB], w=[mtB[1]])
            P.op("dve", lambda e, n=n: e.tensor_tensor(out=Ec[:, :, n:2 * n], in0=tA[:, :, 0:n], in1=tB_[:, :, 0:n], op=ALU.subtract), r=[mtB[0], mtB[1]], w=[EB])
            P.op("dve", lambda e, n=n, ss=ss: e.tensor_tensor(out=tA[:, :, 0:n], in0=Ec[:, :, 0:n], in1=ss, op=ALU.mult), r=[EB], w=[mtB[0]])
            P.op("dve", lambda e, n=n, cc=cc: e.tensor_tensor(out=tB_[:, :, 0:n], in0=Es[:, :, 0:n], in1=cc, op=ALU.mult), r=[EB], w=[mtB[1]])
            P.op("dve", lambda e, n=n: e.tensor_tensor(out=Es[:, :, n:2 * n], in0=tA[:, :, 0:n], in1=tB_[:, :, 0:n], op=ALU.add), r=[mtB[0], mtB[1]], w=[EB])
            n *= 2
        P.op("dve", lambda e: e.memset(Xst[:].rearrange("p a b -> p (a b)"), 0.0), w=[XstB])
        P.op("act", lambda e: e.copy(out=E16[:, 0].rearrange("p a b -> p (a b)"), in_=Ec[:].rearrange("p a b -> p (a b)")), r=[EB], w=[E16B])
        P.op("act", lambda e: e.copy(out=E16[:, 1].rearrange("p a b -> p (a b)"), in_=Es[:].rearrange("p a b -> p (a b)")), r=[EB], w=[E16B])
        s5["MAG"] = MAG

    def s5_gen(s, P):
        a = s5["a"]
        uT32, uTb, uTB, YT, YTB, sdT, sdB = a["uT32"], a["uTb"], a["uTB"], a["YT"], a["YTB"], a["sdT"], a["sdB"]
        Ec, Es, EB, WB, WBB, WC, WCB = s5["Ec"], s5["Es"], s5["EB"], s5["WB"], s5["WBB"], s5["WC"], s5["WCB"]
        E16, E16B = s5["E16"], s5["E16B"]
        sm, smB, Xst, XstB, ct, ctB = s5["sm"], s5["smB"], s5["Xst"], s5["XstB"], s5["ct"], s5["ctB"]
        mt, mtB = s5["mt"], s5["mtB"]
        zb, zbB, pr, prB = s5["zb"], s5["zbB"], s5["pr"], s5["prB"]
        glb, glB, wglu, wgluB = s5["glb"], s5["glB"], s5["wglu"], s5["wgluB"]
        MAG = s5["MAG"]
        HW = 256
        v2 = lambda ap: ap.rearrange("p (n t) -> p n t", n=2)
        unit = 0
        for half in range(2):
            tcols = slice(half * HW, (half + 1) * HW)
            for cb in range(8):
                st_ = unit % 2
                unit += 1
                kc = cb // 4
                T = [mt[i][:, st_ * HW:(st_ + 1) * HW] for i in range(4)]
                TB = [mtB[i][st_] for i in range(4)]
                ZB, ZBB = zb[:, st_], zbB[st_]
                PR, PRB = pr[:, st_], prB[st_]
                bank = st_
                Ecb = Ec[:, cb, None, :].to_broadcast([128, 2, TS])
                Esb = Es[:, cb, None, :].to_broadcast([128, 2, TS])
                Ec16 = E16[:, 0, cb, None, :].to_broadcast([128, 2, TS])
                Es16 = E16[:, 1, cb, None, :].to_broadcast([128, 2, TS])
                P.op("pe", lambda e: e.matmul(pb[bank][:, 0:HW], lhsT=WB[:, cb * 2, :], rhs=uTb[:, kc, tcols], start=True, stop=True),
                     r=[WBB, uTB], w=[pbB[bank]], inc=False)
                P.op("pe", lambda e: e.matmul(pb[bank][:, HW:2 * HW], lhsT=WB[:, cb * 2 + 1, :], rhs=uTb[:, kc, tcols], start=True, stop=True),
                     r=[WBB, uTB], w=[pbB[bank]])
                bur, bui = v2(pb[bank][:, 0:HW]), v2(pb[bank][:, HW:2 * HW])
                P.op("dve", lambda e: e.tensor_tensor(out=v2(T[0]), in0=bur, in1=Ecb, op=ALU.mult), r=[pbB[bank], EB], w=[TB[0]])
                P.op("dve", lambda e: e.tensor_tensor(out=v2(T[1]), in0=bui, in1=Esb, op=ALU.mult), r=[pbB[bank], EB], w=[TB[1]])
                P.op("dve", lambda e: e.tensor_tensor(out=v2(T[2]), in0=bui, in1=Ecb, op=ALU.mult), r=[pbB[bank], EB], w=[TB[2]])
                P.op("dve", lambda e: e.tensor_tensor(out=v2(T[3]), in0=bur, in1=Esb, op=ALU.mult), r=[pbB[bank], EB], w=[TB[3]])
                P.op("pool", lambda e: e.tensor_tensor(out=T[0], in0=T[0], in1=T[1], op=ALU.add), r=[TB[0], TB[1]], w=[TB[0]])
                P.op("pool", lambda e: e.tensor_tensor(out=T[2], in0=T[2], in1=T[3], op=ALU.subtract), r=[TB[2], TB[3]], w=[TB[2]])
                yield
                magb = sm[:, MAG, cb:cb + 1].to_broadcast([128, TS])
                for n in range(2):
                    cs_ = slice(n * TS, (n + 1) * TS)
                    P.op("dve", lambda e: e.tensor_tensor_scan(
                        out=T[1][:, cs_], data0=magb, data1=T[0][:, cs_], initial=Xst[:, 0, cb:cb + 1], op0=ALU.mult, op1=ALU.add),
                        r=[TB[0], XstB, smB], w=[TB[1]])
                    P.op("dve", lambda e: e.tensor_tensor_scan(
                        out=T[3][:, cs_], data0=magb, data1=T[2][:, cs_], initial=Xst[:, 1, cb:cb + 1], op0=ALU.mult, op1=ALU.add),
                        r=[TB[2], XstB, smB], w=[TB[3]])
                    last = (n + 1) * TS - 1
                    zrl, zil = T[1][:, last:last + 1], T[3][:, last:last + 1]
                    ecl, esl = Ec[:, cb, TS - 1:TS], Es[:, cb, TS - 1:TS]
                    P.op("dve", lambda e: e.tensor_tensor(out=ct[:, 0:1], in0=zil, in1=esl, op=ALU.mult), r=[TB[3], EB], w=[ctB])
                    P.op("dve", lambda e: e.tensor_tensor(out=ct[:, 1:2], in0=zil, in1=ecl, op=ALU.mult), r=[TB[3], EB], w=[ctB])
                    P.op("dve", lambda e: e.scalar_tensor_tensor(
                        out=Xst[:, 0, cb:cb + 1], in0=zrl, scalar=ecl, in1=ct[:, 0:1], op0=ALU.mult, op1=ALU.subtract),
                        r=[TB[1], EB, ctB], w=[XstB])
                    P.op("dve", lambda e: e.scalar_tensor_tensor(
                        out=Xst[:, 1, cb:cb + 1], in0=zrl, scalar=esl, in1=ct[:, 1:2], op0=ALU.mult, op1=ALU.add),
                        r=[TB[1], EB, ctB], w=[XstB])
                P.op("act", lambda e: e.copy(out=ZB[:, 0, :], in_=T[1]), r=[TB[1]], w=[ZBB[0]])
                P.op("act", lambda e: e.copy(out=ZB[:, 1, :], in_=T[3]), r=[TB[3]], w=[ZBB[1]])
                yield
                for (i_, zi_, tab) in ((0, 0, Ec16), (1, 1, Es16), (2, 0, Es16), (3, 1, Ec16)):
                    P.op("dve", lambda e, i_=i_, zi_=zi_, tab=tab: e.tensor_tensor(
                        out=v2(PR[:, i_, :]), in0=v2(ZB[:, zi_, :]), in1=tab, op=ALU.mult), r=[ZBB[zi_], E16B], w=[PRB[i_]])
                for (i_, wsel) in ((0, 0), (1, 2), (2, 1), (3, 1)):
                    P.op("pe", lambda e, i_=i_, wsel=wsel: e.matmul(
                        pb[6][:, 0:HW], lhsT=WC[:, cb * 3 + wsel, :], rhs=PR[:, i_, :], start=(cb % 4 == 0 and i_ == 0),
                        stop=(cb % 4 == 3 and i_ == 3)), r=[WCB, PRB[i_]], w=[pbB[6]], inc=(i_ == 3))
                if cb % 4 == 3:
                    mc = cb // 4
                    P.op("dve", lambda e: e.scalar_tensor_tensor(
                        out=T[0], in0=uT32[:, mc, tcols], scalar=sdT[:, mc:mc + 1], in1=pb[6][:, 0:HW], op0=ALU.mult, op1=ALU.add),
                        r=[uTB, sdB, pbB[6]], w=[TB[0]])
                    P.op("act", lambda e: e.activation(out=glb[:, mc, tcols], in_=T[0], func=AF.Gelu_apprx_tanh),
                         r=[TB[0]], w=[glB])
                yield
        for m in range(2):
            P.begin()
            for k in range(2):
                P.op("pe", lambda e, m=m, k=k: e.matmul(pb[7][:], lhsT=wglu[:, k, m * 128:(m + 1) * 128], rhs=glb[:, k, :],
                                                        start=(k == 0), stop=(k == 1)), r=[wgluB, glB], w=[pbB[7]], inc=(k == 1))
            P.op("act", lambda e, m=m: e.activation(out=mt[m][:], in_=pb[7][:], func=AF.Sigmoid, bias=sdT[:, 2 + m:3 + m], scale=1.0),
                 r=[pbB[7], sdB], w=[mtB[m]])
            P.end()
            P.op("dve", lambda e, m=m: e.tensor_tensor(out=YT[:, 4 + m, :], in0=glb[:, m, :], in1=mt[m][:], op=ALU.mult),
                 r=[glB, mtB[m]], w=[YTB[4 + m]])
        yield

    ctx["s5_setup"] = s5_setup
    ctx["s5_gen"] = s5_gen

    HS = min(S, 2048)
    NHALF = S // HS
    cmb_d = nc.dram_tensor("cmb_scr", [32, HS], BF16, kind="Internal")
    cmbdB = Buf("cmb_d")
    NST = HS // 512
    BIG = 1.0e30

    def moe_phase(l, a):
        sh2, g2, gm2, modB, gmB = a["sh2"], a["g2"], a["gm2"], a["modB"], a["gmB"]
        with ExitStack() as es:
            def sba(name, shape, dt):
                return es.enter_context(nc.sbuf_tensor("B%d_%s" % (l, name), list(shape), dt))
            acc = sba("acc", [128, 8, HS], F32); accB = [Buf("acc%d" % i) for i in range(NST)]
            h2T = sba("h2T", [128, 8, HS], BF16); h2B = [Buf("h2T%d" % i) for i in range(NST)]
            h32 = sba("h32", [128, 8, 512], F32); h32B = Buf("h32")
            sq = [sba("sq%d" % i, [128, 512], BF16) for i in range(2)]; sqB = [Buf("sq%d" % i) for i in range(2)]
            rs = sba("rs", [128, 512], F32); rsB = Buf("rs")
            tmp = [sba("tmp0", [128, 512], F32)] * 2; tmpB = [Buf("tmp0")] * 2
            wr_sb = sba("wr", [128, 8, 36], F32); wrB = Buf("wr")
            rb_bc = sba("rb", [128, 36], F32); rbB = Buf("rb")
            combT = sba("combT", [128, HS], BF16); combB = [Buf("combT%d" % i) for i in range(NST)]
            sel = sba("sel", [128, 32, 128], BF16); selB = Buf("sel")
            wgu = [[sba("wgu%d_%d" % (i, j), [128, 8, 512], BF16) for j in range(2)] for i in range(2)]
            wguB = [[Buf("wgu%d_%d" % (i, j)) for j in range(2)] for i in range(2)]
            wd = [[sba("wd%d_%d" % (i, j), [128, 2, 1024], BF16) for j in range(2)] for i in range(2)]
            wdB = [[Buf("wd%d_%d" % (i, j)) for j in range(2)] for i in range(2)]
            sgt = sba("sgt", [128, 2, 512], BF16); sgB = [Buf("sg0"), Buf("sg1")]
            tt = sba("tt", [128, 2, 512], BF16); ttB = [Buf("tt0"), Buf("tt1")]
            actT = [sba("actT%d" % i, [128, 2, 2, 512], BF16) for i in range(2)]
            actB = [[[Buf("act%d_%d_%d" % (i, j, f)) for f in range(2)] for j in range(2)] for i in range(2)]
            wa2 = [sba("wa2_%d" % i, [128, 8, 128], BF16) for i in range(2)]; wa2B = [Buf("wa2_%d" % i) for i in range(2)]
            cbs = [sba("cbs%d" % i, [128, 2, 512], BF16) for i in range(2)]
            cbsB = [[Buf("cbs%d_%d" % (i, j)) for j in range(2)] for i in range(2)]
            rt = actT[0][:].rearrange("p a b c -> p (a b c)").bitcast(F32); rtB = Buf("rt")
            cB = Buf("comb")

            P.op("dve", lambda e: e.memset(sel[:].rearrange("p a b -> p (a b)"), 0.0), w=[selB])
            P.op("dve", lambda e: e.tensor_copy(sel[0:32, :, :], identb[0:32, 0:32, None].to_broadcast([32, 32, 128])),
                 r=[B_const], w=[selB])
            P.op("dve", lambda e: e.memset(combT[:], 0.0), w=combB)
            P.dma("sp", wr_sb[:, :, 0:4], Wd["moe_w_group"].ap()[l].rearrange("(k p) n -> p k n", p=128), w=[wrB])
            P.dma("sp", wr_sb[:, :, 4:36], Wd["moe_w_expert"].ap()[l].rearrange("(k p) n -> p k n", p=128), w=[wrB])
            P.dma("sp", rb_bc[:, 0:4], Wd["moe_b_group"].ap()[l:l + 1, :].to_broadcast([128, 4]), w=[rbB])
            P.dma("sp", rb_bc[:, 4:36], Wd["moe_b_expert"].ap()[l:l + 1, :].to_broadcast([128, 32]), w=[rbB])

            def load_pair(p):
                for j in range(2):
                    e = 2 * p + j
                    W_, WB_ = wgu[p % 2][j], wguB[p % 2][j]
                    P.dma("pool", W_[:, :, 0:256], Wd["moe_w_gate"].ap()[l, e].rearrange("(k p) f -> p k f", p=128), w=[WB_])
                    P.dma("pool", W_[:, :, 256:512], Wd["moe_w_up"].ap()[l, e].rearrange("(k p) f -> p k f", p=128), w=[WB_])
                    P.dma("pool", wd[p % 2][j][:], Wd["moe_w_down"].ap()[l, e].rearrange("(k p) d -> p k d", p=128),
                          w=[wdB[p % 2][j]])

            for hf in range(NHALF):
                t0 = hf * HS
                def norm_part(PP, st):
                    cols = slice(st * 512, (st + 1) * 512)
                    gs = (t0 // 512) + st
                    X = acc[:, :, cols]
                    PP.dma("sp", X, x1T_v[:, :, t0 + st * 512:t0 + (st + 1) * 512], r=[x1T_b[gs]], w=[accB[st]])
                    rms_stats(PP, pb, pbB, st % 2, ones_bf, B_const, X, accB[st], sq, sqB, rs, rsB)
                    for k in range(8):
                        T_, TB_ = tmp[k % 2], tmpB[k % 2]
                        PP.op("dve", lambda e, k=k, T_=T_, X=X: e.scalar_tensor_tensor(
                            out=T_[:], in0=X[:, k, :], scalar=gm2(k), in1=rs[:], op0=ALU.mult, op1=ALU.mult),
                            r=[accB[st], rsB, gmB], w=[TB_])
                        PP.op("act", lambda e, k=k, T_=T_: e.activation(out=h32[:, k, :], in_=T_[:], func=AF.Identity,
                                                                        bias=sh2(k), scale=1.0), r=[TB_, modB], w=[h32B])
                    PP.op("pool", lambda e, cols=cols: e.tensor_copy(h2T[:, :, cols], h32[:]), r=[h32B], w=[h2B[st]])

                def router_mm(st):
                    bk = 2 + st % 2
                    for t in range(4):
                        tok = slice(t * 128, (t + 1) * 128)
                        for k in range(8):
                            P.op("pe", lambda e, k=k, bk=bk, tok=tok, t=t: e.matmul(
                                pb[bk][:, t * 36:(t + 1) * 36], lhsT=h32[:, k, tok], rhs=wr_sb[:, k, :], start=(k == 0),
                                stop=(k == 7)), r=[h32B, wrB], w=[pbB[bk]], inc=(k == 7))

                def router_chain(PP, st):
                    bk = 2 + st % 2
                    LGS = rt[:, 0:144].rearrange("p (t x) -> p t x", t=4)
                    off = [144]

                    def alloc(n):
                        o = off[0]; off[0] += 4 * n
                        return rt[:, o:o + 4 * n].rearrange("p (t x) -> p t x", t=4)
                    GM, GD, GOH, GEX, GS, PEN = alloc(1), alloc(4), alloc(4), alloc(4), alloc(1), alloc(4)
                    MK, M1, D1, OH1, MK2, M2, OH2 = alloc(32), alloc(1), alloc(32), alloc(32), alloc(32), alloc(1), alloc(32)
                    DD, W1, W2, CMB = alloc(1), alloc(1), alloc(1), alloc(32)
                    bc = lambda ap, n: ap.to_broadcast([128, 4, n])

                    def dv(fn, extra_r=()):
                        PP.op("dve", fn, r=[rtB] + list(extra_r), w=[rtB])

                    PP.op("dve", lambda e, bk=bk: e.tensor_tensor(
                        out=LGS, in0=pb[bk][:, 0:144].rearrange("p (t x) -> p t x", t=4),
                        in1=rb_bc[:, None, :].to_broadcast([128, 4, 36]), op=ALU.add), r=[pbB[bk], rbB, rtB], w=[rtB])
                    dv(lambda e: e.tensor_reduce(out=GM, in_=LGS[:, :, 0:4], axis=AX.X, op=ALU.max))
                    dv(lambda e: e.tensor_tensor(out=GD, in0=LGS[:, :, 0:4], in1=bc(GM, 4), op=ALU.subtract))
                    dv(lambda e: e.tensor_single_scalar(out=GOH, in_=GD, scalar=0.0, op=ALU.is_equal))
                    PP.op("act", lambda e: e.activation(out=GEX, in_=GD, func=AF.Exp), r=[rtB], w=[rtB])
                    dv(lambda e: e.tensor_reduce(out=GS, in_=GEX, axis=AX.X, op=ALU.add))
                    dv(lambda e: e.reciprocal(GS, GS))
                    dv(lambda e: e.tensor_scalar(out=PEN, in0=GOH, scalar1=-1.0, scalar2=BIG, op0=ALU.add, op1=ALU.mult))
                    for t in range(4):
                        mk3 = MK[:, t, :].rearrange("p (g x) -> p g x", g=4)
                        el3 = LGS[:, t, 4:36].rearrange("p (g x) -> p g x", g=4)
                        dv(lambda e, mk3=mk3, el3=el3, t=t: e.tensor_tensor(
                            out=mk3, in0=el3, in1=GOH[:, t, :, None].to_broadcast([128, 4, 8]), op=ALU.mult))
                        dv(lambda e, mk3=mk3, t=t: e.tensor_tensor(
                            out=mk3, in0=mk3, in1=PEN[:, t, :, None].to_broadcast([128, 4, 8]), op=ALU.add))
                    dv(lambda e: e.tensor_reduce(out=M1, in_=MK, axis=AX.X, op=ALU.max))
                    dv(lambda e: e.tensor_tensor(out=D1, in0=MK, in1=bc(M1, 32), op=ALU.subtract))
                    dv(lambda e: e.tensor_single_scalar(out=OH1, in_=D1, scalar=0.0, op=ALU.is_equal))
                    dv(lambda e: e.scalar_tensor_tensor(out=MK2, in0=OH1, scalar=-BIG, in1=MK, op0=ALU.mult, op1=ALU.add))
                    dv(lambda e: e.tensor_reduce(out=M2, in_=MK2, axis=AX.X, op=ALU.max))
                    dv(lambda e: e.tensor_tensor(out=D1, in0=MK2, in1=bc(M2, 32), op=ALU.subtract))
                    dv(lambda e: e.tensor_single_scalar(out=OH2, in_=D1, scalar=0.0, op=ALU.is_equal))
                    dv(lambda e: e.tensor_tensor(out=DD, in0=M2, in1=M1, op=ALU.subtract))
                    PP.op("act", lambda e: e.activation(out=DD, in_=DD, func=AF.Exp), r=[rtB], w=[rtB])
                    dv(lambda e: e.tensor_scalar(out=W1, in0=DD, scalar1=1.0, scalar2=None, op0=ALU.add))
                    dv(lambda e: e.reciprocal(W1, W1))
                    dv(lambda e: e.tensor_tensor(out=W2, in0=DD, in1=W1, op=ALU.mult))
                    dv(lambda e: e.tensor_tensor(out=W1, in0=W1, in1=GS, op=ALU.mult))
                    dv(lambda e: e.tensor_tensor(out=W2, in0=W2, in1=GS, op=ALU.mult))
                    dv(lambda e: e.tensor_tensor(out=OH1, in0=OH1, in1=bc(W1, 32), op=ALU.mult))
                    dv(lambda e: e.tensor_tensor(out=OH2, in0=OH2, in1=bc(W2, 32), op=ALU.mult))
                    PP.op("dve", lambda e: e.tensor_tensor(out=CMB, in0=OH1, in1=OH2, op=ALU.add), r=[rtB], w=[rtB, cB])
                    for t in range(4):
                        PP.op("pe", lambda e, t=t: e.transpose(pb[4][0:32, t * 128:(t + 1) * 128], CMB[:, t, :], ident[:]),
                             r=[rtB, cB, B_const], w=[pbB[4]], inc=(t == 3))
                    PP.op("act", lambda e, st=st: e.copy(out=combT[0:32, st * 512:(st + 1) * 512], in_=pb[4][0:32, :]),
                         r=[pbB[4]], w=[combB[st]])
                    if "comb" in tapset:
                        if "tapcomb" not in ctx:
                            ctx["tapcomb"] = tap_out("comb", [L, S, 32]); ctx["tapcombB"] = Buf("tapcomb")
                        for t in range(4):
                            g0 = t0 + st * 512 + t * 128
                            PP.dma("sp", ctx["tapcomb"].ap()[l][g0:g0 + 128, :], CMB[:, t, :], r=[rtB, cB], w=[ctx["tapcombB"]])

                barrier(P)
                norm_part(P, 0)
                for st in range(NST):
                    router_mm(st)
                    d1 = Deferred()
                    router_chain(d1, st)
                    ds = [d1]
                    if st + 1 < NST:
                        d2 = Deferred()
                        norm_part(d2, st + 1)
                        ds.append(d2)
                    run_interleaved(P, ds)
                barrier(P)
                units = [(p, st) for p in range(N_EXP // 2) for st in range(NST)]
                dcnt = [0]

                def emit_expert(i, j, mid=None):
                    p, st = units[i]
                    e = 2 * p + j
                    cols = slice(st * 512, (st + 1) * 512)
                    W_, WB_ = wgu[p % 2][j], wguB[p % 2][j]
                    CB, CBB = cbs[i % 2], cbsB[i % 2][j]
                    for f in range(2):
                        if f == 1 and mid is not None:
                            mid()
                        for (bank, col0) in ((0 + f, f * 128), (2 + f, 256 + f * 128)):
                            for k in range(8):
                                P.op("pe", lambda ee, k=k, bank=bank, col0=col0: ee.matmul(
                                    pb[bank][:], lhsT=W_[:, k, col0:col0 + 128], rhs=h2T[:, k, cols], start=(k == 0),
                                    stop=(k == 7)), r=[WB_, h2B[st]], w=[pbB[bank]], inc=(k == 7))
                        P.op("act", lambda ee, f=f: ee.activation(out=sgt[:, f, :], in_=pb[f][:], func=AF.Silu),
                             r=[pbB[f]], w=[sgB[f]])
                        P.op("dve", lambda ee, f=f: ee.tensor_tensor(out=tt[:, f, :], in0=pb[2 + f][:], in1=sgt[:, f, :],
                                                                     op=ALU.mult), r=[pbB[2 + f], sgB[f]], w=[ttB[f]])
                        P.op("dve", lambda ee, f=f: ee.tensor_tensor(out=actT[i % 2][:, j, f, :], in0=tt[:, f, :],
                                                                     in1=CB[:, j, :], op=ALU.mult),
                             r=[ttB[f], CBB], w=[actB[i % 2][j][f]])

                def emit_down(i):
                    p, st = units[i]
                    cols = slice(st * 512, (st + 1) * 512)
                    for d in range(8):
                        bank = 5 + dcnt[0] % 3
                        dcnt[0] += 1
                        n = 0
                        for j in range(2):
                            for f in range(2):
                                P.op("pe", lambda ee, d=d, f=f, j=j, bank=bank, n=n: ee.matmul(
                                    pb[bank][:], lhsT=wd[p % 2][j][:, f, d * 128:(d + 1) * 128], rhs=actT[i % 2][:, j, f, :],
                                    start=(n == 0), stop=(n == 3)), r=[wdB[p % 2][j], actB[i % 2][j][f]], w=[pbB[bank]],
                                    inc=(n == 3))
                                n += 1
                        P.op("dve", lambda ee, d=d, bank=bank: ee.scalar_tensor_tensor(
                            out=acc[:, d, cols], in0=pb[bank][:], scalar=g2(d), in1=acc[:, d, cols], op0=ALU.mult, op1=ALU.add),
                            r=[pbB[bank], accB[st], modB], w=[accB[st]])

                P.dma("sp", cmb_d.ap(), combT[0:32, :], r=combB, w=[cmbdB])

                def load_cb(i):
                    p_, st_ = units[i]
                    for j_ in range(2):
                        P.dma("sp", cbs[i % 2][:, j_, :],
                              cmb_d.ap()[2 * p_ + j_:2 * p_ + j_ + 1, st_ * 512:(st_ + 1) * 512].to_broadcast([128, 512]),
                              r=[cmbdB], w=[cbsB[i % 2][j_]])
                load_cb(0)
                load_pair(0)
                pre = (hf == NHALF - 1 and l + 1 < L)
                if pre:
                    wavn = Wd["w_ada"].ap()[l + 1].rearrange("(k p) n -> p k n", p=128)
                    NPIECE = 48
                    every = max(1, (len(units) - 4) // NPIECE)

                    def mod_dma(j):
                        P.dma("pool", wa2[j % 2][:], wavn[:, :, j * 128:(j + 1) * 128], w=[wa2B[j % 2]])

                    def mod_piece(j):
                        W_, WB_ = wa2[j % 2], wa2B[j % 2]
                        for k in range(8):
                            P.op("pe", lambda e, k=k: e.matmul(
                                pb[4][:, 0:1], lhsT=W_[:, k, :], rhs=condTb[:, k:k + 1],
                                start=(k == 0), stop=(k == 7)), r=[WB_, condB], w=[pbB[4]], inc=(k == 7))
                        P.op("act", lambda e: e.copy(out=modN[:, j:j + 1], in_=pb[4][:, 0:1]), r=[pbB[4]], w=[modNB])
                    mod_dma(0)
                    mod_dma(1)
                for i in range(len(units)):
                    p, st = units[i]
                    if pre and i % every == 0 and i // every < NPIECE:
                        j = i // every
                        mod_piece(j)
                        if j + 2 < NPIECE:
                            mod_dma(j + 2)
                    if i + 1 < len(units):
                        load_cb(i + 1)
                    emit_expert(i, 0)

                    def mid(i=i, p=p, st=st):
                        if i >= 1:
                            emit_down(i - 1)
                        if st == 0 and p + 1 < N_EXP // 2:
                            load_pair(p + 1)
                    emit_expert(i, 1, mid=mid)
                emit_down(len(units) - 1)
                if pre:
                    for j in range(min(NPIECE, (len(units) + every - 1) // every), NPIECE):
                        mod_piece(j)
                        if j + 2 < NPIECE:
                            mod_dma(j + 2)
                for st in range(NST):
                    gs = (t0 // 512) + st
                    P.dma("sp", xT_v[:, :, t0 + st * 512:t0 + (st + 1) * 512], acc[:, :, st * 512:(st + 1) * 512],
                          r=[accB[st]], w=[xT_b[gs]])

    ctx["moe_phase"] = moe_phase

    if "YT" in tapset:
        ctx["tapYT"] = tap_out("YT", [L, D, S])
        ctx["tapYTB"] = Buf("tapYT")
        ctx["tapst"] = sb("tapst", [128, 512], F32)
        ctx["tapstB"] = Buf("tapst")
    if not ctx["do_moe"]:
        xt0 = sb("cpx", [128, 8, 32], F32); xt0B = Buf("cp")

    for l in range(L):
        P.dma("sp", stA[0:48, :], Wd["b_ada"].ap()[l].rearrange("(r p) -> r p", p=128), w=[stAB])
        P.dma("sp", stA[48:56, :], Wd["norm1_g"].ap()[l].rearrange("(r p) -> r p", p=128), w=[stAB])
        P.dma("sp", stA[56:64, :], Wd["norm2_g"].ap()[l].rearrange("(r p) -> r p", p=128), w=[stAB])
        P.dma("sp", stA[64:72, :], Wd["ssm_a_re"].ap()[l].rearrange("(cb j) p -> cb (j p)", j=2), w=[stAB])
        P.dma("sp", stA[72:80, :], Wd["ssm_a_im"].ap()[l].rearrange("(cb j) p -> cb (j p)", j=2), w=[stAB])
        transpose_to(vecT[:], vecB, stA[:], stAB, 80)
        P.dma("sp", st_ld[:], Wd["ssm_log_dt"].ap()[l].rearrange("(cb j) -> cb j", j=2), w=[st2B])
        P.op("dve", lambda e: e.tensor_copy(st2[:].rearrange("p (j q) -> p j q", j=2),
                                            st_ld[:, :, None].to_broadcast([8, 2, 64])), r=[st2B], w=[st2B])
        transpose_to(ldtT[:], ldtB, st2[:], st2B, 8)
        P.dma("sp", st3[0:2, :], Wd["ssm_d"].ap()[l].rearrange("(r p) -> r p", p=128), w=[st3B])
        P.dma("sp", st3[2:4, :], Wd["ssm_b_glu"].ap()[l].rearrange("(r p) -> r p", p=128), w=[st3B])
        transpose_to(sdT[:], sdB, st3[:], st3B, 4)
        es1 = ExitStack()
        if l == 0 or not ctx["do_moe"]:
            wav = Wd["w_ada"].ap()[l].rearrange("(k p) n -> p k n", p=128)
            wa = [es1.enter_context(nc.sbuf_tensor("wa%d_%d" % (l, i), [128, 8, 512], BF16)) for i in range(2)]
            waB = [Buf("wa%d" % i) for i in range(2)]
            for j in range(12):
                W_, WB_ = wa[j % 2], waB[j % 2]
                P.dma("pool", W_[:], wav[:, :, j * 512:(j + 1) * 512], w=[WB_])
                for m in range(4):
                    col = 4 * j + m
                    for k in range(8):
                        P.op("pe", lambda e, W_=W_, m=m, k=k, col=col: e.matmul(
                            pb[6][:, col:col + 1], lhsT=W_[:, k, m * 128:(m + 1) * 128], rhs=condTb[:, k:k + 1],
                            start=(k == 0), stop=(k == 7)), r=[WB_, condB], w=[pbB[6]], inc=(k == 7))

            P.op("act", lambda e: e.copy(out=modN[:], in_=pb[6][:, 0:48]), r=[pbB[6]], w=[modNB])
        P.op("dve", lambda e: e.tensor_tensor(out=modT[:], in0=modN[:], in1=vecT[:, C_BADA:C_BADA + 48],
                                              op=ALU.add), r=[modNB, vecB], w=[modB])
        P.op("dve", lambda e: e.scalar_tensor_tensor(out=gm[:, 0:8], in0=modT[:, 8:16], scalar=1.0,
                                                     in1=vecT[:, C_N1:C_N1 + 8], op0=ALU.add, op1=ALU.mult),
             r=[modB, vecB], w=[gmB])
        P.op("dve", lambda e: e.scalar_tensor_tensor(out=gm[:, 8:16], in0=modT[:, 32:40], scalar=1.0,
                                                     in1=vecT[:, C_N2:C_N2 + 8], op0=ALU.add, op1=ALU.mult),
             r=[modB, vecB], w=[gmB])
        sh1 = lambda k: modT[:, k:k + 1]
        g1 = lambda k: modT[:, 16 + k:17 + k]
        sh2 = lambda k: modT[:, 24 + k:25 + k]
        g2 = lambda k: modT[:, 40 + k:41 + k]
        gm1 = lambda k: gm[:, k:k + 1]
        gm2 = lambda k: gm[:, 8 + k:9 + k]

        barrier(P)
        es1.close()
        with ExitStack() as es:
            def sba(name, shape, dt):
                return es.enter_context(nc.sbuf_tensor("A%d_%s" % (l, name), list(shape), dt))
            w_in_sb = sba("w_in", [128, 8, IN_W], BF16); winB = Buf("w_in")
            w_out_sb = sba("w_out", [128, 8, D], BF16); woutB = Buf("w_out")
            wiv = Wd["w_in"].ap()[l].rearrange("(k p) n -> p k n", p=128)
            wov = Wd["w_out"].ap()[l].rearrange("(k p) n -> p k n", p=128)
            for k in range(8):
                for hh in range(2):
                    P.dma("pool", w_in_sb[:, k, hh * 1408:(hh + 1) * 1408], wiv[:, k, hh * 1408:(hh + 1) * 1408],
                          w=[winB])
            for k in range(8):
                P.dma("pool", w_out_sb[:, k, :], wov[:, k, :], w=[woutB])
            xt = [sba("xt%d" % i, [128, 8, 512], F32) for i in range(1)]
            xtB = [Buf("xt%d" % i) for i in range(1)]
            sq = [sba("sq%d" % i, [128, 512], BF16) for i in range(2)]; sqB = [Buf("sq%d" % i) for i in range(2)]
            rs = sba("rs", [128, 512], F32); rsB = Buf("rs")
            mt = [sba("mt%d" % i, [128, 512], F32) for i in range(4)]
            mtB = [[Buf("mt%dA" % i), Buf("mt%dB" % i)] for i in range(4)]
            tmp = mt[0:2]; tmpB = mtB[0:2]
            hT = sba("hT", [128, 8, 512], BF16); hTB = Buf("hT")
            qT = sba("qT", [128, 2, 4, 512], BF16); qTB = Buf("qT")
            kT = sba("kT", [128, 4, 1024], BF16); kTB = [Buf("kT%d" % i) for i in range(8)]
            Vr = sba("Vr", [128, 8, 8, 65], BF16); VrB = [Buf("Vr%d" % i) for i in range(8)]
            uT32 = sba("uT32", [128, 2, 512], F32); uTb = sba("uTb", [128, 2, 512], BF16); uTB = Buf("uT")
            YT = sba("YT", [128, 8, 512], BF16)
            YTB = [Buf("YT%d" % i) for i in range(8)]
            expB = sba("expB", [128, 8, 5, 128], BF16); expBB = Buf("expB")
            Pt = [sba("Pt%d" % i, [128, 5, 128], BF16) for i in range(3)]
            PtB = [Buf("Pt%d" % i) for i in range(3)]
            trB = Buf("tr4")
            pb4b = pb[4][:].bitcast(BF16)
            ya = sba("ya", [128, 8, 64], BF16); yaB = Buf("ya")
            rec = sba("rec", [128, 8], F32); recB = Buf("rec")
            tailB = [Buf("tail%d" % i) for i in range(4)]
            cs = [sba("cs%d" % i, [128, 512], F32) for i in range(1)] * 2; csB = [Buf("cs0")] * 2
            rmt = sba("rmt", [128, 2, 256], F32)
            rm = [rmt[:, i % 2, :].rearrange("p (h f) -> p h f", h=8) for i in range(4)]
            rmB = [Buf("rm0"), Buf("rm1")] * 2
            qkrot = sba("qkrot", [128, 8, 64], BF16); qkrotB = Buf("qkrot")
            qTr = sba("qTr", [128, 2, 2, 128], BF16); kTr = sba("kTr", [128, 2, 128], BF16); qkTB = Buf("qkT")
            vr = sba("vr", [128, 256], BF16); vrB = Buf("vr")
            sg = sba("sg", [128, 256], F32); sgB = Buf("sg")
            Am = sba("Am", [128, 4, 128], BF16); AmB = Buf("Am")
            Sst = sba("Sst", [128, 2, 64], F32); Sbf = sba("Sbf", [128, 2, 64], BF16); SstB = Buf("Sst"); SbfB = Buf("Sbf")
            ysb = sba("ysb", [128, 256], F32); ysbB = Buf("ysb")
            ysq = sba("ysq", [128, 256], F32); ysqB = Buf("ysq")
            gst = sba("gst", [128, 16], F32); gstB = Buf("gst")
            gn_bc = sba("gn_bc", [128, 256], F32); gnB = Buf("gn_bc")
            yr = sba("yr", [128, 256], BF16); yrB = Buf("yr")

            P.op("dve", lambda e: e.memset(Vr[:].rearrange("p a b c -> p (a b c)"), 1.0), w=VrB)
            P.op("dve", lambda e: e.memset(Sst[:].rearrange("p a b -> p (a b)"), 0.0), w=[SstB])
            P.op("dve", lambda e: e.memset(Sbf[:].rearrange("p a b -> p (a b)"), 0.0), w=[SbfB])
            P.op("dve", lambda e: e.memset(YT[:].rearrange("p a b -> p (a b)"), 0.0), w=YTB)
            P.op("dve", lambda e: e.memset(qT[:].rearrange("p a b c -> p (a b c)"), 0.0), w=[qTB])
            P.op("dve", lambda e: e.memset(qTr[:].rearrange("p a b c -> p (a b c)"), 0.0), w=[qkTB])
            P.dma("sp", gn_bc[:], Wd["ret_gn_g"].ap()[l:l + 1, :].to_broadcast([128, 256]), w=[gnB])
            es2 = ExitStack()
            Hk = es2.enter_context(nc.sbuf_tensor("Hk%d" % l, [128, 5, 128], F32)); HkB = Buf("Hk")
            Fsb = es2.enter_context(nc.sbuf_tensor("Fsb%d" % l, [8, 768], F32)); FsbB = Buf("Fsb")
            P.dma("sp", Fsb[:, 511:703], Wd["attn_rel_bias"].ap()[l], w=[FsbB])
            P.op("dve", lambda e: e.tensor_copy(Fsb[:, 0:511], Fsb[:, 511:512].to_broadcast([8, 511])),
                 r=[FsbB], w=[FsbB])
            P.op("dve", lambda e: e.tensor_copy(Fsb[:, 703:768], Fsb[:, 702:703].to_broadcast([8, 65])),
                 r=[FsbB], w=[FsbB])
            P.dma("sp", Fd.ap()[l], Fsb[:], r=[FsbB], w=[FdB])
            for h in range(8):
                src = bass.AP(Fd, (l * 8 + h) * 768, [[1, 128], [128, 5], [1, 128]])
                P.dma("sp", Hk[:], src, r=[FdB], w=[HkB])
                P.op("act", lambda e, h=h: e.activation(out=expB[:, h, :, :], in_=_rev_last(Hk[:], 128), func=AF.Exp),
                     r=[HkB], w=[expBB])
            P.op("dve", lambda e: e.tensor_tensor(
                out=expB[:], in0=expB[:],
                in1=amask_sb[:].rearrange("p (b q) -> p b q", b=5)[:, None, :, :].to_broadcast([128, 8, 5, 128]),
                op=ALU.mult), r=[expBB, B_const], w=[expBB])

            barrier(P)
            es2.close()
            if do_s5:
                ctx["s5_setup"](l, es, dict(vecT=vecT, vecB=vecB, ldtT=ldtT, ldtB=ldtB, sdT=sdT, sdB=sdB, uT32=uT32,
                                            uTb=uTb, uTB=uTB, YT=YT, YTB=YTB, mt=mt, mtB=mtB))
            unit = [0]
            sbank = [0]

            def evac(i, out_ap, in_ap, r, w):
                if i % 2 == 0:
                    P.op("act", lambda e: e.copy(out=out_ap, in_=in_ap), r=r, w=w)
                else:
                    P.op("dve", lambda e: e.tensor_copy(out_ap, in_ap), r=r, w=w)

            mmc = [0]

            def mmbank():
                mmc[0] += 1
                return mmc[0] % 2

            for s in range(NT):
                X, XB = xt[0], xtB[0]
                P.dma("sp", X[:], xT_v[:, :, s * 512:(s + 1) * 512], r=[xT_b[s]], w=[XB])
                rms_stats(P, pb, pbB, mmbank(), ones_bf, B_const, X, XB, sq, sqB, rs, rsB)
                for k in range(8):
                    T_, TB_ = tmp[k % 2], tmpB[k % 2]
                    P.op("dve", lambda e, k=k, T_=T_: e.scalar_tensor_tensor(
                        out=T_[:], in0=X[:, k, :], scalar=gm1(k), in1=rs[:], op0=ALU.mult, op1=ALU.mult),
                        r=[XB, rsB, gmB], w=[TB_])
                    P.op("act", lambda e, k=k, T_=T_: e.activation(out=hT[:, k, :], in_=T_[:], func=AF.Identity,
                                                                    bias=sh1(k), scale=1.0),
                         r=[TB_, modB], w=[hTB])
                ring0 = (4 * s) % 8
                fm = [("q", c, c * 128) for c in range(4)] + [("k", c, 512 + c * 128) for c in range(4)] + \
                     [("u", c, 1536 + c * 128) for c in range(2)]
                for i, (kind, c, col) in enumerate(fm):
                    bk = mmbank()
                    for kk in range(8):
                        P.op("pe", lambda e, kk=kk, col=col, bk=bk: e.matmul(
                            pb[bk][:], lhsT=w_in_sb[:, kk, col:col + 128], rhs=hT[:, kk, :], start=(kk == 0),
                            stop=(kk == 7)), r=[winB, hTB], w=[pbB[bk]], inc=(kk == 7))
                    if kind == "q":
                        evac(0, qT[0:64, 0, c, :], pb[bk][0:64, :], [pbB[bk]], [qTB])
                        evac(1, qT[64:128, 1, c, :], pb[bk][64:128, :], [pbB[bk]], [qTB])
                    elif kind == "k":
                        evac(i, kT[:, c, ring0 * 128:ring0 * 128 + 512], pb[bk][:], [pbB[bk]], kTB[ring0:ring0 + 4])
                    else:
                        P.op("act", lambda e, c=c, bk=bk: e.copy(out=uT32[:, c, :], in_=pb[bk][:]), r=[pbB[bk]], w=[uTB])
                        P.op("dve", lambda e, c=c, bk=bk: e.tensor_copy(uTb[:, c, :], pb[bk][:]), r=[pbB[bk]], w=[uTB])
                for t in range(4):
                    gb = 4 * s + t
                    slot = gb % 8
                    tok = slice(t * 128, (t + 1) * 128)
                    bk = mmbank()
                    for kk in range(8):
                        P.op("pe", lambda e, kk=kk, bk=bk, tok=tok: e.matmul(
                            pb[bk][:], lhsT=hT[:, kk, tok], rhs=w_in_sb[:, kk, 1024:1536], start=(kk == 0),
                            stop=(kk == 7)), r=[winB, hTB], w=[pbB[bk]], inc=(kk == 7))
                    P.op("act", lambda e, bk=bk, slot=slot: e.copy(
                        out=Vr[:, slot, :, 0:64], in_=pb[bk][:].rearrange("p (h d) -> p h d", h=8)),
                        r=[pbB[bk]], w=[VrB[slot]])

                def attn_gen(P):
                    for t in range(4):
                        gb = 4 * s + t
                        tok = slice(t * 128, (t + 1) * 128)
                        nbk = min(5, gb + 1)
                        b0 = 5 - nbk
                        info = {}

                        def stage_a(h, gb=gb, tok=tok, nbk=nbk, b0=b0, info=info):
                            c = h // 2
                            u = unit[0]; unit[0] += 1
                            PT, PTB = Pt[u % 3], PtB[u % 3]
                            info[h] = (PT, PTB)
                            pieces = ([(b0, 4)] if nbk > 1 else []) + [(4, 5)]
                            for (ba_, bb_) in pieces:
                                bank = 2 + sbank[0] % 2
                                sbank[0] += 1
                                for b in range(ba_, bb_):
                                    kslot = (gb - 4 + b) % 8
                                    P.op("pe", lambda e, b=b, kslot=kslot, c=c, h=h, bank=bank, ba_=ba_: e.matmul(
                                        pb[bank][:, (b - ba_) * 128:(b - ba_ + 1) * 128],
                                        lhsT=kT[:, c, kslot * 128:(kslot + 1) * 128],
                                        rhs=qT[:, h % 2, c, tok], start=True, stop=True),
                                        r=[kTB[kslot], qTB], w=[pbB[bank]], inc=(b == bb_ - 1))
                                P.op("act", lambda e, bank=bank, PT=PT, ba_=ba_, bb_=bb_: e.activation(
                                    out=PT[:, ba_:bb_, :].rearrange("p a b -> p (a b)"),
                                    in_=pb[bank][:, 0:(bb_ - ba_) * 128], func=AF.Exp, scale=0.125),
                                    r=[pbB[bank]], w=[PTB])
                            P.op("dve", lambda e, PT=PT, h=h: e.tensor_tensor(
                                out=PT[:, b0:5, :], in0=PT[:, b0:5, :], in1=expB[:, h, b0:5, :], op=ALU.mult),
                                r=[PTB, expBB], w=[PTB])

                        def stage_b(h, gb=gb, b0=b0, info=info):
                            PT, PTB = info[h]
                            for b in range(b0, 5):
                                kslot = (gb - 4 + b) % 8
                                P.op("pe", lambda e, b=b, kslot=kslot, h=h, PT=PT: e.matmul(
                                    pb[5][:, (h % 4) * 65:(h % 4) * 65 + 65], lhsT=PT[:, b, :],
                                    rhs=Vr[:, kslot, h, :], start=(b == b0), stop=(b == 4)),
                                    r=[PTB, VrB[kslot]], w=[pbB[5]], inc=(b == 4))
                            if h % 4 == 3:
                                hh = h // 4
                                pvv = pb[5][:, 0:260].rearrange("p (h d) -> p h d", h=4)
                                P.op("dve", lambda e, pvv=pvv, hh=hh: e.reciprocal(
                                    rec[:, hh * 4:hh * 4 + 4], pvv[:, :, 64]), r=[pbB[5]], w=[recB])
                                P.op("dve", lambda e, pvv=pvv, hh=hh: e.tensor_tensor(
                                    out=ya[:, hh * 4:hh * 4 + 4, :], in0=pvv[:, :, 0:64],
                                    in1=rec[:, hh * 4:hh * 4 + 4, None].to_broadcast([128, 4, 64]), op=ALU.mult),
                                    r=[pbB[5], recB], w=[yaB])

                        stage_a(0)
                        yield
                        for h in range(1, 8):
                            stage_a(h)
                            stage_b(h - 1)
                            yield
                        stage_b(7)
                        P.begin()
                        for c in range(4):
                            P.op("pe", lambda e, c=c: e.transpose(
                                pb7b[:, c * 128:(c + 1) * 128],
                                ya[:, 2 * c:2 * c + 2, :].rearrange("p a b -> p (a b)"), identb[:]),
                                r=[yaB, B_const], w=[pbB[7]], inc=(c == 3))
                        P.op("act", lambda e, tok=tok: e.copy(out=YT[:, 0:4, tok],
                                                              in_=pb7b[:, 0:512].rearrange("p (c q) -> p c q", c=4)),
                             r=[pbB[7]], w=YTB[0:4])
                        P.end()
                        yield

                def ret_gen(P):
                    for t in range(4):
                        gb = 4 * s + t
                        slot = gb % 8
                        tok = slice(t * 128, (t + 1) * 128)
                        if do_ret:
                            bq = 4
                            for kk in range(8):
                                P.op("pe", lambda e, kk=kk, bq=bq: e.matmul(
                                    pb[bq][:], lhsT=hT[:, kk, tok], rhs=w_in_sb[:, kk, 1792:2304], start=(kk == 0),
                                    stop=(kk == 7)), r=[winB, hTB], w=[pbB[bq]], inc=(kk == 7))
                            CS, CSB = cs[gb % 2], csB[gb % 2]
                            P.dma("sp", CS[:], Cd["retcs"].ap()[gb * 128:(gb + 1) * 128, :], w=[CSB])
                            srcv = pb[bq][:].rearrange("p (h two f) -> p h two f", h=8, two=2)
                            x1 = srcv[:, :, 0, :]
                            x2 = srcv[:, :, 1, :]
                            cosv = CS[:, 0:256].rearrange("p (h f) -> p h f", h=8)
                            sinv = CS[:, 256:512].rearrange("p (h f) -> p h f", h=8)
                            for (lo_, a1, b1, a2, b2, op_) in ((0, x1, cosv, x2, sinv, ALU.subtract), (32, x1, sinv, x2, cosv, ALU.add)):
                                P.op("dve", lambda e: e.tensor_tensor(out=rm[0], in0=a1, in1=b1, op=ALU.mult),
                                     r=[pbB[bq], CSB], w=[rmB[0]])
                                P.op("dve", lambda e: e.tensor_tensor(out=rm[1], in0=a2, in1=b2, op=ALU.mult),
                                     r=[pbB[bq], CSB], w=[rmB[1]])
                                P.op("dve", lambda e: e.tensor_tensor(out=qkrot[:, :, lo_:lo_ + 32], in0=rm[0], in1=rm[1], op=op_),
                                     r=[rmB[0], rmB[1]], w=[qkrotB])
                            yield
                            if RS >= 2:
                                bv = 4
                                for kk in range(8):
                                    P.op("pe", lambda e, kk=kk, bv=bv: e.matmul(
                                        pb[bv][:], lhsT=hT[:, kk, tok], rhs=w_in_sb[:, kk, 2304:2816], start=(kk == 0),
                                        stop=(kk == 7)), r=[winB, hTB], w=[pbB[bv]], inc=(kk == 7))
                                P.op("act", lambda e, bv=bv: e.copy(out=vr[:], in_=pb[bv][:, 0:256]), r=[pbB[bv]], w=[vrB])
                                P.op("act", lambda e, bv=bv: e.activation(out=sg[:], in_=pb[bv][:, 256:512], func=AF.Silu),
                                 r=[pbB[bv]], w=[sgB])
                        if do_ret:
                            yield
                            if RS >= 3:
                                P.begin()
                                for c in range(4):
                                    P.op("pe", lambda e, c=c: e.transpose(
                                        pb7b[:, 512 + c * 128:512 + (c + 1) * 128],
                                        qkrot[:, 2 * c:2 * c + 2, :].rearrange("p a b -> p (a b)"), identb[:]),
                                        r=[qkrotB, B_const], w=[pbB[7]], inc=(c == 3))
                                P.op("act", lambda e: e.copy(out=qTr[0:64, 0, :, :].rearrange("p a b -> p (a b)"),
                                                             in_=pb7b[0:64, 512:768]), r=[pbB[7]], w=[qkTB])
                                P.op("act", lambda e: e.copy(out=qTr[64:128, 1, :, :].rearrange("p a b -> p (a b)"),
                                                             in_=pb7b[64:128, 512:768]), r=[pbB[7]], w=[qkTB])
                                P.op("dve", lambda e: e.tensor_copy(kTr[:].rearrange("p a b -> p (a b)"), pb7b[:, 768:1024]),
                                     r=[pbB[7]], w=[qkTB])
                                P.end()
                            yield
                            if RS >= 4:
                                ba = 4
                                for h in range(4):
                                    c, pbase = h // 2, 64 * (h % 2)
                                    P.op("pe", lambda e, h=h, c=c, pbase=pbase, ba=ba: e.matmul(
                                        pb[ba][:, h * 128:(h + 1) * 128], lhsT=kTr[:, c, :],
                                        rhs=qTr[:, h % 2, c, :], start=True, stop=True),
                                        r=[qkTB], w=[pbB[ba]], inc=(h == 3))
                                if RS >= 4.5: P.op("dve", lambda e, ba=ba: e.tensor_tensor(
                                    out=Am[:], in0=pb[ba][:].rearrange("p (h i) -> p h i", h=4),
                                    in1=tri_sb[:, None, :].to_broadcast([128, 4, 128]), op=ALU.mult),
                                    r=[pbB[ba], B_const], w=[AmB])
                            yield
                            if RS >= 5:
                                by = 4
                                for h in range(4):
                                    c, pbase = h // 2, 64 * (h % 2)
                                    P.op("pe", lambda e, h=h, by=by: e.matmul(
                                        pb[by][:, h * 64:(h + 1) * 64], lhsT=Am[:, h, :], rhs=vr[:, h * 64:(h + 1) * 64],
                                        start=True, stop=False), r=[AmB, vrB], w=[pbB[by]], inc=False)
                                    P.op("pe", lambda e, h=h, c=c, pbase=pbase, by=by: e.matmul(
                                        pb[by][:, h * 64:(h + 1) * 64], lhsT=qTr[:, h % 2, c, :],
                                        rhs=Sbf[:, c, :], start=False, stop=True),
                                        r=[qkTB, SbfB], w=[pbB[by]], inc=(h == 3))
                                for c in range(2):
                                    P.op("pe", lambda e, c=c, by=by: e.matmul(
                                        pb[by][:, 256 + c * 128:256 + (c + 1) * 128],
                                        lhsT=qkrot[:, 4 + 2 * c:6 + 2 * c, :].rearrange("p a b -> p (a b)"),
                                        rhs=vr[:, c * 128:(c + 1) * 128], start=True, stop=True),
                                        r=[qkrotB, vrB], w=[pbB[by]], inc=(c == 1))
                            if RS >= 5.2:
                                P.op("act", lambda e, by=by: e.copy(out=ysb[:], in_=pb[by][:, 0:256]), r=[pbB[by]], w=[ysbB])
                                for hh in range(2):
                                    ps_ = slice(64 * hh, 64 * hh + 64)
                                    if hh == 0:
                                        P.op("act", lambda e, by=by: e.copy(out=ysq[:], in_=pb[by][:, 256:512]),
                                             r=[pbB[by]], w=[ysqB])
                                    kvv = ysq[ps_, :].rearrange("p (c x) -> p c x", c=2)[:, :, 64 * hh:64 * hh + 64]
                                    if RS >= 5.4 + 0.2 * hh: P.op("dve", lambda e, ps_=ps_, kvv=kvv: e.tensor_tensor(
                                        out=Sst[ps_, :, :], in0=kvv, in1=Sst[ps_, :, :], op=ALU.add),
                                        r=[ysqB, SstB], w=[SstB])
                                    if RS >= 5.5 + 0.2 * hh: P.op("dve", lambda e, ps_=ps_: e.tensor_tensor(
                                        out=Sst[ps_, :, :], in0=Sst[ps_, :, :],
                                        in1=retG_sb[ps_, :].rearrange("p (c x) -> p c x", c=2), op=ALU.mult),
                                        r=[SstB, B_const], w=[SstB])
                                P.op("act", lambda e: e.copy(out=Sbf[:].rearrange("p a b -> p (a b)"),
                                                             in_=Sst[:].rearrange("p a b -> p (a b)")), r=[SstB], w=[SbfB])
                            yield
                            if RS >= 7:
                                y3 = ysb[:].rearrange("p (h x) -> p h x", h=4)
                                P.op("dve", lambda e: e.tensor_reduce(out=gst[:, 0:4], in_=y3, axis=AX.X, op=ALU.add),
                                     r=[ysbB], w=[gstB])
                                P.op("act", lambda e: e.activation(out=ysq[:], in_=ysb[:], func=AF.Square), r=[ysbB], w=[ysqB])
                                P.op("dve", lambda e: e.tensor_reduce(out=gst[:, 4:8], in_=ysq[:].rearrange("p (h x) -> p h x", h=4),
                                                                      axis=AX.X, op=ALU.add), r=[ysqB], w=[gstB])
                                P.op("dve", lambda e: e.tensor_scalar(out=gst[:, 0:4], in0=gst[:, 0:4], scalar1=1.0 / 64, scalar2=None,
                                                                      op0=ALU.mult), r=[gstB], w=[gstB])
                                P.op("dve", lambda e: e.tensor_tensor(out=gst[:, 8:12], in0=gst[:, 0:4], in1=gst[:, 0:4],
                                                                      op=ALU.mult), r=[gstB], w=[gstB])
                                P.op("dve", lambda e: e.scalar_tensor_tensor(out=gst[:, 12:16], in0=gst[:, 4:8], scalar=1.0 / 64,
                                                                             in1=gst[:, 8:12], op0=ALU.mult, op1=ALU.subtract),
                                     r=[gstB], w=[gstB])
                                P.op("act", lambda e: e.activation(out=gst[:, 12:16], in_=gst[:, 12:16], func=AF.Sqrt, bias=EPS,
                                                                   scale=1.0), r=[gstB], w=[gstB])
                                P.op("dve", lambda e: e.reciprocal(gst[:, 12:16], gst[:, 12:16]), r=[gstB], w=[gstB])
                                P.op("dve", lambda e: e.tensor_tensor(out=y3, in0=y3,
                                                                      in1=gst[:, 0:4, None].to_broadcast([128, 4, 64]),
                                                                      op=ALU.subtract), r=[ysbB, gstB], w=[ysbB])
                                P.op("dve", lambda e: e.tensor_tensor(out=y3, in0=y3,
                                                                      in1=gst[:, 12:16, None].to_broadcast([128, 4, 64]),
                                                                      op=ALU.mult), r=[ysbB, gstB], w=[ysbB])
                                P.op("dve", lambda e: e.tensor_tensor(out=ysb[:], in0=ysb[:], in1=gn_bc[:], op=ALU.mult),
                                     r=[ysbB, gnB], w=[ysbB])
                                P.op("dve", lambda e: e.tensor_tensor(out=yr[:], in0=ysb[:], in1=sg[:], op=ALU.mult),
                                     r=[ysbB, sgB], w=[yrB])
                            yield
                            if RS >= 8:
                                P.begin()
                                for c in range(2):
                                    P.op("pe", lambda e, c=c: e.transpose(
                                        pb7b[:, c * 128:(c + 1) * 128], yr[:, c * 128:(c + 1) * 128], identb[:]),
                                        r=[yrB, B_const], w=[pbB[7]], inc=(c == 1))
                                P.op("act", lambda e: e.copy(out=YT[:, 6:8, tok],
                                                             in_=pb7b[:, 0:256].rearrange("p (c q) -> p c q", c=2)),
                                     r=[pbB[7]], w=YTB[6:8])
                                P.end()

                        yield
                recs = []
                for mk in ((attn_gen if do_attn else None), ((lambda PP: ctx["s5_gen"](s, PP)) if do_s5 else None),
                           (ret_gen if do_ret else None)):
                    if mk is not None:
                        d = Deferred()
                        for _ in mk(d):
                            pass
                        recs.append(d)
                run_interleaved(P, recs)
                if "YT" in tapset:
                    for kk in range(8):
                        P.op("dve", lambda e, kk=kk: e.tensor_copy(ctx["tapst"][:], YT[:, kk, :]), r=[YTB[kk]],
                             w=[ctx["tapstB"]])
                        P.dma("sp", ctx["tapYT"].ap()[l][kk * 128:(kk + 1) * 128, s * 512:(s + 1) * 512],
                              ctx["tapst"][:], r=[ctx["tapstB"]], w=[ctx["tapYTB"]])
                for d in range(8):
                    bk = mmbank()
                    for kk in range(8):
                        P.op("pe", lambda e, kk=kk, d=d, bk=bk: e.matmul(
                            pb[bk][:], lhsT=w_out_sb[:, kk, d * 128:(d + 1) * 128], rhs=YT[:, kk, :], start=(kk == 0),
                            stop=(kk == 7)), r=[woutB, YTB[kk]], w=[pbB[bk]], inc=(kk == 7))
                    P.op("dve", lambda e, d=d, bk=bk: e.scalar_tensor_tensor(
                        out=X[:, d, :], in0=pb[bk][:], scalar=g1(d), in1=X[:, d, :], op0=ALU.mult, op1=ALU.add),
                        r=[pbB[bk], XB, modB], w=[XB])
                P.dma("sp", x1T_v[:, :, s * 512:(s + 1) * 512], X[:], r=[XB], w=[x1T_b[s]])
        barrier(P)
        if ctx["do_moe"]:
            ctx["moe_phase"](l, dict(sh2=sh2, g2=g2, gm2=gm2, modB=modB, gmB=gmB))
        else:
            for s in range(16 * NT):
                P.dma("sp", xt0[:], x1T_v[:, :, s * 32:(s + 1) * 32], r=[x1T_b[s // 16]], w=[xt0B])
                P.dma("sp", xT_v[:, :, s * 32:(s + 1) * 32], xt0[:], r=[xt0B], w=[xT_b[s // 16]])
        barrier(P)
    return xT_v, xT_b


_CACHE = {}


def _get_program(S, L):
    key = (S, L)
    if key not in _CACHE:
        nc = bass.Bass("TRN2", target_bir_lowering=False)
        P, _ = build_program(nc, S, L)
        _CACHE[key] = nc
    return _CACHE[key]


def kernel(**inputs):
    x = np.asarray(inputs["x"], dtype=np.float32)
    B, S, _ = x.shape
    L = int(np.asarray(inputs["w_in"]).shape[0])
    nc = _get_program(S, L)
    cst = host_constants(S)
    shared = {k: np.ascontiguousarray(np.asarray(inputs[k], dtype=np.float32)) for k in WEIGHT_SHAPES(L)}
    shared.update(cst)
    c = np.asarray(inputs["c"], dtype=np.float32)
    in_maps = []
    for b in range(B):
        m = dict(shared)
        m["x"] = np.ascontiguousarray(x[b])
        m["c"] = np.ascontiguousarray(c[b].reshape(8, 128))
        in_maps.append(m)
    res = run_bass_kernel_spmd(nc, in_maps, core_ids=list(range(B)))
    return np.stack([np.asarray(r["y"], dtype=np.float32) for r in res.results], axis=0)
```
